# Optimizing a Trainium2 kernel written in Bass

```python
import jax
import jax.numpy as jnp
from jax import lax
import numpy as np

D_MODEL = 2048
BATCH = 2
SEQ = 4096
DEPTH = 1
DEC_BATCH = 32
DEC_SEQ = 64
PAST_LEN = 4096

CHUNK = 64
HEAD_DIM = 128
N_HEADS_SB = 8
N_HEADS_BAND = 8
W_SB = N_HEADS_SB * HEAD_DIM
W_BAND = N_HEADS_BAND * HEAD_DIM
BAND_LEFT_CHUNKS = 8
BAND_PAST = BAND_LEFT_CHUNKS * CHUNK
REL_CLIP = 128
Q_BLOCK = 128
N_GROUPS = 4
EXPERTS_PER_GROUP = 8
N_EXPERTS = N_GROUPS * EXPERTS_PER_GROUP
TOP_K_INNER = 2
D_EXPERT = 512
EPS = 1e-6
SPLITS = [W_SB, 2 * W_SB, 3 * W_SB, 3 * W_SB + W_BAND, 3 * W_SB + 2 * W_BAND, 3 * W_SB + 3 * W_BAND, 3 * W_SB + 3 * W_BAND + D_MODEL]
D_IN = 3 * W_SB + 3 * W_BAND + 2 * D_MODEL

kernel_name = 'streaming_stickbreak_band_hmoe_step'


def rms_norm(x, g):
    xf = x.astype(jnp.float32)
    y = xf * lax.rsqrt(jnp.mean(xf * xf, axis=-1, keepdims=True) + EPS)
    return (y * g.astype(jnp.float32)).astype(x.dtype)


def ada_modulation(c, w_ada, b_ada):
    m = jax.nn.silu(c) @ w_ada + b_ada
    return jnp.split(m[:, None, :], 6, axis=-1)


def stick_breaking_block(q, k, v, q_pos, k_pos):
    z = jnp.einsum('bqhd,bkhd->bhqk', q, k).astype(jnp.float32) * (HEAD_DIM ** -0.5)
    valid = k_pos[None, :] < q_pos[:, None]
    log_keep = jnp.where(valid, jax.nn.log_sigmoid(-z), 0.0)
    tail = lax.cumsum(log_keep, axis=3, reverse=True) - log_keep
    a = jnp.where(valid, jnp.exp(jax.nn.log_sigmoid(z) + tail), 0.0)
    return jnp.einsum('bhqk,bkhd->bqhd', a, v.astype(jnp.float32))


def stick_breaking_prompt(q, k, v):
    b, s, h, d = q.shape
    nb = s // Q_BLOCK
    pos = jnp.arange(s)
    q_blocks = jnp.moveaxis(q.reshape(b, nb, Q_BLOCK, h, d), 1, 0)
    out = lax.map(lambda qp: stick_breaking_block(qp[0], k, v, qp[1], pos), (q_blocks, pos.reshape(nb, Q_BLOCK)))
    return jnp.moveaxis(out, 0, 1).reshape(b, s, h * d)


def stick_breaking_step(q, k_new, v_new, cache_k, cache_v):
    b, t, h, d = q.shape
    p = cache_k.shape[1]
    k = jnp.concatenate([cache_k, k_new.astype(cache_k.dtype)], axis=1)
    v = jnp.concatenate([cache_v, v_new.astype(cache_v.dtype)], axis=1)
    out = stick_breaking_block(q, k, v, p + jnp.arange(t), jnp.arange(p + t))
    return out.reshape(b, t, h * d)


def rel_bias(table, rel):
    return table[:, jnp.clip(rel, -REL_CLIP, REL_CLIP) + REL_CLIP].astype(jnp.float32)


def biased_attention(q, k, v, bias, valid):
    s = jnp.einsum('...qhd,...khd->...hqk', q, k).astype(jnp.float32) * (HEAD_DIM ** -0.5) + bias
    if valid is not None:
        s = jnp.where(valid, s, -jnp.inf)
    p = jax.nn.softmax(s, axis=-1)
    return jnp.einsum('...hqk,...khd->...qhd', p, v.astype(jnp.float32))


def band_prompt(q, k, v, table):
    b, s, h, d = q.shape
    nc = s // CHUNK
    span = (BAND_LEFT_CHUNKS + 1) * CHUNK

    def gather_band(x):
        xc = jnp.pad(x.reshape(b, nc, CHUNK, h, d), ((0, 0), (BAND_LEFT_CHUNKS, 0), (0, 0), (0, 0), (0, 0)))
        return jnp.concatenate([xc[:, i:i + nc] for i in range(BAND_LEFT_CHUNKS + 1)], axis=2)

    j = jnp.arange(span)
    rel = jnp.arange(CHUNK)[:, None] - j[None, :] + BAND_PAST
    k_pos = (jnp.arange(nc)[:, None] - BAND_LEFT_CHUNKS) * CHUNK + j[None, :]
    valid = (k_pos >= 0)[None, :, None, None, :]
    out = biased_attention(q.reshape(b, nc, CHUNK, h, d), gather_band(k), gather_band(v), rel_bias(table, rel), valid)
    return out.reshape(b, s, h * d)


def band_step(q, k_new, v_new, cache_k, cache_v, table):
    b, t, h, d = q.shape
    lb = cache_k.shape[1]
    k = jnp.concatenate([cache_k, k_new.astype(cache_k.dtype)], axis=1)
    v = jnp.concatenate([cache_v, v_new.astype(cache_v.dtype)], axis=1)
    rel = jnp.arange(t)[:, None] - jnp.arange(lb + t)[None, :] + lb
    out = biased_attention(q, k, v, rel_bias(table, rel), None)
    return out.reshape(b, t, h * d)


def hier_moe(h, w_rg, b_rg, w_re, b_re, w_gate, w_up, w_down):
    lead = h.shape[:-1]
    x = h.reshape(-1, D_MODEL)
    g_logits = (x @ w_rg).astype(jnp.float32) + b_rg.astype(jnp.float32)
    g_onehot = jax.nn.one_hot(jnp.argmax(g_logits, axis=-1), N_GROUPS, dtype=jnp.float32)
    g_weight = jnp.sum(jax.nn.softmax(g_logits, axis=-1) * g_onehot, axis=-1, keepdims=True)
    e_logits = (x @ w_re).astype(jnp.float32).reshape(-1, N_GROUPS, EXPERTS_PER_GROUP) + b_re.astype(jnp.float32)
    e_sel = jnp.einsum('ng,nge->ne', g_onehot, e_logits)
    top_v, top_i = lax.top_k(e_sel, TOP_K_INNER)
    inner = jnp.einsum('nk,nke->ne', jax.nn.softmax(top_v, axis=-1), jax.nn.one_hot(top_i, EXPERTS_PER_GROUP, dtype=jnp.float32))
    combine = (g_onehot[:, :, None] * (g_weight * inner)[:, None, :]).reshape(-1, N_EXPERTS)
    hidden = jax.nn.silu(jnp.einsum('nd,edf->nef', x, w_gate)) * jnp.einsum('nd,edf->nef', x, w_up)
    y = jnp.einsum('nef,efd->nd', hidden * combine[:, :, None].astype(hidden.dtype), w_down)
    return y.reshape(*lead, D_MODEL)


def trunk_layer(x, c, attend, norm_mix, norm_ffn, w_ada, b_ada, w_in, q_norm_band, k_norm_band, w_proj_sb, w_proj_band, w_out, w_rg, b_rg, w_re, b_re, w_gate, w_up, w_down):
    shift_m, scale_m, gate_m, shift_f, scale_f, gate_f = ada_modulation(c, w_ada, b_ada)
    h = rms_norm(x, norm_mix) * (1 + scale_m) + shift_m
    lead = h.shape[:-1]
    qa, ka, va, qb, kb, vb, ga, gb = jnp.split(h @ w_in, SPLITS, axis=-1)
    qa = qa.reshape(*lead, N_HEADS_SB, HEAD_DIM)
    ka = ka.reshape(*lead, N_HEADS_SB, HEAD_DIM)
    va = va.reshape(*lead, N_HEADS_SB, HEAD_DIM)
    qb = rms_norm(qb.reshape(*lead, N_HEADS_BAND, HEAD_DIM), q_norm_band)
    kb = rms_norm(kb.reshape(*lead, N_HEADS_BAND, HEAD_DIM), k_norm_band)
    vb = vb.reshape(*lead, N_HEADS_BAND, HEAD_DIM)
    o_sb, o_band = attend(qa, ka, va, qb, kb, vb)
    merged = jax.nn.sigmoid(ga) * (o_sb.astype(x.dtype) @ w_proj_sb) + jax.nn.sigmoid(gb) * (o_band.astype(x.dtype) @ w_proj_band)
    x = x + gate_m * (merged @ w_out)
    h2 = rms_norm(x, norm_ffn) * (1 + scale_f) + shift_f
    x = x + gate_f * hier_moe(h2, w_rg, b_rg, w_re, b_re, w_gate, w_up, w_down)
    return x, ka, va, kb, vb


def setup_inputs(seed: int = 0) -> dict:
    key = jax.random.key(seed)
    ks = jax.random.split(key, 26)

    def nrm(k, shape, scale):
        return jax.random.normal(k, shape, jnp.float32) * scale

    lb = min(BAND_PAST, PAST_LEN)
    L = DEPTH
    return {
        'x_prompt': nrm(ks[0], (BATCH, SEQ, D_MODEL), 1.0),
        'x_sample': nrm(ks[1], (DEC_BATCH, DEC_SEQ, D_MODEL), 1.0),
        'cache_sb_k': nrm(ks[2], (L, DEC_BATCH, PAST_LEN, N_HEADS_SB, HEAD_DIM), 1.0),
        'cache_sb_v': nrm(ks[3], (L, DEC_BATCH, PAST_LEN, N_HEADS_SB, HEAD_DIM), 1.0),
        'cache_band_k': nrm(ks[4], (L, DEC_BATCH, lb, N_HEADS_BAND, HEAD_DIM), 1.0),
        'cache_band_v': nrm(ks[5], (L, DEC_BATCH, lb, N_HEADS_BAND, HEAD_DIM), 1.0),
        'c_prompt': nrm(ks[6], (BATCH, D_MODEL), 1.0),
        'c_sample': nrm(ks[7], (DEC_BATCH, D_MODEL), 1.0),
        'norm_mix': 1.0 + nrm(ks[8], (L, D_MODEL), 0.1),
        'norm_ffn': 1.0 + nrm(ks[9], (L, D_MODEL), 0.1),
        'w_ada': nrm(ks[10], (L, D_MODEL, 6 * D_MODEL), D_MODEL ** -0.5),
        'b_ada': nrm(ks[11], (L, 6 * D_MODEL), 0.02),
        'w_in': nrm(ks[12], (L, D_MODEL, D_IN), D_MODEL ** -0.5),
        'q_norm_band': 1.0 + nrm(ks[13], (L, N_HEADS_BAND, HEAD_DIM), 0.1),
        'k_norm_band': 1.0 + nrm(ks[14], (L, N_HEADS_BAND, HEAD_DIM), 0.1),
        'rel_bias_band': nrm(ks[15], (L, N_HEADS_BAND, 2 * REL_CLIP + 1), 0.5),
        'w_proj_sb': nrm(ks[16], (L, W_SB, D_MODEL), W_SB ** -0.5),
        'w_proj_band': nrm(ks[17], (L, W_BAND, D_MODEL), W_BAND ** -0.5),
        'w_out': nrm(ks[18], (L, D_MODEL, D_MODEL), D_MODEL ** -0.5),
        'w_router_group': nrm(ks[19], (L, D_MODEL, N_GROUPS), D_MODEL ** -0.5),
        'b_router_group': nrm(ks[20], (L, N_GROUPS), 0.01),
        'w_router_expert': nrm(ks[21], (L, D_MODEL, N_EXPERTS), D_MODEL ** -0.5),
        'b_router_expert': nrm(ks[22], (L, N_GROUPS, EXPERTS_PER_GROUP), 0.01),
        'w_gate': nrm(ks[23], (L, N_EXPERTS, D_MODEL, D_EXPERT), D_MODEL ** -0.5),
        'w_up': nrm(ks[24], (L, N_EXPERTS, D_MODEL, D_EXPERT), D_MODEL ** -0.5),
        'w_down': nrm(ks[25], (L, N_EXPERTS, D_EXPERT, D_MODEL), D_EXPERT ** -0.5),
    }


def reference(x_prompt, x_sample, cache_sb_k, cache_sb_v, cache_band_k, cache_band_v, c_prompt, c_sample, norm_mix, norm_ffn, w_ada, b_ada, w_in, q_norm_band, k_norm_band, rel_bias_band, w_proj_sb, w_proj_band, w_out, w_router_group, b_router_group, w_router_expert, b_router_expert, w_gate, w_up, w_down):
    n_band_prompt = min(BAND_PAST, x_prompt.shape[1])
    xp, xs = x_prompt, x_sample
    p_sb_k, p_sb_v, p_band_k, p_band_v = [], [], [], []
    s_sb_k, s_sb_v, s_band_k, s_band_v = [], [], [], []
    for l in range(DEPTH):
        table = rel_bias_band[l]
        layer_w = (norm_mix[l], norm_ffn[l], w_ada[l], b_ada[l], w_in[l], q_norm_band[l], k_norm_band[l],
                   w_proj_sb[l], w_proj_band[l], w_out[l], w_router_group[l], b_router_group[l],
                   w_router_expert[l], b_router_expert[l], w_gate[l], w_up[l], w_down[l])

        def prompt_attend(qa, ka, va, qb, kb, vb, table=table):
            return stick_breaking_prompt(qa, ka, va), band_prompt(qb, kb, vb, table)

        def sample_attend(qa, ka, va, qb, kb, vb, table=table, l=l):
            return (stick_breaking_step(qa, ka, va, cache_sb_k[l], cache_sb_v[l]),
                    band_step(qb, kb, vb, cache_band_k[l], cache_band_v[l], table))

        xp, ka, va, kb, vb = trunk_layer(xp, c_prompt, prompt_attend, *layer_w)
        p_sb_k.append(ka)
        p_sb_v.append(va)
        p_band_k.append(kb[:, -n_band_prompt:])
        p_band_v.append(vb[:, -n_band_prompt:])
        xs, ka, va, kb, vb = trunk_layer(xs, c_sample, sample_attend, *layer_w)
        s_sb_k.append(ka)
        s_sb_v.append(va)
        s_band_k.append(kb)
        s_band_v.append(vb)
    return (xp, xs, jnp.stack(p_sb_k, axis=0), jnp.stack(p_sb_v, axis=0), jnp.stack(p_band_k, axis=0), jnp.stack(p_band_v, axis=0), jnp.stack(s_sb_k, axis=0), jnp.stack(s_sb_v, axis=0), jnp.stack(s_band_k, axis=0), jnp.stack(s_band_v, axis=0))
```

```python
import numpy as np
from contextlib import ExitStack
from itertools import zip_longest
import concourse.bass as bass
import concourse.mybir as mybir
from concourse.bass_utils import run_bass_kernel_spmd

F32 = mybir.dt.float32
BF16 = mybir.dt.bfloat16
AF = mybir.ActivationFunctionType
ALU = mybir.AluOpType

D = 2048
NCH = 16
DIN = 10240
NPRE = 24
NOWN = 1280
SC = 128 ** -0.5
EPS = 1e-6
NEG = -30000.0
ENGS = ("pe", "act", "dve", "pool", "sp")
EPOCH = 3000
BANKS = {f"b{i}" for i in range(8)}
TBS = [(0, 512), (512, 512), (1024, 256)]


class Rec:
    def __init__(self, nc):
        self.nc = nc
        self.ops = []
        self.last_w = {}
        self.readers = {}
        self.dma_keys = []
        self.fence = None
        self.last_eng = {}
        self.last_dma = {}

    def add(self, eng, fn, reads=(), writes=(), dma=None):
        writes = list(writes) + [r for r in reads if r in BANKS]
        reads = [r for r in reads if r not in BANKS]
        oid = len(self.ops)
        deps = set()
        if self.fence is not None:
            deps.add(self.fence)
        for r in reads:
            w = self.last_w.get(r)
            if w is not None:
                deps.add(w)
        for w_ in writes:
            w = self.last_w.get(w_)
            if w is not None:
                deps.add(w)
            deps.update(self.readers.get(w_, {}).values())
        rk = ("d", dma) if dma is not None else ("e", eng)
        for r in reads:
            self.readers.setdefault(r, {})[rk] = oid
        for w_ in writes:
            self.last_w[w_] = oid
            self.readers[w_] = {}
        if dma is not None:
            if dma not in self.dma_keys:
                self.dma_keys.append(dma)
            self.last_dma[dma] = oid
        else:
            self.last_eng[eng] = oid
        self.ops.append(dict(eng=eng, fn=fn, deps=deps, dma=dma))
        return oid

    def barrier(self, fn):
        oid = len(self.ops)
        deps = set(self.last_eng.values()) | set(self.last_dma.values())
        if self.fence is not None:
            deps.add(self.fence)
        self.ops.append(dict(eng="dve", fn=fn, deps=deps, dma=None))
        self.last_eng["dve"] = oid
        self.fence = oid

    def emit(self, stack):
        nc = self.nc
        ops = self.ops
        src = set()
        for o in ops:
            for d in o["deps"]:
                od = ops[d]
                if od["dma"] is None and od["eng"] == "pe" and o["eng"] == "pe" and o["dma"] is None:
                    continue
                src.add(d)
        cnt = {e: 0 for e in ENGS}
        dcnt = {k: 0 for k in self.dma_keys}
        tick = {}
        for i, o in enumerate(ops):
            if o["dma"] is not None:
                dcnt[o["dma"]] += 16
                tick[i] = ("d", o["dma"], 0, dcnt[o["dma"]])
            elif i in src:
                c = cnt[o["eng"]]
                cnt[o["eng"]] += 1
                tick[i] = ("e", o["eng"], c // EPOCH, c % EPOCH + 1)
        sems = {}
        for e in ENGS:
            for ep in range(max(cnt[e] - 1, 0) // EPOCH + 1):
                sems[("e", e, ep)] = stack.enter_context(nc.semaphore(f"s_{e}_{ep}"))
        for k in self.dma_keys:
            assert dcnt[k] < 60000, (k, dcnt[k])
            sems[("d", k, 0)] = stack.enter_context(nc.semaphore(f"d_{k}"))
        engobj = {"pe": nc.tensor, "act": nc.scalar, "dve": nc.vector, "pool": nc.gpsimd, "sp": nc.sync}
        per = {e: [] for e in ENGS}
        for i, o in enumerate(ops):
            per[o["eng"]].append(i)
        final = dict(dcnt)

        def run_engine(e):
            eng = engobj[e]
            seen = {}
            for i in per[e]:
                o = ops[i]
                need = {}
                for d in o["deps"]:
                    if d not in tick:
                        continue
                    kind, key, ep, val = tick[d]
                    if kind == "e" and key == e and e == "pe" and o["dma"] is None:
                        continue
                    sk = (kind, key, ep)
                    if seen.get(sk, 0) >= val:
                        continue
                    if need.get(sk, 0) < val:
                        need[sk] = val
                for sk, val in need.items():
                    eng.wait_ge(sems[sk], val)
                    seen[sk] = val
                ins = o["fn"](eng)
                if i in tick:
                    kind, key, ep, val = tick[i]
                    if kind == "d":
                        ins.then_inc(sems[("d", key, 0)], 16)
                    else:
                        ins.then_inc(sems[("e", key, ep)], 1)
            if e == "sp":
                for k, v in final.items():
                    if v > 0:
                        eng.wait_ge(sems[("d", k, 0)], v)

        with nc.Block() as block:
            @block.tensor
            def _(t):
                run_engine("pe")

            @block.scalar
            def _(t):
                run_engine("act")

            @block.vector
            def _(t):
                run_engine("dve")

            @block.gpsimd
            def _(t):
                run_engine("pool")

            @block.sync
            def _(t):
                run_engine("sp")


class Arena:
    def __init__(self, nc, nbytes):
        self.t = nc.alloc_sbuf_tensor("arena", [128, nbytes], mybir.dt.uint8)
        self.n = nbytes
        self.off = 0

    def mark(self):
        return self.off

    def set(self, off):
        self.off = off

    def release(self, m):
        self.off = m

    def a(self, cols, dt):
        nb = cols * (4 if dt == F32 else 2)
        nb = (nb + 63) // 64 * 64
        assert self.off + nb <= self.n, ("SBUF overflow", self.off, nb)
        ap = self.t[:, self.off:self.off + cols * (4 if dt == F32 else 2)].bitcast(dt)
        self.off += nb
        return ap


_DBG = {}


class _Stop(Exception):
    pass


def build(upto=None, dbg=False):
    nc = bass.Bass("TRN2", target_bir_lowering=False)

    def din(name, shape, dt=F32):
        return nc.dram_tensor(name, list(shape), dt, kind="ExternalInput").ap()

    def dout(name, shape):
        return nc.dram_tensor(name, list(shape), F32, kind="ExternalOutput").ap()

    x_tok = din("x_tok", [34 * 128, D])
    kbias_d = din("kbias", [128, 32])
    c5 = din("c5", [5, D])
    csk = din("csk", [4, 4096, 1024])
    csv = din("csv", [4, 4096, 1024])
    cbk = din("cbk", [4, 512, 1024])
    cbv = din("cbv", [4, 512, 1024])
    btile = din("btile", [128, 24, 128])
    norm_mix = din("norm_mix", [16, 128])
    norm_ffn = din("norm_ffn", [16, 128])
    w_ada = din("w_ada", [D, 6 * D])
    b_ada = din("b_ada", [96, 128])
    w_in = din("w_in", [D, DIN])
    qn_d = din("q_norm", [8, 128])
    kn_d = din("k_norm", [8, 128])
    w_psb = din("w_psb", [1024, D])
    w_pbd = din("w_pbd", [1024, D])
    w_out = din("w_out", [D, D])
    w_rt = din("w_rt", [D, 36])
    b_rt = din("b_rt", [1, 36])
    w_gate = din("w_gate", [32, D, 512])
    w_up = din("w_up", [32, D, 512])
    w_down = din("w_down", [32, 512, D])

    y_out = dout("y_out", [NOWN, D])
    ksb_out = dout("ksb_out", [NOWN, 1024])
    vsb_out = dout("vsb_out", [NOWN, 1024])
    kbd_out = dout("kbd_out", [NOWN, 1024])
    vbd_out = dout("vbd_out", [NOWN, 1024])

    kk = dict(kind="ExternalOutput") if dbg else {}
    KTs = nc.dram_tensor("KTs", [8, 128, 4096], BF16, **kk).ap()
    Vs = nc.dram_tensor("Vs", [8, 4096, 128], BF16, **kk).ap()
    dbg_outs = {}

    def DUMP(R, name, ap, cols, dt, res):
        if not dbg:
            return
        d = nc.dram_tensor("dbg_" + name, [128, cols], dt, kind="ExternalOutput").ap()
        dbg_outs[name] = d
        R.add("sp", lambda e: e.dma_start(out=d, in_=ap), reads=[res], dma="dbg")

    st = ExitStack()
    with st:
        A = Arena(nc, 206 * 1024)
        banks = [nc.alloc_psum_tensor(f"bank{i}", [128, 512], F32) for i in range(8)]
        R = Rec(nc)
        try:
            bk = lambda i: banks[i][:, :]
            uid = [0]

            def U(p):
                uid[0] += 1
                return f"{p}{uid[0]}"

            identf = A.a(128, F32)
            identb = A.a(128, BF16)
            negtri = A.a(128, BF16)
            negones = A.a(128, BF16)
            onesb = A.a(128, BF16)
            dmask = A.a(128, BF16)
            tmpf = A.a(128, F32)
            epsc = A.a(1, F32)
            zeroc = A.a(1, F32)
            onec = A.a(1, F32)
            kbias = A.a(32, F32)
            modT = A.a(96 * 5, F32).rearrange("p (b s) -> p b s", s=5)
            gmm = A.a(80, F32).rearrange("p (b s) -> p b s", s=5)
            gmf = A.a(80, F32).rearrange("p (b s) -> p b s", s=5)
            nmix = A.a(16, F32)
            nffn = A.a(16, F32)
            qgs = A.a(8, F32)
            kg = A.a(8, F32)
            dummy = A.a(16, F32)

            R.add("pool", lambda e: e.memset(identf, 1.0), writes=["identf"])
            R.add("pool", lambda e: e.affine_select(out=identf, in_=identf, pattern=[[-1, 128]], compare_op=ALU.is_equal, fill=0.0, base=0, channel_multiplier=1), reads=["identf"], writes=["identf"])
            R.add("dve", lambda e: e.tensor_copy(out=identb, in_=identf), reads=["identf"], writes=["identb"])
            R.add("pool", lambda e: e.memset(tmpf, -1.0), writes=["tmpf"])
            R.add("pool", lambda e: e.affine_select(out=tmpf, in_=tmpf, pattern=[[-1, 128]], compare_op=ALU.is_ge, fill=0.0, base=0, channel_multiplier=1), reads=["tmpf"], writes=["tmpf"])
            R.add("dve", lambda e: e.tensor_copy(out=negtri, in_=tmpf), reads=["tmpf"], writes=["negtri"])
            R.add("pool", lambda e: e.memset(tmpf, 1.0), reads=["tmpf"], writes=["tmpf"])
            R.add("pool", lambda e: e.affine_select(out=tmpf, in_=tmpf, pattern=[[1, 128]], compare_op=ALU.is_gt, fill=0.0, base=0, channel_multiplier=-1), reads=["tmpf"], writes=["tmpf"])
            R.add("dve", lambda e: e.tensor_copy(out=dmask, in_=tmpf), reads=["tmpf"], writes=["dmask"])
            R.add("pool", lambda e: e.memset(negones, -1.0), writes=["negones"])
            R.add("pool", lambda e: e.memset(onesb, 1.0), writes=["onesb"])
            R.add("pool", lambda e: e.memset(epsc, EPS), writes=["epsc"])
            R.add("pool", lambda e: e.memset(zeroc, 0.0), writes=["zeroc"])
            R.add("pool", lambda e: e.memset(onec, 1.0), writes=["onec"])
            R.add("pool", lambda e: e.memset(dummy, 0.0), writes=["dummy"])
            R.add("sp", lambda e: e.dma_start(out=kbias, in_=kbias_d), writes=["kbias"], dma="kbias")
            stg = A.a(128, F32)

            def load_T(src_ap, rows, dst, post=None):
                n = U("ld")
                R.add("sp", lambda e: e.dma_start(out=stg[0:rows, :], in_=src_ap), writes=["stg"], dma="stg")
                R.add("pe", lambda e: e.transpose(out=banks[7][:, 0:rows], in_=stg[0:rows, :], identity=identf[0:rows, 0:rows]), reads=["stg", "identf"], writes=["b7"])
                if post is None:
                    R.add("dve", lambda e: e.tensor_copy(out=dst, in_=banks[7][:, 0:rows]), reads=["b7"], writes=[n, "cparam"])
                else:
                    R.add("dve", lambda e: e.tensor_scalar(out=dst, in0=banks[7][:, 0:rows], scalar1=post, scalar2=None, op0=ALU.mult), reads=["b7"], writes=[n, "cparam"])

            bT = A.a(96, F32)
            load_T(b_ada, 96, bT)
            load_T(norm_mix, 16, nmix)
            load_T(norm_ffn, 16, nffn)
            load_T(qn_d, 8, qgs, post=SC)
            load_T(kn_d, 8, kg)

            assert A.off <= 8192, A.off
            A.set(173 * 1024)
            scT = A.a(16 * 5, BF16).rearrange("p (k s) -> p k s", s=5)
            A.set(174 * 1024)
            wa = [A.a(16 * 512, BF16).rearrange("p (k n) -> p k n", n=512) for _ in range(2)]
            c_sb = wa[1].rearrange("p k n -> p (k n)")[:, 0:2 * D].bitcast(F32)
            R.add("sp", lambda e: e.dma_start(out=c_sb[0:5, :], in_=c5), writes=["wa1"], dma="c_sb")
            R.add("act", lambda e: e.activation(out=c_sb[0:5, :], in_=c_sb[0:5, :], func=AF.Silu), reads=["wa1"], writes=["wa1"])
            for k in range(16):
                R.add("pe", lambda e, k=k: e.transpose(out=banks[6][:, k * 5:(k + 1) * 5], in_=c_sb[0:5, k * 128:(k + 1) * 128], identity=identf[0:5, 0:5]), reads=["wa1", "identf"], writes=["b6"])
            R.add("dve", lambda e: e.tensor_copy(out=scT.rearrange("p k s -> p (k s)"), in_=banks[6][:, 0:80]), reads=["b6"], writes=["scT"])
            b5v = banks[5][:, 0:480].rearrange("p (b s) -> p b s", s=5)

            def ada_panel(pn):
                wb_ = wa[pn % 2]
                wn = f"wa{pn % 2}"
                R.add("pool", lambda e: e.dma_start(out=wb_, in_=w_ada[:, pn * 512:(pn + 1) * 512].rearrange("(k p) n -> p k n", p=128)), writes=[wn], dma=wn)
                for fb in range(4):
                    blk = pn * 4 + fb
                    for k in range(16):
                        R.add("pe", lambda e, fb=fb, k=k, blk=blk: e.matmul(banks[5][:, blk * 5:(blk + 1) * 5], lhsT=wb_[:, k, fb * 128:(fb + 1) * 128], rhs=scT[:, k, :], start=(k == 0), stop=(k == 15)), reads=[wn, "scT"], writes=["b5"])

            def ada_finish(b0, b1, tok):
                for s in range(5):
                    R.add("dve", lambda e, s=s: e.tensor_tensor(out=modT[:, b0:b1, s], in0=b5v[:, b0:b1, s], in1=bT[:, b0:b1], op=ALU.add), reads=["b5", "cparam"], writes=[tok])

            for pn in range(8):
                ada_panel(pn)
            ada_finish(0, 32, "modT1")
            for s in range(5):
                R.add("dve", lambda e, s=s: e.scalar_tensor_tensor(out=gmm[:, :, s], in0=modT[:, 16:32, s], scalar=1.0, in1=nmix, op0=ALU.add, op1=ALU.mult), reads=["modT1", "cparam"], writes=["gmm"])
            ada_next = [8]

            def ada_more(n):
                for _ in range(n):
                    if ada_next[0] < 24:
                        ada_panel(ada_next[0])
                        ada_next[0] += 1
                        if ada_next[0] == 24:
                            ada_finish(32, 96, "modT2")
                            for s in range(5):
                                R.add("dve", lambda e, s=s: e.scalar_tensor_tensor(out=gmf[:, :, s], in0=modT[:, 64:80, s], scalar=1.0, in1=nffn, op0=ALU.add, op1=ALU.mult), reads=["modT2", "cparam"], writes=["gmf"])


            KB = 1024
            HT0, OAT0, OBT0, PH0 = 8 * KB, 64 * KB, 84 * KB, 104 * KB
            HTC = NOWN + 512

            ncnt = [0]

            def norm_bufs():
                return dict(xts=[A.a(D, F32) for _ in range(2)], junk=A.a(D, BF16), ybs=[A.a(D, BF16) for _ in range(2)], ssq=A.a(4, F32))

            def norm_tile(NB, ti, segs, dst_fn):
                i = ncnt[0] % 2
                ncnt[0] += 1
                xt, yb, junk, ssq = NB["xts"][i], NB["ybs"][i], NB["junk"], NB["ssq"]
                xn, yn = f"xt{i}", f"yb{i}"
                R.add("sp", lambda e: e.dma_start(out=xt, in_=x_tok[ti * 128:(ti + 1) * 128, :]), writes=[xn], dma=xn)
                R.add("dve", lambda e: e.memset(ssq[:, 0:1], 0.0), writes=["ssq"])
                R.add("act", lambda e: e.activation(out=junk, in_=xt, func=AF.Square, accum_out=ssq[:, 0:1]), reads=[xn, "ssq"], writes=["junk", "ssq"])
                R.add("act", lambda e: e.activation(out=ssq[:, 1:2], in_=ssq[:, 0:1], func=AF.Ln, scale=1.0 / D, bias=epsc), reads=["ssq", "epsc"], writes=["ssq"])
                R.add("act", lambda e: e.activation(out=ssq[:, 2:3], in_=ssq[:, 1:2], func=AF.Exp, scale=-0.5), reads=["ssq"], writes=["ssq"])
                R.add("dve", lambda e: e.tensor_scalar(out=yb, in0=xt, scalar1=ssq[:, 2:3], scalar2=None, op0=ALU.mult), reads=[xn, "ssq"], writes=[yn])
                for half in range(2):
                    pb = banks[6 + half][:, 0:512].bitcast(BF16)
                    bn = f"b{6 + half}"
                    for k8 in range(8):
                        dc = half * 8 + k8
                        R.add("pe", lambda e, pb=pb, k8=k8, dc=dc: e.transpose(out=pb[:, k8 * 128:(k8 + 1) * 128], in_=yb[:, dc * 128:(dc + 1) * 128], identity=identb), reads=[yn, "identb"], writes=[bn])
                    for k8 in range(8):
                        dc = half * 8 + k8
                        for (p0, n, seq) in segs:
                            dst, dn = dst_fn(dc, p0, n)
                            R.add("dve", lambda e, pb=pb, k8=k8, dc=dc, p0=p0, n=n, seq=seq, dst=dst: e.tensor_scalar(out=dst, in0=pb[:, k8 * 128 + p0:k8 * 128 + p0 + n], scalar1=gmm[:, dc, seq:seq + 1], scalar2=modT[:, dc, seq:seq + 1], op0=ALU.mult, op1=ALU.add), reads=[bn, "gmm", "modT1"], writes=[dn])

            mmb = [0]

            def next_bank():
                mmb[0] ^= 1
                return mmb[0], f"b{mmb[0]}"

            phase = [0]

            LABELS = ["A", "B1", "SB", "BAND", "MERGE", "WOUT", "ROUTER", "MOE"]

            def FENCE(label=None, soft=False):
                if label is None:
                    label = LABELS[phase[0]]
                    phase[0] += 1
                    if soft and upto != label:
                        return
                elif upto != label:
                    return
                R.barrier(lambda e: e.memset(dummy, 0.0))
                if upto is not None and label == upto:
                    raise _Stop()

            A.set(HT0)
            NB = norm_bufs()
            wkv = A.a(16 * 2048, BF16).rearrange("p (k n) -> p k n", n=2048)
            for q4 in range(4):
                R.add("pool", lambda e, q4=q4: e.dma_start(out=wkv[:, :, q4 * 512:(q4 + 1) * 512], in_=w_in[:, 1024 + q4 * 512:1024 + (q4 + 1) * 512].rearrange("(k p) n -> p k n", p=128)), writes=["wkv"], dma="wkv")
            hTb = [A.a(16 * 512, BF16).rearrange("p (k n) -> p k n", n=512) for _ in range(2)]
            ktblk = [A.a(8 * 512, BF16).rearrange("p (h n) -> p h n", n=512) for _ in range(2)]
            vblk = [A.a(4 * 1024, BF16).rearrange("p (t n) -> p t n", n=1024) for _ in range(2)]
            assert A.off <= 173 * 1024, A.off
            def normA(blk, t4):
                i = blk % 2
                hb, hn = hTb[i], f"hTb{i}"
                norm_tile(NB, blk * 4 + t4, [(0, 128, 0)], lambda dc, p0, n: (hb[:, dc, t4 * 128 + p0:t4 * 128 + p0 + n], hn))

            def projA(blk):
                i = blk % 2
                hb, hn = hTb[i], f"hTb{i}"
                items = []

                def kitem(h):
                    bi, bn = next_bank()
                    for k in range(16):
                        R.add("pe", lambda e, k=k: e.matmul(bk(bi), lhsT=wkv[:, k, h * 128:(h + 1) * 128], rhs=hb[:, k, :], start=(k == 0), stop=(k == 15)), reads=["wkv", hn], writes=[bn])
                    R.add("act", lambda e: e.copy(out=ktblk[i][:, h, :], in_=bk(bi)), reads=[bn], writes=[f"ktblk{i}"])
                    if h == 7:
                        R.add("sp", lambda e: e.dma_start(out=KTs[:, :, blk * 512:(blk + 1) * 512].rearrange("h p n -> p h n"), in_=ktblk[i]), reads=[f"ktblk{i}"], writes=["KTs"], dma=f"ktw{i}")

                def vitem(t4, cb):
                    bi, bn = next_bank()
                    for k in range(16):
                        R.add("pe", lambda e, k=k: e.matmul(bk(bi), lhsT=hb[:, k, t4 * 128:(t4 + 1) * 128], rhs=wkv[:, k, 1024 + cb * 512:1024 + (cb + 1) * 512], start=(k == 0), stop=(k == 15)), reads=["wkv", hn], writes=[bn])
                    R.add("act", lambda e: e.copy(out=vblk[i][:, t4, cb * 512:(cb + 1) * 512], in_=bk(bi)), reads=[bn], writes=[f"vblk{i}"])
                    if cb == 1:
                        R.add("sp", lambda e: e.dma_start(out=Vs[:, blk * 512 + t4 * 128:blk * 512 + (t4 + 1) * 128, :].rearrange("h p d -> p h d"), in_=vblk[i][:, t4, :].rearrange("p (h d) -> p h d", d=128)), reads=[f"vblk{i}"], writes=["Vs"], dma=f"vw{i}")

                for h in range(8):
                    items.append(lambda h=h: kitem(h))
                for t4 in range(4):
                    for cb in range(2):
                        items.append(lambda t4=t4, cb=cb: vitem(t4, cb))
                return items

            for t4 in range(4):
                normA(0, t4)
            for blk in range(6):
                items = projA(blk)
                for t4 in range(4):
                    if blk + 1 < 6:
                        normA(blk + 1, t4)
                    for it in items[t4 * 4:(t4 + 1) * 4]:
                        it()
                ada_more(3)
            ada_more(24)
            FENCE()

            A.set(HT0)
            hT = A.a(16 * HTC, BF16).rearrange("p (k n) -> p k n", n=HTC)
            assert A.off <= OAT0
            A.set(OAT0)
            oAT = A.a(8 * NOWN, BF16).rearrange("p (h n) -> p h n", n=NOWN)
            oBT = A.a(8 * NOWN, BF16).rearrange("p (h n) -> p h n", n=NOWN)
            assert A.off <= PH0
            A.set(PH0)
            qT = A.a(8 * NOWN, BF16).rearrange("p (h n) -> p h n", n=NOWN)
            kTS = A.a(8 * 256, BF16).rearrange("p (h n) -> p h n", n=256)
            vS = A.a(4 * 1024, BF16).rearrange("p (t n) -> p t n", n=1024)
            mB1 = A.mark()
            NB = norm_bufs()
            for t4 in range(4):
                norm_tile(NB, 20 + t4, [(0, 128, 0)], lambda dc, p0, n, t4=t4: (hT[:, dc, NOWN + t4 * 128 + p0:NOWN + t4 * 128 + p0 + n], "hT"))
            for t in range(8):
                norm_tile(NB, 24 + t, [(0, 128, 0)], lambda dc, p0, n, t=t: (hT[:, dc, t * 128 + p0:t * 128 + p0 + n], "hT"))
            for t in range(2):
                norm_tile(NB, 32 + t, [(0, 64, 1 + 2 * t), (64, 64, 2 + 2 * t)], lambda dc, p0, n, t=t: (hT[:, dc, 1024 + t * 128 + p0:1024 + t * 128 + p0 + n], "hT"))

            FENCE("B1n")
            wcs = [A.a(16 * 512, BF16).rearrange("p (k n) -> p k n", n=512) for _ in range(2)]
            ostg = [A.a(512, F32) for _ in range(2)]
            osi = [0]
            ktown = [A.a(512, BF16) for _ in range(2)]
            kti = [0]
            vstg = [A.a(512, BF16) for _ in range(2)]
            vsi = [0]
            TMT = [(t * 128, 128) for t in range(8)] + [(1024 + s * 64, 64) for s in range(4)]
            wci = [0]

            def load_wc(cb, nbuf=2):
                i = wci[0] % nbuf
                wci[0] += 1
                buf = wcs[i]
                R.add("pool", lambda e: e.dma_start(out=buf, in_=w_in[:, cb * 512:(cb + 1) * 512].rearrange("(k p) n -> p k n", p=128)), writes=[f"wc{i}"], dma=f"wc{i}")
                return buf, f"wc{i}"

            def fm_proj(wc, wn, hh, c0, n):
                bi, bn = next_bank()
                for k in range(16):
                    R.add("pe", lambda e, k=k: e.matmul(banks[bi][:, 0:n], lhsT=wc[:, k, hh * 128:(hh + 1) * 128], rhs=hT[:, k, c0:c0 + n], start=(k == 0), stop=(k == 15)), reads=[wn, "hT"], writes=[bn])
                return bi, bn

            def tm_proj(wc, wn, c0, n):
                bi, bn = next_bank()
                for k in range(16):
                    R.add("pe", lambda e, k=k: e.matmul(banks[bi][0:n, :], lhsT=hT[:, k, c0:c0 + n], rhs=wc[:, k, :], start=(k == 0), stop=(k == 15)), reads=[wn, "hT"], writes=[bn])
                return bi, bn

            def tm_out(bi, bn, n, dst_ap):
                i = osi[0] % 2
                osi[0] += 1
                ob_ = ostg[i]
                R.add("act", lambda e: e.copy(out=ob_[0:n, :], in_=banks[bi][0:n, :]), reads=[bn], writes=[f"ostg{i}"])
                R.add("sp", lambda e: e.dma_start(out=dst_ap, in_=ob_[0:n, :]), reads=[f"ostg{i}"], dma=f"ostg{i}")

            for cb in range(6):
                ty = cb // 2
                half = cb % 2
                wc, wn = load_wc(cb)
                if ty in (0, 1):
                    for hh in range(4):
                        h = half * 4 + hh
                        for (c0, n) in TBS:
                            bi, bn = fm_proj(wc, wn, hh, c0, n)
                            if ty == 0:
                                R.add("act", lambda e, bi=bi, n=n, h=h, c0=c0: e.activation(out=qT[:, h, c0:c0 + n], in_=banks[bi][:, 0:n], func=AF.Copy, scale=SC), reads=[bn], writes=["qT"])
                            elif c0 < 1024:
                                i = kti[0] % 2
                                kti[0] += 1
                                R.add("act", lambda e, bi=bi, i=i: e.copy(out=ktown[i], in_=bk(bi)), reads=[bn], writes=[f"ktown{i}"])
                                R.add("sp", lambda e, i=i, h=h, c0=c0: e.dma_start(out=KTs[h, :, 3072 + c0:3072 + c0 + 512], in_=ktown[i]), reads=[f"ktown{i}"], writes=["KTs"], dma=f"ktown{i}")
                            else:
                                R.add("act", lambda e, bi=bi, h=h: e.copy(out=kTS[:, h, :], in_=banks[bi][:, 0:256]), reads=[bn], writes=["kTS"])
                if ty in (1, 2):
                    for (c0, n) in TMT:
                        bi, bn = tm_proj(wc, wn, c0, n)
                        dst = ksb_out if ty == 1 else vsb_out
                        tm_out(bi, bn, n, dst[c0:c0 + n, half * 512:(half + 1) * 512])
                        if ty == 2:
                            if c0 < 1024:
                                i = vsi[0] % 2
                                vsi[0] += 1
                                R.add("dve", lambda e, bi=bi, i=i: e.tensor_copy(out=vstg[i], in_=bk(bi)), reads=[bn], writes=[f"vstg{i}"])
                                R.add("sp", lambda e, i=i, c0=c0, half=half: e.dma_start(out=Vs[half * 4:(half + 1) * 4, 3072 + c0:3072 + c0 + 128, :].rearrange("h p d -> p h d"), in_=vstg[i].rearrange("p (h d) -> p h d", d=128)), reads=[f"vstg{i}"], writes=["Vs"], dma=f"vstg{i}")
                            else:
                                s = (c0 - 1024) // 64
                                R.add("dve", lambda e, bi=bi, s=s, half=half: e.tensor_copy(out=vS[0:64, s, half * 512:(half + 1) * 512], in_=banks[bi][0:64, :]), reads=[bn], writes=["vS"])
                FENCE("B1c%d" % cb)
            DUMP(R, "hT", hT.rearrange("p k n -> p (k n)"), 16 * HTC, BF16, "hT")
            DUMP(R, "qT", qT.rearrange("p h n -> p (h n)"), 8 * NOWN, BF16, "qT")
            DUMP(R, "kTS", kTS.rearrange("p h n -> p (h n)"), 8 * 256, BF16, "kTS")
            FENCE()
            A.release(mB1)

            ktH = [A.a(4096, BF16) for _ in range(2)]
            vHP = A.a(32 * 256, BF16).rearrange("p (t d) -> p t d", d=256)
            krawP = A.a(32 * 256, BF16).rearrange("p (t d) -> p t d", d=256)
            vH = [vHP[:, :, sid * 128:(sid + 1) * 128] for sid in range(2)]
            kraw = [krawP[:, :, sid * 128:(sid + 1) * 128] for sid in range(2)]
            e_sb = [A.a(512, F32) for _ in range(2)]
            sp_sb = [A.a(512, BF16) for _ in range(2)]
            a_sb = [A.a(512, BF16) for _ in range(2)]
            lsum = [A.a(128, BF16) for _ in range(2)]
            oslot = [0, 0]

            def sb_group(sid, q_ap, nq, tiles, first, last, kb_ap, diag, oslot_i, out_ap):
                zb, zn = banks[2 + sid], f"b{2 + sid}"
                tb, tn = banks[4 + sid], f"b{4 + sid}"
                ob = banks[6 + sid][:, oslot_i * 128:oslot_i * 128 + nq]
                on = f"b{6 + sid}"
                G = len(tiles)
                W = G * nq
                nk0 = tiles[0][4]
                en, spn, an, ln = f"e{sid}", f"sp{sid}", f"a{sid}", f"ls{sid}"

                def s0():
                    for i, (kT, kn, v, vn, nk) in enumerate(tiles):
                        R.add("pe", lambda e, i=i, kT=kT, nk=nk: e.matmul(zb[0:nk, i * nq:(i + 1) * nq], lhsT=kT, rhs=q_ap, start=True, stop=True), reads=[kn, "qT"], writes=[zn])

                def s1():
                    R.add("act", lambda e: e.activation(out=e_sb[sid][0:nk0, 0:W], in_=zb[0:nk0, 0:W], func=AF.Exp, bias=kb_ap[0:nk0, :]), reads=[zn, "kbias", "zeroc"], writes=[en])

                def s2():
                    R.add("act", lambda e: e.activation(out=sp_sb[sid][0:nk0, 0:W], in_=e_sb[sid][0:nk0, 0:W], func=AF.Ln, bias=onec[0:nk0, :]), reads=[en, "onec"], writes=[spn])
                    if diag:
                        R.add("dve", lambda e: e.tensor_tensor(out=sp_sb[sid][0:nk0, 0:nq], in0=sp_sb[sid][0:nk0, 0:nq], in1=dmask[0:nk0, 0:nq], op=ALU.mult), reads=[spn, "dmask"], writes=[spn])

                def s3():
                    for i, (kT, kn, v, vn, nk) in enumerate(tiles):
                        sl = slice(i * nq, (i + 1) * nq)
                        R.add("pe", lambda e, sl=sl, nk=nk: e.matmul(tb[0:nk, sl], lhsT=negtri[0:nk, 0:nk], rhs=sp_sb[sid][0:nk, sl], start=True, stop=False), reads=[spn, "negtri"], writes=[tn])
                        if not first:
                            R.add("pe", lambda e, sl=sl, nk=nk: e.matmul(tb[0:nk, sl], lhsT=negones[:, 0:nk], rhs=lsum[sid][:, 0:nq], start=False, stop=False), reads=[ln, "negones"], writes=[tn])
                        for i2 in range(i):
                            nk2 = tiles[i2][4]
                            R.add("pe", lambda e, sl=sl, nk=nk, i2=i2, nk2=nk2: e.matmul(tb[0:nk, sl], lhsT=negones[0:nk2, 0:nk], rhs=sp_sb[sid][0:nk2, i2 * nq:(i2 + 1) * nq], start=False, stop=False), reads=[spn, "negones"], writes=[tn])
                        R.add("pe", lambda e, sl=sl, nk=nk, kT=kT: e.matmul(tb[0:nk, sl], lhsT=kT, rhs=q_ap, start=False, stop=True), reads=[kn, "qT"], writes=[tn])
                    for i, (kT, kn, v, vn, nk) in enumerate(tiles):
                        if first and i == 0:
                            R.add("dve", lambda e: e.memset(lsum[sid], 0.0), writes=[ln])
                        R.add("dve", lambda e, i=i, nk=nk: e.tensor_tensor(out=lsum[sid][0:nk, 0:nq], in0=lsum[sid][0:nk, 0:nq], in1=sp_sb[sid][0:nk, i * nq:(i + 1) * nq], op=ALU.add), reads=[ln, spn], writes=[ln])

                def s4():
                    R.add("act", lambda e: e.activation(out=a_sb[sid][0:nk0, 0:W], in_=tb[0:nk0, 0:W], func=AF.Exp, bias=kb_ap[0:nk0, :]), reads=[tn, "kbias", "zeroc"], writes=[an])
                    if diag:
                        R.add("dve", lambda e: e.tensor_tensor(out=a_sb[sid][0:nk0, 0:nq], in0=a_sb[sid][0:nk0, 0:nq], in1=dmask[0:nk0, 0:nq], op=ALU.mult), reads=[an, "dmask"], writes=[an])

                def s5():
                    for i, (kT, kn, v, vn, nk) in enumerate(tiles):
                        R.add("pe", lambda e, i=i, v=v, nk=nk: e.matmul(ob, lhsT=v, rhs=a_sb[sid][0:nk, i * nq:(i + 1) * nq], start=(first and i == 0), stop=(last and i == G - 1)), reads=[vn, an], writes=[on])
                    if last:
                        R.add("dve", lambda e: e.tensor_copy(out=out_ap, in_=ob), reads=[on], writes=["oAT"])

                return [s0, s1, s2, s3, s4, s5]

            def interleave(streams):
                rows = list(zip_longest(*streams))

                def call(r, k):
                    if r is None:
                        return
                    for g in r:
                        if g is not None:
                            g[k]()

                if not rows:
                    return
                for k in range(3):
                    call(rows[0], k)
                for ri in range(len(rows)):
                    nxt = rows[ri + 1] if ri + 1 < len(rows) else None
                    call(rows[ri], 3)
                    call(nxt, 0)
                    call(rows[ri], 4)
                    call(nxt, 1)
                    call(nxt, 2)
                    call(rows[ri], 5)

            def chunks(lst, n):
                return [lst[i:i + n] for i in range(0, len(lst), n)]

            for hp in range(4):
                streams = []
                for sid in range(2):
                    h = hp * 2 + sid
                    R.add("sp", lambda e, sid=sid, h=h: e.dma_start(out=ktH[sid], in_=KTs[h]), reads=["KTs"], writes=[f"ktH{sid}"], dma=f"ktH{sid}")
                    R.add("sp", lambda e, sid=sid, h=h: e.dma_start(out=vH[sid], in_=Vs[h].rearrange("(t p) d -> p t d", p=128)), reads=["Vs"], writes=[f"vH{sid}"], dma=f"vH{sid}")
                    gl = []
                    for t in range(8):
                        q_ap = qT[:, h, t * 128:(t + 1) * 128]
                        tl = lambda j, sid=sid: (ktH[sid][:, j * 128:(j + 1) * 128], f"ktH{sid}", vH[sid][:, j, :], f"vH{sid}", 128)
                        groups = [([tl(24 + t)], zeroc, True)]
                        for ch in chunks(list(range(24 + t - 1, 23, -1)), 4):
                            groups.append(([tl(j) for j in ch], zeroc, False))
                        for ch in chunks(list(range(23, -1, -1)), 4):
                            groups.append(([tl(j) for j in ch], kbias[:, ch[0]:ch[0] + 1], False))
                        osl = oslot[sid] % 4
                        oslot[sid] += 1
                        for gi, (tiles, kb_ap, dg) in enumerate(groups):
                            gl.append(sb_group(sid, q_ap, 128, tiles, gi == 0, gi == len(groups) - 1, kb_ap, dg, osl, oAT[:, h, t * 128:(t + 1) * 128]))
                    streams.append(gl)
                interleave(streams)

            for s in range(4):
                for hp in range(4):
                    streams = []
                    R.add("pool", lambda e, hp=hp, s=s: e.dma_start(out=krawP, in_=csk[s, :, hp * 256:(hp + 1) * 256].rearrange("(t p) d -> p t d", p=128)), writes=["kraw0", "kraw1"], dma="kraw0")
                    R.add("pool", lambda e, hp=hp, s=s: e.dma_start(out=vHP, in_=csv[s, :, hp * 256:(hp + 1) * 256].rearrange("(t p) d -> p t d", p=128)), writes=["vH0", "vH1"], dma="vH0")
                    for sid in range(2):
                        h = hp * 2 + sid
                        for t8 in range(4):
                            pb = banks[sid][:, 0:512].bitcast(BF16)
                            for j in range(8):
                                R.add("pe", lambda e, pb=pb, j=j, t8=t8, sid=sid: e.transpose(out=pb[:, j * 128:(j + 1) * 128], in_=kraw[sid][:, t8 * 8 + j, :], identity=identb), reads=[f"kraw{sid}", "identb"], writes=[f"b{sid}"])
                            R.add("dve", lambda e, pb=pb, t8=t8, sid=sid: e.tensor_copy(out=ktH[sid][:, t8 * 1024:(t8 + 1) * 1024], in_=pb), reads=[f"b{sid}"], writes=[f"ktH{sid}"])
                        q_ap = qT[:, h, 1024 + s * 64:1024 + (s + 1) * 64]
                        tl = lambda j, sid=sid: (ktH[sid][:, j * 128:(j + 1) * 128], f"ktH{sid}", vH[sid][:, j, :], f"vH{sid}", 128)
                        groups = [([(kTS[:, h, s * 64:(s + 1) * 64], "kTS", vS[0:64, s, h * 128:(h + 1) * 128], "vS", 64)], True)]
                        for ch in chunks(list(range(31, -1, -1)), 4):
                            groups.append(([tl(j) for j in ch], False))
                        osl = oslot[sid] % 4
                        oslot[sid] += 1
                        gl = []
                        for gi, (tiles, dg) in enumerate(groups):
                            gl.append(sb_group(sid, q_ap, 64, tiles, gi == 0, gi == len(groups) - 1, zeroc, dg, osl, oAT[:, h, 1024 + s * 64:1024 + (s + 1) * 64]))
                        streams.append(gl)
                    interleave(streams)
            DUMP(R, "oAT", oAT.rearrange("p h n -> p (h n)"), 8 * NOWN, BF16, "oAT")
            FENCE()

            A.set(PH0)
            bt = A.a(32 * 128, F32).rearrange("p (t q) -> p t q", q=128)
            for h in range(8):
                for j in range(3):
                    R.add("sp", lambda e, h=h, j=j: e.dma_start(out=bt[:, h * 4 + j, :], in_=btile[:, h * 3 + j, :]), writes=["bt"], dma="bt")
            for h in range(8):
                R.add("dve", lambda e, h=h: e.tensor_copy(out=bt[:, h * 4 + 3, :], in_=bt[:, h * 4 + 2, :]), reads=["bt"], writes=["bt"])
                R.add("pool", lambda e, h=h: e.memset(bt[0:64, h * 4 + 3, 64:128], NEG), reads=["bt"], writes=["bt"])
                R.add("pool", lambda e, h=h: e.memset(bt[64:128, h * 4 + 0, 0:64], NEG), reads=["bt"], writes=["bt"])
            qbT = A.a(4 * NOWN, BF16).rearrange("p (h n) -> p h n", n=NOWN)
            kbT = A.a(4 * HTC, BF16).rearrange("p (h n) -> p h n", n=HTC)
            vbP = A.a(12 * 512, BF16).rearrange("p (t n) -> p t n", n=512)
            vbS = A.a(4 * 512, BF16).rearrange("p (t n) -> p t n", n=512)
            wcs = [A.a(16 * 512, BF16).rearrange("p (k n) -> p k n", n=512)]
            ostg = [A.a(512, F32) for _ in range(2)]
            sqb = A.a(512, BF16)
            rs1 = A.a(512, F32)
            rs2 = A.a(512, F32)
            kn32 = A.a(512, F32)
            sB = [A.a(640, F32) for _ in range(2)]
            pB = [A.a(640, BF16) for _ in range(2)]
            rden = [A.a(128, F32) for _ in range(2)]
            kbc = [A.a(4 * 128, BF16).rearrange("p (t d) -> p t d", d=128) for _ in range(2)]
            vbc = [A.a(4 * 128, BF16).rearrange("p (t d) -> p t d", d=128) for _ in range(2)]
            kbcT = [A.a(512, BF16) for _ in range(2)]

            def band_norm(bi, bn, n, gain_ap, dst_bf, dn, want32):
                R.add("act", lambda e: e.activation(out=sqb[:, 0:n], in_=banks[bi][:, 0:n], func=AF.Square), reads=[bn], writes=["sqb"])
                R.add("pe", lambda e: e.matmul(banks[5][:, 0:n], lhsT=onesb, rhs=sqb[:, 0:n], start=True, stop=True), reads=["sqb", "onesb"], writes=["b5"])
                R.add("act", lambda e: e.activation(out=rs1[:, 0:n], in_=banks[5][:, 0:n], func=AF.Ln, scale=1.0 / 128, bias=epsc), reads=["b5", "epsc"], writes=["rs1"])
                R.add("act", lambda e: e.activation(out=rs2[:, 0:n], in_=rs1[:, 0:n], func=AF.Exp, scale=-0.5), reads=["rs1"], writes=["rs2"])
                if not want32:
                    R.add("dve", lambda e: e.scalar_tensor_tensor(out=dst_bf, in0=banks[bi][:, 0:n], scalar=gain_ap, in1=rs2[:, 0:n], op0=ALU.mult, op1=ALU.mult), reads=[bn, "rs2", "cparam"], writes=[dn])
                else:
                    R.add("dve", lambda e: e.scalar_tensor_tensor(out=kn32[:, 0:n], in0=banks[bi][:, 0:n], scalar=gain_ap, in1=rs2[:, 0:n], op0=ALU.mult, op1=ALU.mult), reads=[bn, "rs2", "cparam"], writes=["kn32"])
                    R.add("act", lambda e: e.copy(out=dst_bf, in_=kn32[:, 0:n]), reads=["kn32"], writes=[dn])

            def band_unit(sid, q_ap, nq, tiles, out_ap):
                z0, zn0 = banks[2 + sid], f"b{2 + sid}"
                dnb, dnn = banks[4 + sid], f"b{4 + sid}"
                ob, on = banks[6 + sid], f"b{6 + sid}"
                G = len(tiles)
                sn, pn, rn = f"sB{sid}", f"pB{sid}", f"rden{sid}"
                zb2, zn2 = banks[sid], f"b{sid}"

                def zslice(i, nk):
                    if i < 4:
                        return z0[0:nk, i * nq:(i + 1) * nq], zn0
                    return zb2[0:nk, 0:nq], zn2

                def s0():
                    for i, (kT, kn, v, vn, nk, b_ap, kb_ap) in enumerate(tiles):
                        zs, zn = zslice(i, nk)
                        R.add("pe", lambda e, zs=zs, kT=kT: e.matmul(zs, lhsT=kT, rhs=q_ap, start=True, stop=True), reads=[kn, "qbT"], writes=[zn])

                def s1():
                    for i, (kT, kn, v, vn, nk, b_ap, kb_ap) in enumerate(tiles):
                        zs, zn = zslice(i, nk)
                        R.add("dve", lambda e, zs=zs, i=i, nk=nk, b_ap=b_ap: e.tensor_tensor(out=sB[sid][0:nk, i * nq:(i + 1) * nq], in0=zs, in1=b_ap, op=ALU.add), reads=[zn, "bt"], writes=[sn])

                def s2():
                    for i, (kT, kn, v, vn, nk, b_ap, kb_ap) in enumerate(tiles):
                        R.add("act", lambda e, i=i, nk=nk, kb_ap=kb_ap: e.activation(out=pB[sid][0:nk, i * nq:(i + 1) * nq], in_=sB[sid][0:nk, i * nq:(i + 1) * nq], func=AF.Exp, bias=kb_ap), reads=[sn, "kbias", "zeroc"], writes=[pn])

                def s3():
                    for i, (kT, kn, v, vn, nk, b_ap, kb_ap) in enumerate(tiles):
                        R.add("pe", lambda e, i=i, nk=nk: e.matmul(dnb[:, 0:nq], lhsT=onesb[0:nk, :], rhs=pB[sid][0:nk, i * nq:(i + 1) * nq], start=(i == 0), stop=(i == G - 1)), reads=[pn, "onesb"], writes=[dnn])
                    for i, (kT, kn, v, vn, nk, b_ap, kb_ap) in enumerate(tiles):
                        R.add("pe", lambda e, i=i, nk=nk, v=v: e.matmul(ob[:, 0:nq], lhsT=v, rhs=pB[sid][0:nk, i * nq:(i + 1) * nq], start=(i == 0), stop=(i == G - 1)), reads=[pn, vn], writes=[on])

                def s4():
                    R.add("dve", lambda e: e.reciprocal(out=rden[sid][:, 0:nq], in_=dnb[:, 0:nq]), reads=[dnn], writes=[rn])

                def s5():
                    R.add("dve", lambda e: e.tensor_tensor(out=out_ap, in0=ob[:, 0:nq], in1=rden[sid][:, 0:nq], op=ALU.mult), reads=[on, rn], writes=["oBT"])

                return [s0, s1, s2, s3, s4, s5]

            FMB = TBS + [(NOWN, 512)]
            for half in range(2):
                for ty in (3, 4, 5):
                    cb = ty * 2 + half
                    wc, wn = load_wc(cb, nbuf=1)
                    if ty in (3, 4):
                        for hh in range(4):
                            h = half * 4 + hh
                            for (c0, n) in (TBS if ty == 3 else FMB):
                                bi, bn = fm_proj(wc, wn, hh, c0, n)
                                if ty == 3:
                                    band_norm(bi, bn, n, qgs[:, h:h + 1], qbT[:, hh, c0:c0 + n], "qbT", False)
                                else:
                                    own = c0 < NOWN
                                    band_norm(bi, bn, n, kg[:, h:h + 1], kbT[:, hh, c0:c0 + n], "kbT", own)
                                    if own:
                                        for j in range(n // 128):
                                            R.add("pe", lambda e, j=j: e.transpose(out=banks[7][:, j * 128:(j + 1) * 128], in_=kn32[:, j * 128:(j + 1) * 128], identity=identf), reads=["kn32", "identf"], writes=["b7"])
                                        i = osi[0] % 2
                                        osi[0] += 1
                                        ob_ = ostg[i]
                                        R.add("act", lambda e, ob_=ob_, n=n: e.copy(out=ob_[:, 0:n], in_=banks[7][:, 0:n]), reads=["b7"], writes=[f"ostg{i}"])
                                        R.add("sp", lambda e, ob_=ob_, n=n, c0=c0, h=h: e.dma_start(out=kbd_out[c0:c0 + n, h * 128:(h + 1) * 128].rearrange("(j p) d -> p j d", p=128), in_=ob_[:, 0:n].rearrange("p (j d) -> p j d", d=128)), reads=[f"ostg{i}"], dma=f"ostg{i}")
                    else:
                        for (c0, n) in TMT + [(NOWN + t4 * 128, 128) for t4 in range(4)]:
                            bi, bn = tm_proj(wc, wn, c0, n)
                            if c0 < NOWN:
                                tm_out(bi, bn, n, vbd_out[c0:c0 + n, half * 512:(half + 1) * 512])
                            if c0 < 1024:
                                t = c0 // 128
                                R.add("dve", lambda e, bi=bi, t=t: e.tensor_copy(out=vbP[:, 4 + t, :], in_=bk(bi)), reads=[bn], writes=["vbP"])
                            elif c0 < NOWN:
                                s = (c0 - 1024) // 64
                                R.add("dve", lambda e, bi=bi, s=s: e.tensor_copy(out=vbS[0:64, s, :], in_=banks[bi][0:64, :]), reads=[bn], writes=["vbS"])
                            else:
                                t4 = (c0 - NOWN) // 128
                                R.add("dve", lambda e, bi=bi, t4=t4: e.tensor_copy(out=vbP[:, t4, :], in_=bk(bi)), reads=[bn], writes=["vbP"])

                def kcol(kt):
                    return NOWN + kt * 128 if kt < 4 else (kt - 4) * 128

                for hp in range(2):
                    streams = []
                    for sid in range(2):
                        hh = hp * 2 + sid
                        h = half * 4 + hh
                        gl = []
                        for t in range(8):
                            tiles = []
                            for j in range(5):
                                kt = 4 + t - j
                                kb_ap = kbias[:, 20 + kt:21 + kt] if kt < 4 else zeroc
                                bidx = h * 4 + (3 if j == 4 else min(j, 2))
                                tiles.append((kbT[:, hh, kcol(kt):kcol(kt) + 128], "kbT", vbP[:, kt, hh * 128:(hh + 1) * 128], "vbP", 128, bt[:, bidx, :], kb_ap))
                            gl.append(band_unit(sid, qbT[:, hh, t * 128:(t + 1) * 128], 128, tiles, oBT[:, h, t * 128:(t + 1) * 128]))
                        streams.append(gl)
                    interleave(streams)
                for s in range(4):
                    for hp in range(2):
                        streams = []
                        for sid in range(2):
                            hh = hp * 2 + sid
                            h = half * 4 + hh
                            R.add("pool", lambda e, sid=sid, h=h, s=s: e.dma_start(out=kbc[sid], in_=cbk[s, :, h * 128:(h + 1) * 128].rearrange("(t p) d -> p t d", p=128)), writes=[f"kbc{sid}"], dma=f"kbc{sid}")
                            R.add("pool", lambda e, sid=sid, h=h, s=s: e.dma_start(out=vbc[sid], in_=cbv[s, :, h * 128:(h + 1) * 128].rearrange("(t p) d -> p t d", p=128)), writes=[f"vbc{sid}"], dma=f"vbc{sid}")
                            pb = banks[sid][:, 0:256].bitcast(BF16)
                            for j in range(4):
                                R.add("pe", lambda e, pb=pb, j=j, sid=sid: e.transpose(out=pb[:, j * 128:(j + 1) * 128], in_=kbc[sid][:, j, :], identity=identb), reads=[f"kbc{sid}", "identb"], writes=[f"b{sid}"])
                            R.add("act", lambda e, pb=pb, sid=sid: e.copy(out=kbcT[sid], in_=pb), reads=[f"b{sid}"], writes=[f"kbcT{sid}"])
                            c0 = 1024 + s * 64
                            tiles = [(kbT[:, hh, c0:c0 + 64], "kbT", vbS[0:64, s, hh * 128:(hh + 1) * 128], "vbS", 64, bt[0:64, h * 4 + 0, 0:64], zeroc[0:64, :])]
                            for m in range(3, -1, -1):
                                jj = 1 if m == 3 else 2
                                tiles.append((kbcT[sid][:, m * 128:(m + 1) * 128], f"kbcT{sid}", vbc[sid][:, m, :], f"vbc{sid}", 128, bt[:, h * 4 + jj, 0:64], zeroc))
                            streams.append([band_unit(sid, qbT[:, hh, c0:c0 + 64], 64, tiles, oBT[:, h, c0:c0 + 64])])
                        interleave(streams)
            DUMP(R, "oBT", oBT.rearrange("p h n -> p (h n)"), 8 * NOWN, BF16, "oBT")
            FENCE()

            A.set(PH0)
            mgT = A.a(16 * NOWN, BF16).rearrange("p (k n) -> p k n", n=NOWN)
            wg_ = [A.a(16 * 128, BF16).rearrange("p (k n) -> p k n", n=128) for _ in range(2)]
            wgb_ = [A.a(16 * 128, BF16).rearrange("p (k n) -> p k n", n=128) for _ in range(2)]
            wpa_ = [A.a(8 * 128, BF16).rearrange("p (k n) -> p k n", n=128) for _ in range(2)]
            wpb_ = [A.a(8 * 128, BF16).rearrange("p (k n) -> p k n", n=128) for _ in range(2)]
            sga = A.a(512, F32)
            sgb = A.a(512, F32)
            m1 = A.a(512, F32)
            m2 = A.a(512, F32)
            for fc in range(16):
                i = fc % 2
                a0, a1 = fc * 128, (fc + 1) * 128
                R.add("pool", lambda e, i=i, a0=a0, a1=a1: e.dma_start(out=wg_[i], in_=w_in[:, 6144 + a0:6144 + a1].rearrange("(k p) n -> p k n", p=128)), writes=[f"wg{i}"], dma=f"wg{i}")
                R.add("pool", lambda e, i=i, a0=a0, a1=a1: e.dma_start(out=wgb_[i], in_=w_in[:, 8192 + a0:8192 + a1].rearrange("(k p) n -> p k n", p=128)), writes=[f"wgb{i}"], dma=f"wgb{i}")
                R.add("pool", lambda e, i=i, a0=a0, a1=a1: e.dma_start(out=wpa_[i], in_=w_psb[:, a0:a1].rearrange("(k p) n -> p k n", p=128)), writes=[f"wpa{i}"], dma=f"wpa{i}")
                R.add("pool", lambda e, i=i, a0=a0, a1=a1: e.dma_start(out=wpb_[i], in_=w_pbd[:, a0:a1].rearrange("(k p) n -> p k n", p=128)), writes=[f"wpb{i}"], dma=f"wpb{i}")
                for (c0, n) in TBS:
                    for k in range(16):
                        R.add("pe", lambda e, i=i, k=k, c0=c0, n=n: e.matmul(banks[0][:, 0:n], lhsT=wg_[i][:, k, :], rhs=hT[:, k, c0:c0 + n], start=(k == 0), stop=(k == 15)), reads=[f"wg{i}", "hT"], writes=["b0"])
                    for k in range(16):
                        R.add("pe", lambda e, i=i, k=k, c0=c0, n=n: e.matmul(banks[1][:, 0:n], lhsT=wgb_[i][:, k, :], rhs=hT[:, k, c0:c0 + n], start=(k == 0), stop=(k == 15)), reads=[f"wgb{i}", "hT"], writes=["b1"])
                    for k in range(8):
                        R.add("pe", lambda e, i=i, k=k, c0=c0, n=n: e.matmul(banks[2][:, 0:n], lhsT=wpa_[i][:, k, :], rhs=oAT[:, k, c0:c0 + n], start=(k == 0), stop=(k == 7)), reads=[f"wpa{i}", "oAT"], writes=["b2"])
                    for k in range(8):
                        R.add("pe", lambda e, i=i, k=k, c0=c0, n=n: e.matmul(banks[3][:, 0:n], lhsT=wpb_[i][:, k, :], rhs=oBT[:, k, c0:c0 + n], start=(k == 0), stop=(k == 7)), reads=[f"wpb{i}", "oBT"], writes=["b3"])
                    R.add("act", lambda e, n=n: e.activation(out=sga[:, 0:n], in_=banks[0][:, 0:n], func=AF.Sigmoid), reads=["b0"], writes=["sga"])
                    R.add("act", lambda e, n=n: e.activation(out=sgb[:, 0:n], in_=banks[1][:, 0:n], func=AF.Sigmoid), reads=["b1"], writes=["sgb"])
                    R.add("dve", lambda e, n=n: e.tensor_tensor(out=m1[:, 0:n], in0=banks[2][:, 0:n], in1=sga[:, 0:n], op=ALU.mult), reads=["b2", "sga"], writes=["m1"])
                    R.add("dve", lambda e, n=n: e.tensor_tensor(out=m2[:, 0:n], in0=banks[3][:, 0:n], in1=sgb[:, 0:n], op=ALU.mult), reads=["b3", "sgb"], writes=["m2"])
                    R.add("dve", lambda e, n=n, fc=fc, c0=c0: e.tensor_tensor(out=mgT[:, fc, c0:c0 + n], in0=m1[:, 0:n], in1=m2[:, 0:n], op=ALU.add), reads=["m1", "m2"], writes=["mgT"])
            DUMP(R, "mgT", mgT.rearrange("p k n -> p (k n)"), 16 * NOWN, BF16, "mgT")
            FENCE()

            A.set(HT0)
            yacc = A.a(16 * NOWN, F32).rearrange("p (k n) -> p k n", n=NOWN)
            h2T = A.a(16 * NOWN, BF16).rearrange("p (k n) -> p k n", n=NOWN)
            A.set(PH0 + 40 * KB)
            xts = [A.a(D, F32) for _ in range(2)]
            wo = [A.a(16 * 128, BF16).rearrange("p (k n) -> p k n", n=128) for _ in range(2)]
            for t in range(10):
                i = t % 2
                R.add("sp", lambda e, i=i, t=t: e.dma_start(out=xts[i], in_=x_tok[(24 + t) * 128:(25 + t) * 128, :]), writes=[f"xx{i}"], dma=f"xx{i}")
                for q4 in range(4):
                    bi, bn = next_bank()
                    for j in range(4):
                        dc = q4 * 4 + j
                        R.add("pe", lambda e, bi=bi, j=j, dc=dc, i=i: e.transpose(out=banks[bi][:, j * 128:(j + 1) * 128], in_=xts[i][:, dc * 128:(dc + 1) * 128], identity=identf), reads=[f"xx{i}", "identf"], writes=[bn])
                    R.add("act", lambda e, bi=bi, q4=q4, t=t: e.copy(out=yacc[:, q4 * 4:(q4 + 1) * 4, t * 128:(t + 1) * 128], in_=banks[bi][:, :].rearrange("p (j n) -> p j n", n=128)), reads=[bn], writes=["yacc"])
            SEG = [(0, 512, 0), (512, 512, 0)] + [(1024 + s * 64, 64, 1 + s) for s in range(4)]
            for dc in range(16):
                i = dc % 2
                R.add("pool", lambda e, i=i, dc=dc: e.dma_start(out=wo[i], in_=w_out[:, dc * 128:(dc + 1) * 128].rearrange("(k p) n -> p k n", p=128)), writes=[f"wo{i}"], dma=f"wo{i}")
                for (c0, n) in TBS:
                    bi, bn = next_bank()
                    for k in range(16):
                        R.add("pe", lambda e, bi=bi, i=i, k=k, c0=c0, n=n: e.matmul(banks[bi][:, 0:n], lhsT=wo[i][:, k, :], rhs=mgT[:, k, c0:c0 + n], start=(k == 0), stop=(k == 15)), reads=[f"wo{i}", "mgT"], writes=[bn])
                    for (s0_, sn_, seq) in SEG:
                        if s0_ < c0 or s0_ >= c0 + n:
                            continue
                        R.add("dve", lambda e, bi=bi, dc=dc, s0_=s0_, sn_=sn_, seq=seq, c0=c0: e.scalar_tensor_tensor(out=yacc[:, dc, s0_:s0_ + sn_], in0=banks[bi][:, s0_ - c0:s0_ - c0 + sn_], scalar=modT[:, 32 + dc, seq:seq + 1], in1=yacc[:, dc, s0_:s0_ + sn_], op0=ALU.mult, op1=ALU.add), reads=[bn, "modT2", "yacc"], writes=["yacc"])
            DUMP(R, "yacc", yacc.rearrange("p k n -> p (k n)"), 16 * NOWN, F32, "yacc")
            FENCE()

            A.set(128 * KB)
            combT = A.a(NOWN, BF16)
            selb = A.a(32 * 128, BF16).rearrange("p (e m) -> p e m", m=128)
            cbc = [A.a(NOWN, BF16) for _ in range(2)]
            mMoE = A.mark()
            sq2 = A.a(512, BF16)
            r1 = A.a(512, F32)
            r2 = A.a(512, F32)
            t32 = A.a(512, F32)
            wr = A.a(16 * 36, BF16).rearrange("p (k n) -> p k n", n=36)
            brt = A.a(36, F32)
            lg = A.a(36, F32)
            wk = A.a(64, F32)
            comb = A.a(32, F32)
            R.add("pool", lambda e: e.dma_start(out=wr, in_=w_rt.rearrange("(k p) n -> p k n", p=128)), writes=["wr"], dma="wr")
            R.add("sp", lambda e: e.dma_start(out=brt, in_=b_rt.partition_broadcast(128)), writes=["brt"], dma="brt")
            R.add("pool", lambda e: e.memset(selb[0:32], 1.0), writes=["selb"])
            R.add("pool", lambda e: e.affine_select(out=selb[0:32], in_=selb[0:32], pattern=[[-1, 32], [0, 128]], compare_op=ALU.is_equal, fill=0.0, base=0, channel_multiplier=1), reads=["selb"], writes=["selb"])
            for (c0, n) in TBS:
                for dc in range(16):
                    R.add("act", lambda e, dc=dc, c0=c0, n=n: e.activation(out=sq2[:, 0:n], in_=yacc[:, dc, c0:c0 + n], func=AF.Square), reads=["yacc"], writes=["sq2"])
                    R.add("pe", lambda e, dc=dc, n=n: e.matmul(banks[2][:, 0:n], lhsT=onesb, rhs=sq2[:, 0:n], start=(dc == 0), stop=(dc == 15)), reads=["sq2", "onesb"], writes=["b2"])
                R.add("act", lambda e, n=n: e.activation(out=r1[:, 0:n], in_=banks[2][:, 0:n], func=AF.Ln, scale=1.0 / D, bias=epsc), reads=["b2", "epsc"], writes=["r1"])
                R.add("act", lambda e, n=n: e.activation(out=r2[:, 0:n], in_=r1[:, 0:n], func=AF.Exp, scale=-0.5), reads=["r1"], writes=["r2"])
                for dc in range(16):
                    R.add("dve", lambda e, dc=dc, c0=c0, n=n: e.tensor_tensor(out=t32[:, 0:n], in0=yacc[:, dc, c0:c0 + n], in1=r2[:, 0:n], op=ALU.mult), reads=["yacc", "r2"], writes=["t32"])
                    for (s0_, sn_, seq) in SEG:
                        if s0_ < c0 or s0_ >= c0 + n:
                            continue
                        R.add("dve", lambda e, dc=dc, s0_=s0_, sn_=sn_, seq=seq, c0=c0: e.tensor_scalar(out=h2T[:, dc, s0_:s0_ + sn_], in0=t32[:, s0_ - c0:s0_ - c0 + sn_], scalar1=gmf[:, dc, seq:seq + 1], scalar2=modT[:, 48 + dc, seq:seq + 1], op0=ALU.mult, op1=ALU.add), reads=["t32", "gmf", "modT2"], writes=["h2T"])
            AXX = mybir.AxisListType.X
            for t in range(10):
                for k in range(16):
                    R.add("pe", lambda e, k=k, t=t: e.matmul(banks[3][:, 0:36], lhsT=h2T[:, k, t * 128:(t + 1) * 128], rhs=wr[:, k, :], start=(k == 0), stop=(k == 15)), reads=["h2T", "wr"], writes=["b3"])
                V = lambda a, b: wk[:, a:b]
                ops = []
                ops.append(lambda e: e.tensor_tensor(out=lg, in0=banks[3][:, 0:36], in1=brt, op=ALU.add))
                ops.append(lambda e: e.reduce_max(out=V(0, 1), in_=lg[:, 0:4], axis=AXX))
                ops.append(lambda e: e.tensor_scalar(out=V(4, 8), in0=lg[:, 0:4], scalar1=V(0, 1), scalar2=None, op0=ALU.is_ge))
                ops.append(lambda e: e.tensor_scalar(out=V(8, 12), in0=lg[:, 0:4], scalar1=V(0, 1), scalar2=None, op0=ALU.subtract))
                for o in ops:
                    R.add("dve", o, reads=["b3", "brt", "lg", "wk"], writes=["lg", "wk"])
                R.add("dve", lambda e: e.memset(V(1, 2), 0.0), reads=["wk"], writes=["wk"])
                R.add("act", lambda e: e.activation(out=V(8, 12), in_=V(8, 12), func=AF.Exp, accum_out=V(1, 2)), reads=["wk"], writes=["wk"])
                ops = []
                ops.append(lambda e: e.reciprocal(out=V(2, 3), in_=V(1, 2)))
                ops.append(lambda e: e.tensor_scalar(out=V(16, 24), in0=lg[:, 4:12], scalar1=V(4, 5), scalar2=None, op0=ALU.mult))
                for g in range(1, 4):
                    ops.append(lambda e, g=g: e.scalar_tensor_tensor(out=V(16, 24), in0=lg[:, 4 + 8 * g:12 + 8 * g], scalar=V(4 + g, 5 + g), in1=V(16, 24), op0=ALU.mult, op1=ALU.add))
                ops.append(lambda e: e.reduce_max(out=V(3, 4), in_=V(16, 24), axis=AXX))
                ops.append(lambda e: e.tensor_scalar(out=V(24, 32), in0=V(16, 24), scalar1=V(3, 4), scalar2=None, op0=ALU.is_ge))
                ops.append(lambda e: e.scalar_tensor_tensor(out=V(32, 40), in0=V(24, 32), scalar=-1e30, in1=V(16, 24), op0=ALU.mult, op1=ALU.add))
                ops.append(lambda e: e.reduce_max(out=V(12, 13), in_=V(32, 40), axis=AXX))
                ops.append(lambda e: e.tensor_scalar(out=V(40, 48), in0=V(32, 40), scalar1=V(12, 13), scalar2=None, op0=ALU.is_ge))
                ops.append(lambda e: e.tensor_tensor(out=V(13, 14), in0=V(12, 13), in1=V(3, 4), op=ALU.subtract))
                for o in ops:
                    R.add("dve", o, reads=["wk", "lg"], writes=["wk"])
                R.add("act", lambda e: e.activation(out=V(13, 14), in_=V(13, 14), func=AF.Exp), reads=["wk"], writes=["wk"])
                ops = []
                ops.append(lambda e: e.tensor_scalar(out=V(14, 15), in0=V(13, 14), scalar1=1.0, scalar2=None, op0=ALU.add))
                ops.append(lambda e: e.reciprocal(out=V(14, 15), in_=V(14, 15)))
                ops.append(lambda e: e.tensor_tensor(out=V(15, 16), in0=V(13, 14), in1=V(14, 15), op=ALU.mult))
                ops.append(lambda e: e.tensor_scalar(out=V(48, 56), in0=V(24, 32), scalar1=V(14, 15), scalar2=None, op0=ALU.mult))
                ops.append(lambda e: e.scalar_tensor_tensor(out=V(48, 56), in0=V(40, 48), scalar=V(15, 16), in1=V(48, 56), op0=ALU.mult, op1=ALU.add))
                ops.append(lambda e: e.tensor_scalar(out=V(48, 56), in0=V(48, 56), scalar1=V(2, 3), scalar2=None, op0=ALU.mult))
                for g in range(4):
                    ops.append(lambda e, g=g: e.tensor_scalar(out=comb[:, g * 8:(g + 1) * 8], in0=V(48, 56), scalar1=V(4 + g, 5 + g), scalar2=None, op0=ALU.mult))
                for o in ops:
                    R.add("dve", o, reads=["wk", "comb"], writes=["wk", "comb"])
                R.add("pe", lambda e: e.transpose(out=banks[3][0:32, 128:256], in_=comb, identity=identf), reads=["comb", "identf"], writes=["b3"])
                R.add("act", lambda e, t=t: e.copy(out=combT[0:32, t * 128:(t + 1) * 128], in_=banks[3][0:32, 128:256]), reads=["b3"], writes=["combT"])
            DUMP(R, "h2T", h2T.rearrange("p k n -> p (k n)"), 16 * NOWN, BF16, "h2T")
            DUMP(R, "combT", combT, NOWN, BF16, "combT")
            FENCE(soft=True)

            wgr = [A.a(16 * 128, BF16).rearrange("p (k n) -> p k n", n=128) for _ in range(3)]
            wur = [A.a(16 * 128, BF16).rearrange("p (k n) -> p k n", n=128) for _ in range(3)]
            wdr = [A.a(4 * 2048, BF16).rearrange("p (k n) -> p k n", n=2048) for _ in range(1)]
            hid = A.a(4 * NOWN, BF16).rearrange("p (k n) -> p k n", n=NOWN)
            sgt = [A.a(512, BF16) for _ in range(2)]
            ui = [0]
            for ex in range(32):
                ci = ex % 2
                di = 0
                for (c0, n) in TBS:
                    R.add("pe", lambda e, ex=ex, c0=c0, n=n: e.matmul(banks[6][:, 0:n], lhsT=selb[0:32, ex, :], rhs=combT[0:32, c0:c0 + n], start=True, stop=True), reads=["selb", "combT"], writes=["b6"])
                    R.add("act", lambda e, ci=ci, c0=c0, n=n: e.copy(out=cbc[ci][:, c0:c0 + n], in_=banks[6][:, 0:n]), reads=["b6"], writes=[f"cbc{ci}"])
                for fc in range(4):
                    u = ui[0] % 3
                    ui[0] += 1
                    R.add("pool", lambda e, ex=ex, fc=fc, u=u: e.dma_start(out=wgr[u], in_=w_gate[ex][:, fc * 128:(fc + 1) * 128].rearrange("(k p) n -> p k n", p=128)), writes=[f"wgr{u}"], dma=f"wgr{u}")
                    R.add("pool", lambda e, ex=ex, fc=fc, u=u: e.dma_start(out=wur[u], in_=w_up[ex][:, fc * 128:(fc + 1) * 128].rearrange("(k p) n -> p k n", p=128)), writes=[f"wur{u}"], dma=f"wur{u}")
                    if fc == 1:
                        R.add("pool", lambda e, ex=ex, di=di: e.dma_start(out=wdr[di], in_=w_down[ex].rearrange("(k p) n -> p k n", p=128)), writes=[f"wd{di}"], dma=f"wd{di}")
                    for bi_, (c0, n) in enumerate(TBS):
                        pg = (bi_ + fc) % 2
                        gb, ub = banks[pg * 2], banks[pg * 2 + 1]
                        gn, un = f"b{pg * 2}", f"b{pg * 2 + 1}"
                        for k in range(16):
                            R.add("pe", lambda e, k=k, u=u, c0=c0, n=n, gb=gb: e.matmul(gb[:, 0:n], lhsT=wgr[u][:, k, :], rhs=h2T[:, k, c0:c0 + n], start=(k == 0), stop=(k == 15)), reads=[f"wgr{u}", "h2T"], writes=[gn])
                        for k in range(16):
                            R.add("pe", lambda e, k=k, u=u, c0=c0, n=n, ub=ub: e.matmul(ub[:, 0:n], lhsT=wur[u][:, k, :], rhs=h2T[:, k, c0:c0 + n], start=(k == 0), stop=(k == 15)), reads=[f"wur{u}", "h2T"], writes=[un])
                        R.add("act", lambda e, pg=pg, n=n, gb=gb: e.activation(out=sgt[pg][:, 0:n], in_=gb[:, 0:n], func=AF.Silu), reads=[gn], writes=[f"sgt{pg}"])
                        R.add("dve", lambda e, pg=pg, n=n, c0=c0, ci=ci: e.tensor_tensor(out=sgt[pg][:, 0:n], in0=sgt[pg][:, 0:n], in1=cbc[ci][:, c0:c0 + n], op=ALU.mult), reads=[f"sgt{pg}", f"cbc{ci}"], writes=[f"sgt{pg}"])
                        R.add("dve", lambda e, pg=pg, n=n, c0=c0, fc=fc, ub=ub: e.tensor_tensor(out=hid[:, fc, c0:c0 + n], in0=ub[:, 0:n], in1=sgt[pg][:, 0:n], op=ALU.mult), reads=[un, f"sgt{pg}"], writes=["hid"])
                for dc in range(16):
                    for bi_, (c0, n) in enumerate(TBS):
                        yb_i = (4, 5, 7)[bi_]
                        yb, yn = banks[yb_i], f"b{yb_i}"
                        for fc in range(4):
                            R.add("pe", lambda e, fc=fc, dc=dc, c0=c0, n=n, yb=yb, di=di: e.matmul(yb[:, 0:n], lhsT=wdr[di][:, fc, dc * 128:(dc + 1) * 128], rhs=hid[:, fc, c0:c0 + n], start=(fc == 0), stop=(fc == 3)), reads=[f"wd{di}", "hid"], writes=[yn])
                        if c0 == 1024:
                            tb_, tn_ = (r1, "r1") if dc % 2 == 0 else (r2, "r2")
                            for sq_ in range(4):
                                R.add("act", lambda e, yb=yb, dc=dc, sq_=sq_, tb_=tb_: e.activation(out=tb_[:, sq_ * 64:(sq_ + 1) * 64], in_=yb[:, sq_ * 64:(sq_ + 1) * 64], func=AF.Copy, scale=modT[:, 80 + dc, 1 + sq_:2 + sq_]), reads=[yn, "modT2"], writes=[tn_])
                            R.add("dve", lambda e, dc=dc, tb_=tb_: e.tensor_tensor(out=yacc[:, dc, 1024:1280], in0=yacc[:, dc, 1024:1280], in1=tb_[:, 0:256], op=ALU.add), reads=[tn_, "yacc"], writes=["yacc"])
                            continue
                        for (s0_, sn_, seq) in SEG:
                            if s0_ < c0 or s0_ >= c0 + n:
                                continue
                            R.add("dve", lambda e, yb=yb, dc=dc, s0_=s0_, sn_=sn_, seq=seq, c0=c0: e.scalar_tensor_tensor(out=yacc[:, dc, s0_:s0_ + sn_], in0=yb[:, s0_ - c0:s0_ - c0 + sn_], scalar=modT[:, 80 + dc, seq:seq + 1], in1=yacc[:, dc, s0_:s0_ + sn_], op0=ALU.mult, op1=ALU.add), reads=[yn, "modT2", "yacc"], writes=["yacc"])
            FENCE()

            A.set(88 * KB)
            yst = [A.a(D, F32) for _ in range(2)]
            for t in range(10):
                i = t % 2
                for q4 in range(4):
                    bi, bn = next_bank()
                    for j in range(4):
                        dc = q4 * 4 + j
                        R.add("pe", lambda e, bi=bi, j=j, dc=dc, t=t: e.transpose(out=banks[bi][:, j * 128:(j + 1) * 128], in_=yacc[:, dc, t * 128:(t + 1) * 128], identity=identf), reads=["yacc", "identf"], writes=[bn])
                    if q4 % 2 == 0:
                        R.add("act", lambda e, bi=bi, q4=q4, i=i: e.copy(out=yst[i][:, q4 * 512:(q4 + 1) * 512], in_=bk(bi)), reads=[bn], writes=[f"yst{i}"])
                    else:
                        R.add("dve", lambda e, bi=bi, q4=q4, i=i: e.tensor_copy(out=yst[i][:, q4 * 512:(q4 + 1) * 512], in_=bk(bi)), reads=[bn], writes=[f"yst{i}"])
                R.add("sp", lambda e, i=i, t=t: e.dma_start(out=y_out[t * 128:(t + 1) * 128, :], in_=yst[i]), reads=[f"yst{i}"], dma=f"yst{i}")
        except _Stop:
            pass
        R.emit(st)
    _DBG['outs'] = dbg_outs
    return nc


_CACHE = {}


def kernel(**inp):
    f = lambda k: np.ascontiguousarray(np.asarray(inp[k], dtype=np.float32))
    xp, xs = f("x_prompt"), f("x_sample")
    csk_, csv_, cbk_, cbv_ = f("cache_sb_k")[0], f("cache_sb_v")[0], f("cache_band_k")[0], f("cache_band_v")[0]
    cp, cs = f("c_prompt"), f("c_sample")
    tab = f("rel_bias_band")[0]
    kl = np.arange(128)[:, None]
    ql = np.arange(128)[None, :]
    btile = np.zeros((128, 24, 128), np.float32)
    for j in range(3):
        idx = np.clip(128 * j + ql - kl, -128, 128) + 128
        for h in range(8):
            btile[:, h * 3 + j, :] = tab[h][idx]
    w_rt = np.ascontiguousarray(np.concatenate([f("w_router_group")[0], f("w_router_expert")[0]], axis=1))
    b_rt = np.ascontiguousarray(np.concatenate([f("b_router_group")[0].reshape(1, 4), f("b_router_expert")[0].reshape(1, 32)], axis=1))
    shared = dict(
        btile=btile, norm_mix=f("norm_mix")[0].reshape(16, 128), norm_ffn=f("norm_ffn")[0].reshape(16, 128),
        w_ada=f("w_ada")[0], b_ada=f("b_ada")[0].reshape(96, 128), w_in=f("w_in")[0],
        q_norm=f("q_norm_band")[0], k_norm=f("k_norm_band")[0], w_psb=f("w_proj_sb")[0], w_pbd=f("w_proj_band")[0],
        w_out=f("w_out")[0], w_rt=w_rt, b_rt=b_rt, w_gate=f("w_gate")[0], w_up=f("w_up")[0], w_down=f("w_down")[0])
    in_maps = []
    for c in range(8):
        b, qi = c // 4, c % 4
        x_tok = np.zeros((34 * 128, D), np.float32)
        npre = 1024 * qi
        if npre:
            x_tok[3072 - npre:3072] = xp[b, 0:npre]
        x_tok[3072:4096] = xp[b, npre:npre + 1024]
        x_tok[4096:4352] = xs[4 * c:4 * c + 4].reshape(256, D)
        kbias = np.zeros((128, 32), np.float32)
        kbias[:, 0:(3072 - npre) // 128] = NEG
        m = dict(shared)
        m.update(x_tok=x_tok, kbias=kbias, c5=np.ascontiguousarray(np.concatenate([cp[b:b + 1], cs[4 * c:4 * c + 4]], axis=0)),
                 csk=np.ascontiguousarray(csk_[4 * c:4 * c + 4].reshape(4, 4096, 1024)), csv=np.ascontiguousarray(csv_[4 * c:4 * c + 4].reshape(4, 4096, 1024)),
                 cbk=np.ascontiguousarray(cbk_[4 * c:4 * c + 4].reshape(4, 512, 1024)), cbv=np.ascontiguousarray(cbv_[4 * c:4 * c + 4].reshape(4, 512, 1024)))
        in_maps.append(m)
    if inp.get("_maps_only"):
        return in_maps
    if "nc" not in _CACHE:
        _CACHE["nc"] = build()
    res = run_bass_kernel_spmd(_CACHE["nc"], in_maps, core_ids=list(range(8)))
    r = res.results
    yp = np.zeros((2, 4096, D), np.float32)
    ys = np.zeros((32, 64, D), np.float32)
    pk = np.zeros((1, 2, 4096, 8, 128), np.float32)
    pv = np.zeros_like(pk)
    pbk = np.zeros((1, 2, 512, 8, 128), np.float32)
    pbv = np.zeros_like(pbk)
    sk = np.zeros((1, 32, 64, 8, 128), np.float32)
    sv = np.zeros_like(sk)
    sbk = np.zeros_like(sk)
    sbv = np.zeros_like(sk)
    for c in range(8):
        b, qi = c // 4, c % 4
        o = r[c]
        sl = slice(1024 * qi, 1024 * qi + 1024)
        yp[b, sl] = o["y_out"][0:1024]
        ys[4 * c:4 * c + 4] = o["y_out"][1024:1280].reshape(4, 64, D)
        pk[0, b, sl] = o["ksb_out"][0:1024].reshape(1024, 8, 128)
        pv[0, b, sl] = o["vsb_out"][0:1024].reshape(1024, 8, 128)
        sk[0, 4 * c:4 * c + 4] = o["ksb_out"][1024:1280].reshape(4, 64, 8, 128)
        sv[0, 4 * c:4 * c + 4] = o["vsb_out"][1024:1280].reshape(4, 64, 8, 128)
        sbk[0, 4 * c:4 * c + 4] = o["kbd_out"][1024:1280].reshape(4, 64, 8, 128)
        sbv[0, 4 * c:4 * c + 4] = o["vbd_out"][1024:1280].reshape(4, 64, 8, 128)
        if qi == 3:
            pbk[0, b] = o["kbd_out"][512:1024].reshape(512, 8, 128)
            pbv[0, b] = o["vbd_out"][512:1024].reshape(512, 8, 128)
    return (yp, ys, pk, pv, pbk, pbv, sk, sv, sbk, sbv)
```

```python
import numpy as np
from contextlib import ExitStack
from itertools import zip_longest
import concourse.bass as bass
import concourse.mybir as mybir
from concourse.bass_utils import run_bass_kernel_spmd

F32 = mybir.dt.float32
BF16 = mybir.dt.bfloat16
AF = mybir.ActivationFunctionType
ALU = mybir.AluOpType

D = 2048
NCH = 16
DIN = 10240
NPRE = 24
NOWN = 1280
SC = 128 ** -0.5
EPS = 1e-6
NEG = -30000.0
ENGS = ("pe", "act", "dve", "pool", "sp")
EPOCH = 3000
BANKS = {f"b{i}" for i in range(8)}
TBS = [(0, 512), (512, 512), (1024, 256)]


class Rec:
    def __init__(self, nc):
        self.nc = nc
        self.ops = []
        self.last_w = {}
        self.readers = {}
        self.dma_keys = []
        self.fence = None
        self.last_eng = {}
        self.last_dma = {}

    def add(self, eng, fn, reads=(), writes=(), dma=None):
        writes = list(writes) + [r for r in reads if r in BANKS]
        reads = [r for r in reads if r not in BANKS]
        oid = len(self.ops)
        deps = set()
        if self.fence is not None:
            deps.add(self.fence)
        for r in reads:
            w = self.last_w.get(r)
            if w is not None:
                deps.add(w)
        for w_ in writes:
            w = self.last_w.get(w_)
            if w is not None:
                deps.add(w)
            deps.update(self.readers.get(w_, {}).values())
        rk = ("d", dma) if dma is not None else ("e", eng)
        for r in reads:
            self.readers.setdefault(r, {})[rk] = oid
        for w_ in writes:
            self.last_w[w_] = oid
            self.readers[w_] = {}
        if dma is not None:
            if dma not in self.dma_keys:
                self.dma_keys.append(dma)
            self.last_dma[dma] = oid
        else:
            self.last_eng[eng] = oid
        self.ops.append(dict(eng=eng, fn=fn, deps=deps, dma=dma))
        return oid

    def barrier(self, fn):
        oid = len(self.ops)
        deps = set(self.last_eng.values()) | set(self.last_dma.values())
        if self.fence is not None:
            deps.add(self.fence)
        self.ops.append(dict(eng="dve", fn=fn, deps=deps, dma=None))
        self.last_eng["dve"] = oid
        self.fence = oid

    def emit(self, stack):
        nc = self.nc
        ops = self.ops
        src = set()
        for o in ops:
            for d in o["deps"]:
                od = ops[d]
                if od["dma"] is None and od["eng"] == "pe" and o["eng"] == "pe" and o["dma"] is None:
                    continue
                src.add(d)
        cnt = {e: 0 for e in ENGS}
        dcnt = {k: 0 for k in self.dma_keys}
        tick = {}
        for i, o in enumerate(ops):
            if o["dma"] is not None:
                dcnt[o["dma"]] += 16
                tick[i] = ("d", o["dma"], 0, dcnt[o["dma"]])
            elif i in src:
                c = cnt[o["eng"]]
                cnt[o["eng"]] += 1
                tick[i] = ("e", o["eng"], c // EPOCH, c % EPOCH + 1)
        sems = {}
        for e in ENGS:
            for ep in range(max(cnt[e] - 1, 0) // EPOCH + 1):
                sems[("e", e, ep)] = stack.enter_context(nc.semaphore(f"s_{e}_{ep}"))
        for k in self.dma_keys:
            assert dcnt[k] < 60000, (k, dcnt[k])
            sems[("d", k, 0)] = stack.enter_context(nc.semaphore(f"d_{k}"))
        engobj = {"pe": nc.tensor, "act": nc.scalar, "dve": nc.vector, "pool": nc.gpsimd, "sp": nc.sync}
        per = {e: [] for e in ENGS}
        for i, o in enumerate(ops):
            per[o["eng"]].append(i)
        final = dict(dcnt)

        def run_engine(e):
            eng = engobj[e]
            seen = {}
            for i in per[e]:
                o = ops[i]
                need = {}
                for d in o["deps"]:
                    if d not in tick:
                        continue
                    kind, key, ep, val = tick[d]
                    if kind == "e" and key == e and e == "pe" and o["dma"] is None:
                        continue
                    sk = (kind, key, ep)
                    if seen.get(sk, 0) >= val:
                        continue
                    if need.get(sk, 0) < val:
                        need[sk] = val
                items = list(need.items())
                for sk, val in items[:-1]:
                    eng.wait_ge(sems[sk], val)
                    seen[sk] = val
                ins = o["fn"](eng)
                if items:
                    sk, val = items[-1]
                    ins._wait_ge(sems[sk], val)
                    seen[sk] = val
                if i in tick:
                    kind, key, ep, val = tick[i]
                    if kind == "d":
                        ins.then_inc(sems[("d", key, 0)], 16)
                    else:
                        ins.then_inc(sems[("e", key, ep)], 1)
            if e == "sp":
                for k, v in final.items():
                    if v > 0:
                        eng.wait_ge(sems[("d", k, 0)], v)

        with nc.Block() as block:
            @block.tensor
            def _(t):
                run_engine("pe")

            @block.scalar
            def _(t):
                run_engine("act")

            @block.vector
            def _(t):
                run_engine("dve")

            @block.gpsimd
            def _(t):
                run_engine("pool")

            @block.sync
            def _(t):
                run_engine("sp")


class Arena:
    def __init__(self, nc, nbytes):
        self.t = nc.alloc_sbuf_tensor("arena", [128, nbytes], mybir.dt.uint8)
        self.n = nbytes
        self.off = 0

    def mark(self):
        return self.off

    def set(self, off):
        self.off = off

    def release(self, m):
        self.off = m

    def a(self, cols, dt):
        nb = cols * (4 if dt == F32 else 2)
        nb = (nb + 63) // 64 * 64
        assert self.off + nb <= self.n, ("SBUF overflow", self.off, nb)
        ap = self.t[:, self.off:self.off + cols * (4 if dt == F32 else 2)].bitcast(dt)
        self.off += nb
        return ap


_DBG = {}


class _Stop(Exception):
    pass


def build(upto=None, dbg=False):
    nc = bass.Bass("TRN2", target_bir_lowering=False)

    def din(name, shape, dt=F32):
        return nc.dram_tensor(name, list(shape), dt, kind="ExternalInput").ap()

    def dout(name, shape):
        return nc.dram_tensor(name, list(shape), F32, kind="ExternalOutput").ap()

    x_tok = din("x_tok", [34 * 128, D])
    kbias_d = din("kbias", [128, 32])
    c5 = din("c5", [5, D])
    csk = din("csk", [4, 4096, 1024])
    csv = din("csv", [4, 4096, 1024])
    cbk = din("cbk", [4, 512, 1024])
    cbv = din("cbv", [4, 512, 1024])
    btile = din("btile", [128, 24, 128])
    norm_mix = din("norm_mix", [16, 128])
    norm_ffn = din("norm_ffn", [16, 128])
    w_ada = din("w_ada", [D, 6 * D])
    b_ada = din("b_ada", [96, 128])
    w_in = din("w_in", [D, DIN])
    qn_d = din("q_norm", [8, 128])
    kn_d = din("k_norm", [8, 128])
    w_psb = din("w_psb", [1024, D])
    w_pbd = din("w_pbd", [1024, D])
    w_out = din("w_out", [D, D])
    w_rt = din("w_rt", [D, 36])
    b_rt = din("b_rt", [1, 36])
    w_gate = din("w_gate", [32, D, 512])
    w_up = din("w_up", [32, D, 512])
    w_down = din("w_down", [32, 512, D])

    y_out = dout("y_out", [NOWN, D])
    ksb_out = dout("ksb_out", [NOWN, 1024])
    vsb_out = dout("vsb_out", [NOWN, 1024])
    kbd_out = dout("kbd_out", [NOWN, 1024])
    vbd_out = dout("vbd_out", [NOWN, 1024])

    kk = dict(kind="ExternalOutput") if dbg else {}
    KTs = nc.dram_tensor("KTs", [8, 128, 4096], BF16, **kk).ap()
    Vs = nc.dram_tensor("Vs", [8, 4096, 128], BF16, **kk).ap()
    dbg_outs = {}

    def DUMP(R, name, ap, cols, dt, res):
        if not dbg:
            return
        d = nc.dram_tensor("dbg_" + name, [128, cols], dt, kind="ExternalOutput").ap()
        dbg_outs[name] = d
        R.add("sp", lambda e: e.dma_start(out=d, in_=ap), reads=[res], dma="dbg")

    st = ExitStack()
    with st:
        A = Arena(nc, 206 * 1024)
        banks = [nc.alloc_psum_tensor(f"bank{i}", [128, 512], F32) for i in range(8)]
        R = Rec(nc)
        try:
            bk = lambda i: banks[i][:, :]
            uid = [0]

            def U(p):
                uid[0] += 1
                return f"{p}{uid[0]}"

            identf = A.a(128, F32)
            identb = A.a(128, BF16)
            negtri = A.a(128, BF16)
            negones = A.a(128, BF16)
            onesb = A.a(128, BF16)
            dmask = A.a(128, BF16)
            tmpf = A.a(128, F32)
            epsc = A.a(1, F32)
            zeroc = A.a(1, F32)
            onec = A.a(1, F32)
            kbias = A.a(32, F32)
            modT = A.a(96 * 5, F32).rearrange("p (b s) -> p b s", s=5)
            gmm = A.a(80, F32).rearrange("p (b s) -> p b s", s=5)
            gmf = A.a(80, F32).rearrange("p (b s) -> p b s", s=5)
            nmix = A.a(16, F32)
            nffn = A.a(16, F32)
            qgs = A.a(8, F32)
            kg = A.a(8, F32)
            dummy = A.a(16, F32)

            R.add("pool", lambda e: e.memset(identf, 1.0), writes=["identf"])
            R.add("pool", lambda e: e.affine_select(out=identf, in_=identf, pattern=[[-1, 128]], compare_op=ALU.is_equal, fill=0.0, base=0, channel_multiplier=1), reads=["identf"], writes=["identf"])
            R.add("dve", lambda e: e.tensor_copy(out=identb, in_=identf), reads=["identf"], writes=["identb"])
            R.add("pool", lambda e: e.memset(tmpf, -1.0), writes=["tmpf"])
            R.add("pool", lambda e: e.affine_select(out=tmpf, in_=tmpf, pattern=[[-1, 128]], compare_op=ALU.is_ge, fill=0.0, base=0, channel_multiplier=1), reads=["tmpf"], writes=["tmpf"])
            R.add("dve", lambda e: e.tensor_copy(out=negtri, in_=tmpf), reads=["tmpf"], writes=["negtri"])
            R.add("pool", lambda e: e.memset(tmpf, 1.0), reads=["tmpf"], writes=["tmpf"])
            R.add("pool", lambda e: e.affine_select(out=tmpf, in_=tmpf, pattern=[[1, 128]], compare_op=ALU.is_gt, fill=0.0, base=0, channel_multiplier=-1), reads=["tmpf"], writes=["tmpf"])
            R.add("dve", lambda e: e.tensor_copy(out=dmask, in_=tmpf), reads=["tmpf"], writes=["dmask"])
            R.add("pool", lambda e: e.memset(negones, -1.0), writes=["negones"])
            R.add("pool", lambda e: e.memset(onesb, 1.0), writes=["onesb"])
            R.add("pool", lambda e: e.memset(epsc, EPS), writes=["epsc"])
            R.add("pool", lambda e: e.memset(zeroc, 0.0), writes=["zeroc"])
            R.add("pool", lambda e: e.memset(onec, 1.0), writes=["onec"])
            R.add("pool", lambda e: e.memset(dummy, 0.0), writes=["dummy"])
            R.add("sp", lambda e: e.dma_start(out=kbias, in_=kbias_d), writes=["kbias"], dma="kbias")
            stg = A.a(128, F32)

            def load_T(src_ap, rows, dst, post=None):
                n = U("ld")
                R.add("sp", lambda e: e.dma_start(out=stg[0:rows, :], in_=src_ap), writes=["stg"], dma="stg")
                R.add("pe", lambda e: e.transpose(out=banks[7][:, 0:rows], in_=stg[0:rows, :], identity=identf[0:rows, 0:rows]), reads=["stg", "identf"], writes=["b7"])
                if post is None:
                    R.add("dve", lambda e: e.tensor_copy(out=dst, in_=banks[7][:, 0:rows]), reads=["b7"], writes=[n, "cparam"])
                else:
                    R.add("dve", lambda e: e.tensor_scalar(out=dst, in0=banks[7][:, 0:rows], scalar1=post, scalar2=None, op0=ALU.mult), reads=["b7"], writes=[n, "cparam"])

            bT = A.a(96, F32)
            load_T(b_ada, 96, bT)
            load_T(norm_mix, 16, nmix)
            load_T(norm_ffn, 16, nffn)
            load_T(qn_d, 8, qgs, post=SC)
            load_T(kn_d, 8, kg)

            assert A.off <= 8192, A.off
            A.set(173 * 1024)
            scT = A.a(16 * 5, BF16).rearrange("p (k s) -> p k s", s=5)
            A.set(174 * 1024)
            wa = [A.a(16 * 512, BF16).rearrange("p (k n) -> p k n", n=512) for _ in range(2)]
            c_sb = wa[1].rearrange("p k n -> p (k n)")[:, 0:2 * D].bitcast(F32)
            R.add("sp", lambda e: e.dma_start(out=c_sb[0:5, :], in_=c5), writes=["wa1"], dma="c_sb")
            R.add("act", lambda e: e.activation(out=c_sb[0:5, :], in_=c_sb[0:5, :], func=AF.Silu), reads=["wa1"], writes=["wa1"])
            for k in range(16):
                R.add("pe", lambda e, k=k: e.transpose(out=banks[6][:, k * 5:(k + 1) * 5], in_=c_sb[0:5, k * 128:(k + 1) * 128], identity=identf[0:5, 0:5]), reads=["wa1", "identf"], writes=["b6"])
            R.add("dve", lambda e: e.tensor_copy(out=scT.rearrange("p k s -> p (k s)"), in_=banks[6][:, 0:80]), reads=["b6"], writes=["scT"])
            b5v = banks[5][:, 0:480].rearrange("p (b s) -> p b s", s=5)

            def ada_panel(pn):
                wb_ = wa[pn % 2]
                wn = f"wa{pn % 2}"
                R.add("pool", lambda e: e.dma_start(out=wb_, in_=w_ada[:, pn * 512:(pn + 1) * 512].rearrange("(k p) n -> p k n", p=128)), writes=[wn], dma=wn)
                for fb in range(4):
                    blk = pn * 4 + fb
                    for k in range(16):
                        R.add("pe", lambda e, fb=fb, k=k, blk=blk: e.matmul(banks[5][:, blk * 5:(blk + 1) * 5], lhsT=wb_[:, k, fb * 128:(fb + 1) * 128], rhs=scT[:, k, :], start=(k == 0), stop=(k == 15)), reads=[wn, "scT"], writes=["b5"])

            def ada_finish(b0, b1, tok):
                for s in range(5):
                    R.add("dve", lambda e, s=s: e.tensor_tensor(out=modT[:, b0:b1, s], in0=b5v[:, b0:b1, s], in1=bT[:, b0:b1], op=ALU.add), reads=["b5", "cparam"], writes=[tok])

            for pn in range(8):
                ada_panel(pn)
            ada_finish(0, 32, "modT1")
            for s in range(5):
                R.add("dve", lambda e, s=s: e.scalar_tensor_tensor(out=gmm[:, :, s], in0=modT[:, 16:32, s], scalar=1.0, in1=nmix, op0=ALU.add, op1=ALU.mult), reads=["modT1", "cparam"], writes=["gmm"])
            ada_next = [8]

            def ada_more(n):
                for _ in range(n):
                    if ada_next[0] < 24:
                        ada_panel(ada_next[0])
                        ada_next[0] += 1
                        if ada_next[0] == 24:
                            ada_finish(32, 96, "modT2")
                            for s in range(5):
                                R.add("dve", lambda e, s=s: e.scalar_tensor_tensor(out=gmf[:, :, s], in0=modT[:, 64:80, s], scalar=1.0, in1=nffn, op0=ALU.add, op1=ALU.mult), reads=["modT2", "cparam"], writes=["gmf"])


            KB = 1024
            HT0, OAT0, OBT0, PH0 = 8 * KB, 64 * KB, 84 * KB, 104 * KB
            HTC = NOWN + 512

            ncnt = [0]

            def norm_bufs():
                return dict(xts=[A.a(D, F32) for _ in range(2)], junk=A.a(D, BF16), ybs=[A.a(D, BF16) for _ in range(2)], ssq=A.a(4, F32))

            def norm_tile(NB, ti, segs, dst_fn):
                i = ncnt[0] % 2
                ncnt[0] += 1
                xt, yb, junk, ssq = NB["xts"][i], NB["ybs"][i], NB["junk"], NB["ssq"]
                xn, yn = f"xt{i}", f"yb{i}"
                R.add("sp", lambda e: e.dma_start(out=xt, in_=x_tok[ti * 128:(ti + 1) * 128, :]), writes=[xn], dma=xn)
                R.add("dve", lambda e: e.memset(ssq[:, 0:1], 0.0), writes=["ssq"])
                R.add("act", lambda e: e.activation(out=junk, in_=xt, func=AF.Square, accum_out=ssq[:, 0:1]), reads=[xn, "ssq"], writes=["junk", "ssq"])
                R.add("act", lambda e: e.activation(out=ssq[:, 1:2], in_=ssq[:, 0:1], func=AF.Ln, scale=1.0 / D, bias=epsc), reads=["ssq", "epsc"], writes=["ssq"])
                R.add("act", lambda e: e.activation(out=ssq[:, 2:3], in_=ssq[:, 1:2], func=AF.Exp, scale=-0.5), reads=["ssq"], writes=["ssq"])
                R.add("dve", lambda e: e.tensor_scalar(out=yb, in0=xt, scalar1=ssq[:, 2:3], scalar2=None, op0=ALU.mult), reads=[xn, "ssq"], writes=[yn])
                for half in range(2):
                    pb = banks[6 + half][:, 0:512].bitcast(BF16)
                    bn = f"b{6 + half}"
                    for k8 in range(8):
                        dc = half * 8 + k8
                        R.add("pe", lambda e, pb=pb, k8=k8, dc=dc: e.transpose(out=pb[:, k8 * 128:(k8 + 1) * 128], in_=yb[:, dc * 128:(dc + 1) * 128], identity=identb), reads=[yn, "identb"], writes=[bn])
                    for k8 in range(8):
                        dc = half * 8 + k8
                        for (p0, n, seq) in segs:
                            dst, dn = dst_fn(dc, p0, n)
                            R.add("dve", lambda e, pb=pb, k8=k8, dc=dc, p0=p0, n=n, seq=seq, dst=dst: e.tensor_scalar(out=dst, in0=pb[:, k8 * 128 + p0:k8 * 128 + p0 + n], scalar1=gmm[:, dc, seq:seq + 1], scalar2=modT[:, dc, seq:seq + 1], op0=ALU.mult, op1=ALU.add), reads=[bn, "gmm", "modT1"], writes=[dn])

            mmb = [0]

            def next_bank():
                mmb[0] ^= 1
                return mmb[0], f"b{mmb[0]}"

            phase = [0]

            LABELS = ["A", "B1", "SB", "BAND", "MERGE", "WOUT", "ROUTER", "MOE"]

            def FENCE(label=None, soft=False):
                if label is None:
                    label = LABELS[phase[0]]
                    phase[0] += 1
                    if soft and upto != label:
                        return
                elif upto != label:
                    return
                R.barrier(lambda e: e.memset(dummy, 0.0))
                if upto is not None and label == upto:
                    raise _Stop()

            A.set(HT0)
            NB = norm_bufs()
            wkv = A.a(16 * 2048, BF16).rearrange("p (k n) -> p k n", n=2048)
            for q4 in range(4):
                R.add("pool", lambda e, q4=q4: e.dma_start(out=wkv[:, :, q4 * 512:(q4 + 1) * 512], in_=w_in[:, 1024 + q4 * 512:1024 + (q4 + 1) * 512].rearrange("(k p) n -> p k n", p=128)), writes=["wkv"], dma="wkv")
            hTb = [A.a(16 * 512, BF16).rearrange("p (k n) -> p k n", n=512) for _ in range(2)]
            ktblk = [A.a(8 * 512, BF16).rearrange("p (h n) -> p h n", n=512) for _ in range(2)]
            vblk = [A.a(4 * 1024, BF16).rearrange("p (t n) -> p t n", n=1024) for _ in range(2)]
            assert A.off <= 173 * 1024, A.off
            def normA(blk, t4):
                i = blk % 2
                hb, hn = hTb[i], f"hTb{i}"
                norm_tile(NB, blk * 4 + t4, [(0, 128, 0)], lambda dc, p0, n: (hb[:, dc, t4 * 128 + p0:t4 * 128 + p0 + n], hn))

            def projA(blk):
                i = blk % 2
                hb, hn = hTb[i], f"hTb{i}"
                items = []

                def kitem(h):
                    bi, bn = next_bank()
                    for k in range(16):
                        R.add("pe", lambda e, k=k: e.matmul(bk(bi), lhsT=wkv[:, k, h * 128:(h + 1) * 128], rhs=hb[:, k, :], start=(k == 0), stop=(k == 15)), reads=["wkv", hn], writes=[bn])
                    R.add("act", lambda e: e.copy(out=ktblk[i][:, h, :], in_=bk(bi)), reads=[bn], writes=[f"ktblk{i}"])
                    if h == 7:
                        R.add("sp", lambda e: e.dma_start(out=KTs[:, :, blk * 512:(blk + 1) * 512].rearrange("h p n -> p h n"), in_=ktblk[i]), reads=[f"ktblk{i}"], writes=["KTs"], dma=f"ktw{i}")

                def vitem(t4, cb):
                    bi, bn = next_bank()
                    for k in range(16):
                        R.add("pe", lambda e, k=k: e.matmul(bk(bi), lhsT=hb[:, k, t4 * 128:(t4 + 1) * 128], rhs=wkv[:, k, 1024 + cb * 512:1024 + (cb + 1) * 512], start=(k == 0), stop=(k == 15)), reads=["wkv", hn], writes=[bn])
                    R.add("act", lambda e: e.copy(out=vblk[i][:, t4, cb * 512:(cb + 1) * 512], in_=bk(bi)), reads=[bn], writes=[f"vblk{i}"])
                    if cb == 1:
                        R.add("sp", lambda e: e.dma_start(out=Vs[:, blk * 512 + t4 * 128:blk * 512 + (t4 + 1) * 128, :].rearrange("h p d -> p h d"), in_=vblk[i][:, t4, :].rearrange("p (h d) -> p h d", d=128)), reads=[f"vblk{i}"], writes=["Vs"], dma=f"vw{i}")

                for h in range(8):
                    items.append(lambda h=h: kitem(h))
                for t4 in range(4):
                    for cb in range(2):
                        items.append(lambda t4=t4, cb=cb: vitem(t4, cb))
                return items

            for t4 in range(4):
                normA(0, t4)
            for blk in range(6):
                items = projA(blk)
                for t4 in range(4):
                    if blk + 1 < 6:
                        normA(blk + 1, t4)
                    for it in items[t4 * 4:(t4 + 1) * 4]:
                        it()
                ada_more(3)
            ada_more(24)
            FENCE()

            A.set(HT0)
            hT = A.a(16 * HTC, BF16).rearrange("p (k n) -> p k n", n=HTC)
            assert A.off <= OAT0
            A.set(OAT0)
            oAT = A.a(8 * NOWN, BF16).rearrange("p (h n) -> p h n", n=NOWN)
            oBT = A.a(8 * NOWN, BF16).rearrange("p (h n) -> p h n", n=NOWN)
            assert A.off <= PH0
            A.set(PH0)
            qT = A.a(8 * NOWN, BF16).rearrange("p (h n) -> p h n", n=NOWN)
            kTS = A.a(8 * 256, BF16).rearrange("p (h n) -> p h n", n=256)
            vS = A.a(4 * 1024, BF16).rearrange("p (t n) -> p t n", n=1024)
            mB1 = A.mark()
            NB = norm_bufs()
            for t4 in range(4):
                norm_tile(NB, 20 + t4, [(0, 128, 0)], lambda dc, p0, n, t4=t4: (hT[:, dc, NOWN + t4 * 128 + p0:NOWN + t4 * 128 + p0 + n], "hT"))
            for t in range(8):
                norm_tile(NB, 24 + t, [(0, 128, 0)], lambda dc, p0, n, t=t: (hT[:, dc, t * 128 + p0:t * 128 + p0 + n], "hT"))
            for t in range(2):
                norm_tile(NB, 32 + t, [(0, 64, 1 + 2 * t), (64, 64, 2 + 2 * t)], lambda dc, p0, n, t=t: (hT[:, dc, 1024 + t * 128 + p0:1024 + t * 128 + p0 + n], "hT"))

            FENCE("B1n")
            wcs = [A.a(16 * 512, BF16).rearrange("p (k n) -> p k n", n=512) for _ in range(2)]
            ostg = [A.a(512, F32) for _ in range(2)]
            osi = [0]
            ktown = [A.a(512, BF16) for _ in range(2)]
            kti = [0]
            vstg = [A.a(512, BF16) for _ in range(2)]
            vsi = [0]
            TMT = [(t * 128, 128) for t in range(8)] + [(1024 + s * 64, 64) for s in range(4)]
            wci = [0]

            def load_wc(cb, nbuf=2):
                i = wci[0] % nbuf
                wci[0] += 1
                buf = wcs[i]
                R.add("pool", lambda e: e.dma_start(out=buf, in_=w_in[:, cb * 512:(cb + 1) * 512].rearrange("(k p) n -> p k n", p=128)), writes=[f"wc{i}"], dma=f"wc{i}")
                return buf, f"wc{i}"

            def fm_proj(wc, wn, hh, c0, n):
                bi, bn = next_bank()
                for k in range(16):
                    R.add("pe", lambda e, k=k: e.matmul(banks[bi][:, 0:n], lhsT=wc[:, k, hh * 128:(hh + 1) * 128], rhs=hT[:, k, c0:c0 + n], start=(k == 0), stop=(k == 15)), reads=[wn, "hT"], writes=[bn])
                return bi, bn

            def tm_proj(wc, wn, c0, n):
                bi, bn = next_bank()
                for k in range(16):
                    R.add("pe", lambda e, k=k: e.matmul(banks[bi][0:n, :], lhsT=hT[:, k, c0:c0 + n], rhs=wc[:, k, :], start=(k == 0), stop=(k == 15)), reads=[wn, "hT"], writes=[bn])
                return bi, bn

            def tm_out(bi, bn, n, dst_ap):
                i = osi[0] % 2
                osi[0] += 1
                ob_ = ostg[i]
                R.add("act", lambda e: e.copy(out=ob_[0:n, :], in_=banks[bi][0:n, :]), reads=[bn], writes=[f"ostg{i}"])
                R.add("sp", lambda e: e.dma_start(out=dst_ap, in_=ob_[0:n, :]), reads=[f"ostg{i}"], dma=f"ostg{i}")

            for cb in range(6):
                ty = cb // 2
                half = cb % 2
                wc, wn = load_wc(cb)
                if ty in (0, 1):
                    for hh in range(4):
                        h = half * 4 + hh
                        for (c0, n) in TBS:
                            bi, bn = fm_proj(wc, wn, hh, c0, n)
                            if ty == 0:
                                R.add("act", lambda e, bi=bi, n=n, h=h, c0=c0: e.activation(out=qT[:, h, c0:c0 + n], in_=banks[bi][:, 0:n], func=AF.Copy, scale=SC), reads=[bn], writes=["qT"])
                            elif c0 < 1024:
                                i = kti[0] % 2
                                kti[0] += 1
                                R.add("act", lambda e, bi=bi, i=i: e.copy(out=ktown[i], in_=bk(bi)), reads=[bn], writes=[f"ktown{i}"])
                                R.add("sp", lambda e, i=i, h=h, c0=c0: e.dma_start(out=KTs[h, :, 3072 + c0:3072 + c0 + 512], in_=ktown[i]), reads=[f"ktown{i}"], writes=["KTs"], dma=f"ktown{i}")
                            else:
                                R.add("act", lambda e, bi=bi, h=h: e.copy(out=kTS[:, h, :], in_=banks[bi][:, 0:256]), reads=[bn], writes=["kTS"])
                if ty in (1, 2):
                    for (c0, n) in TMT:
                        bi, bn = tm_proj(wc, wn, c0, n)
                        dst = ksb_out if ty == 1 else vsb_out
                        tm_out(bi, bn, n, dst[c0:c0 + n, half * 512:(half + 1) * 512])
                        if ty == 2:
                            if c0 < 1024:
                                i = vsi[0] % 2
                                vsi[0] += 1
                                R.add("dve", lambda e, bi=bi, i=i: e.tensor_copy(out=vstg[i], in_=bk(bi)), reads=[bn], writes=[f"vstg{i}"])
                                R.add("sp", lambda e, i=i, c0=c0, half=half: e.dma_start(out=Vs[half * 4:(half + 1) * 4, 3072 + c0:3072 + c0 + 128, :].rearrange("h p d -> p h d"), in_=vstg[i].rearrange("p (h d) -> p h d", d=128)), reads=[f"vstg{i}"], writes=["Vs"], dma=f"vstg{i}")
                            else:
                                s = (c0 - 1024) // 64
                                R.add("dve", lambda e, bi=bi, s=s, half=half: e.tensor_copy(out=vS[0:64, s, half * 512:(half + 1) * 512], in_=banks[bi][0:64, :]), reads=[bn], writes=["vS"])
                FENCE("B1c%d" % cb)
            DUMP(R, "hT", hT.rearrange("p k n -> p (k n)"), 16 * HTC, BF16, "hT")
            DUMP(R, "qT", qT.rearrange("p h n -> p (h n)"), 8 * NOWN, BF16, "qT")
            DUMP(R, "kTS", kTS.rearrange("p h n -> p (h n)"), 8 * 256, BF16, "kTS")
            FENCE()
            A.release(mB1)

            ktH = [A.a(4096, BF16) for _ in range(2)]
            vH = [A.a(32 * 128, BF16).rearrange("p (t d) -> p t d", d=128) for _ in range(2)]
            kraw = [A.a(32 * 128, BF16).rearrange("p (t d) -> p t d", d=128) for _ in range(2)]
            e_sb = [A.a(512, F32) for _ in range(2)]
            sp_sb = [A.a(512, BF16) for _ in range(2)]
            a_sb = [A.a(512, BF16) for _ in range(2)]
            lsum = [A.a(128, BF16) for _ in range(2)]
            oslot = [0, 0]

            def sb_group(sid, q_ap, nq, tiles, first, last, kb_ap, diag, oslot_i, out_ap):
                zb, zn = banks[2 + sid], f"b{2 + sid}"
                tb, tn = banks[4 + sid], f"b{4 + sid}"
                ob = banks[6 + sid][:, oslot_i * 128:oslot_i * 128 + nq]
                on = f"b{6 + sid}"
                G = len(tiles)
                W = G * nq
                nk0 = tiles[0][4]
                en, spn, an, ln = f"e{sid}", f"sp{sid}", f"a{sid}", f"ls{sid}"

                def s0():
                    for i, (kT, kn, v, vn, nk) in enumerate(tiles):
                        R.add("pe", lambda e, i=i, kT=kT, nk=nk: e.matmul(zb[0:nk, i * nq:(i + 1) * nq], lhsT=kT, rhs=q_ap, start=True, stop=True), reads=[kn, "qT"], writes=[zn])

                def s1():
                    R.add("act", lambda e: e.activation(out=e_sb[sid][0:nk0, 0:W], in_=zb[0:nk0, 0:W], func=AF.Exp, bias=kb_ap[0:nk0, :]), reads=[zn, "kbias", "zeroc"], writes=[en])

                def s2():
                    R.add("act", lambda e: e.activation(out=sp_sb[sid][0:nk0, 0:W], in_=e_sb[sid][0:nk0, 0:W], func=AF.Ln, bias=onec[0:nk0, :]), reads=[en, "onec"], writes=[spn])
                    if diag:
                        R.add("dve", lambda e: e.tensor_tensor(out=sp_sb[sid][0:nk0, 0:nq], in0=sp_sb[sid][0:nk0, 0:nq], in1=dmask[0:nk0, 0:nq], op=ALU.mult), reads=[spn, "dmask"], writes=[spn])

                def s3():
                    for i, (kT, kn, v, vn, nk) in enumerate(tiles):
                        sl = slice(i * nq, (i + 1) * nq)
                        R.add("pe", lambda e, sl=sl, nk=nk: e.matmul(tb[0:nk, sl], lhsT=negtri[0:nk, 0:nk], rhs=sp_sb[sid][0:nk, sl], start=True, stop=False), reads=[spn, "negtri"], writes=[tn])
                        if not first:
                            R.add("pe", lambda e, sl=sl, nk=nk: e.matmul(tb[0:nk, sl], lhsT=negones[:, 0:nk], rhs=lsum[sid][:, 0:nq], start=False, stop=False), reads=[ln, "negones"], writes=[tn])
                        for i2 in range(i):
                            nk2 = tiles[i2][4]
                            R.add("pe", lambda e, sl=sl, nk=nk, i2=i2, nk2=nk2: e.matmul(tb[0:nk, sl], lhsT=negones[0:nk2, 0:nk], rhs=sp_sb[sid][0:nk2, i2 * nq:(i2 + 1) * nq], start=False, stop=False), reads=[spn, "negones"], writes=[tn])
                        R.add("pe", lambda e, sl=sl, nk=nk, kT=kT: e.matmul(tb[0:nk, sl], lhsT=kT, rhs=q_ap, start=False, stop=True), reads=[kn, "qT"], writes=[tn])
                    for i, (kT, kn, v, vn, nk) in enumerate(tiles):
                        if first and i == 0:
                            R.add("dve", lambda e: e.memset(lsum[sid], 0.0), writes=[ln])
                        R.add("dve", lambda e, i=i, nk=nk: e.tensor_tensor(out=lsum[sid][0:nk, 0:nq], in0=lsum[sid][0:nk, 0:nq], in1=sp_sb[sid][0:nk, i * nq:(i + 1) * nq], op=ALU.add), reads=[ln, spn], writes=[ln])

                def s4():
                    R.add("act", lambda e: e.activation(out=a_sb[sid][0:nk0, 0:W], in_=tb[0:nk0, 0:W], func=AF.Exp, bias=kb_ap[0:nk0, :]), reads=[tn, "kbias", "zeroc"], writes=[an])
                    if diag:
                        R.add("dve", lambda e: e.tensor_tensor(out=a_sb[sid][0:nk0, 0:nq], in0=a_sb[sid][0:nk0, 0:nq], in1=dmask[0:nk0, 0:nq], op=ALU.mult), reads=[an, "dmask"], writes=[an])

                def s5():
                    for i, (kT, kn, v, vn, nk) in enumerate(tiles):
                        R.add("pe", lambda e, i=i, v=v, nk=nk: e.matmul(ob, lhsT=v, rhs=a_sb[sid][0:nk, i * nq:(i + 1) * nq], start=(first and i == 0), stop=(last and i == G - 1)), reads=[vn, an], writes=[on])
                    if last:
                        R.add("dve", lambda e: e.tensor_copy(out=out_ap, in_=ob), reads=[on], writes=["oAT"])

                return [s0, s1, s2, s3, s4, s5]

            def interleave(streams):
                rows = list(zip_longest(*streams))

                def call(r, k):
                    if r is None:
                        return
                    for g in r:
                        if g is not None:
                            g[k]()

                if not rows:
                    return
                for k in range(3):
                    call(rows[0], k)
                for ri in range(len(rows)):
                    nxt = rows[ri + 1] if ri + 1 < len(rows) else None
                    call(rows[ri], 3)
                    call(nxt, 0)
                    call(rows[ri], 4)
                    call(nxt, 1)
                    call(nxt, 2)
                    call(rows[ri], 5)

            def chunks(lst, n):
                return [lst[i:i + n] for i in range(0, len(lst), n)]

            for hp in range(4):
                streams = []
                for sid in range(2):
                    h = hp * 2 + sid
                    R.add("sp", lambda e, sid=sid, h=h: e.dma_start(out=ktH[sid], in_=KTs[h]), reads=["KTs"], writes=[f"ktH{sid}"], dma=f"ktH{sid}")
                    R.add("sp", lambda e, sid=sid, h=h: e.dma_start(out=vH[sid], in_=Vs[h].rearrange("(t p) d -> p t d", p=128)), reads=["Vs"], writes=[f"vH{sid}"], dma=f"vH{sid}")
                    gl = []
                    for t in range(8):
                        q_ap = qT[:, h, t * 128:(t + 1) * 128]
                        tl = lambda j, sid=sid: (ktH[sid][:, j * 128:(j + 1) * 128], f"ktH{sid}", vH[sid][:, j, :], f"vH{sid}", 128)
                        groups = [([tl(24 + t)], zeroc, True)]
                        for ch in chunks(list(range(24 + t - 1, 23, -1)), 4):
                            groups.append(([tl(j) for j in ch], zeroc, False))
                        for ch in chunks(list(range(23, -1, -1)), 4):
                            groups.append(([tl(j) for j in ch], kbias[:, ch[0]:ch[0] + 1], False))
                        osl = oslot[sid] % 4
                        oslot[sid] += 1
                        for gi, (tiles, kb_ap, dg) in enumerate(groups):
                            gl.append(sb_group(sid, q_ap, 128, tiles, gi == 0, gi == len(groups) - 1, kb_ap, dg, osl, oAT[:, h, t * 128:(t + 1) * 128]))
                    streams.append(gl)
                interleave(streams)

            for s in range(4):
                for hp in range(4):
                    streams = []
                    for sid in range(2):
                        h = hp * 2 + sid
                        R.add("pool", lambda e, sid=sid, h=h, s=s: e.dma_start(out=kraw[sid], in_=csk[s, :, h * 128:(h + 1) * 128].rearrange("(t p) d -> p t d", p=128)), writes=[f"kraw{sid}"], dma=f"kraw{sid}")
                        R.add("pool", lambda e, sid=sid, h=h, s=s: e.dma_start(out=vH[sid], in_=csv[s, :, h * 128:(h + 1) * 128].rearrange("(t p) d -> p t d", p=128)), writes=[f"vH{sid}"], dma=f"vH{sid}")
                        for t8 in range(4):
                            pb = banks[sid][:, 0:512].bitcast(BF16)
                            for j in range(8):
                                R.add("pe", lambda e, pb=pb, j=j, t8=t8, sid=sid: e.transpose(out=pb[:, j * 128:(j + 1) * 128], in_=kraw[sid][:, t8 * 8 + j, :], identity=identb), reads=[f"kraw{sid}", "identb"], writes=[f"b{sid}"])
                            R.add("dve", lambda e, pb=pb, t8=t8, sid=sid: e.tensor_copy(out=ktH[sid][:, t8 * 1024:(t8 + 1) * 1024], in_=pb), reads=[f"b{sid}"], writes=[f"ktH{sid}"])
                        q_ap = qT[:, h, 1024 + s * 64:1024 + (s + 1) * 64]
                        tl = lambda j, sid=sid: (ktH[sid][:, j * 128:(j + 1) * 128], f"ktH{sid}", vH[sid][:, j, :], f"vH{sid}", 128)
                        groups = [([(kTS[:, h, s * 64:(s + 1) * 64], "kTS", vS[0:64, s, h * 128:(h + 1) * 128], "vS", 64)], True)]
                        for ch in chunks(list(range(31, -1, -1)), 4):
                            groups.append(([tl(j) for j in ch], False))
                        osl = oslot[sid] % 4
                        oslot[sid] += 1
                        gl = []
                        for gi, (tiles, dg) in enumerate(groups):
                            gl.append(sb_group(sid, q_ap, 64, tiles, gi == 0, gi == len(groups) - 1, zeroc, dg, osl, oAT[:, h, 1024 + s * 64:1024 + (s + 1) * 64]))
                        streams.append(gl)
                    interleave(streams)
            DUMP(R, "oAT", oAT.rearrange("p h n -> p (h n)"), 8 * NOWN, BF16, "oAT")
            FENCE()

            A.set(PH0)
            bt = A.a(32 * 128, F32).rearrange("p (t q) -> p t q", q=128)
            for h in range(8):
                for j in range(3):
                    R.add("sp", lambda e, h=h, j=j: e.dma_start(out=bt[:, h * 4 + j, :], in_=btile[:, h * 3 + j, :]), writes=["bt"], dma="bt")
            for h in range(8):
                R.add("dve", lambda e, h=h: e.tensor_copy(out=bt[:, h * 4 + 3, :], in_=bt[:, h * 4 + 2, :]), reads=["bt"], writes=["bt"])
                R.add("pool", lambda e, h=h: e.memset(bt[0:64, h * 4 + 3, 64:128], NEG), reads=["bt"], writes=["bt"])
                R.add("pool", lambda e, h=h: e.memset(bt[64:128, h * 4 + 0, 0:64], NEG), reads=["bt"], writes=["bt"])
            qbT = A.a(4 * NOWN, BF16).rearrange("p (h n) -> p h n", n=NOWN)
            kbT = A.a(4 * HTC, BF16).rearrange("p (h n) -> p h n", n=HTC)
            vbP = A.a(12 * 512, BF16).rearrange("p (t n) -> p t n", n=512)
            vbS = A.a(4 * 512, BF16).rearrange("p (t n) -> p t n", n=512)
            wcs = [A.a(16 * 512, BF16).rearrange("p (k n) -> p k n", n=512)]
            ostg = [A.a(512, F32) for _ in range(2)]
            sqb = A.a(512, BF16)
            rs1 = A.a(512, F32)
            rs2 = A.a(512, F32)
            kn32 = A.a(512, F32)
            sB = [A.a(640, F32) for _ in range(2)]
            pB = [A.a(640, BF16) for _ in range(2)]
            rden = [A.a(128, F32) for _ in range(2)]
            kbc = [A.a(4 * 128, BF16).rearrange("p (t d) -> p t d", d=128) for _ in range(2)]
            vbc = [A.a(4 * 128, BF16).rearrange("p (t d) -> p t d", d=128) for _ in range(2)]
            kbcT = [A.a(512, BF16) for _ in range(2)]

            def band_norm(bi, bn, n, gain_ap, dst_bf, dn, want32):
                R.add("act", lambda e: e.activation(out=sqb[:, 0:n], in_=banks[bi][:, 0:n], func=AF.Square), reads=[bn], writes=["sqb"])
                R.add("pe", lambda e: e.matmul(banks[5][:, 0:n], lhsT=onesb, rhs=sqb[:, 0:n], start=True, stop=True), reads=["sqb", "onesb"], writes=["b5"])
                R.add("act", lambda e: e.activation(out=rs1[:, 0:n], in_=banks[5][:, 0:n], func=AF.Ln, scale=1.0 / 128, bias=epsc), reads=["b5", "epsc"], writes=["rs1"])
                R.add("act", lambda e: e.activation(out=rs2[:, 0:n], in_=rs1[:, 0:n], func=AF.Exp, scale=-0.5), reads=["rs1"], writes=["rs2"])
                if not want32:
                    R.add("dve", lambda e: e.scalar_tensor_tensor(out=dst_bf, in0=banks[bi][:, 0:n], scalar=gain_ap, in1=rs2[:, 0:n], op0=ALU.mult, op1=ALU.mult), reads=[bn, "rs2", "cparam"], writes=[dn])
                else:
                    R.add("dve", lambda e: e.scalar_tensor_tensor(out=kn32[:, 0:n], in0=banks[bi][:, 0:n], scalar=gain_ap, in1=rs2[:, 0:n], op0=ALU.mult, op1=ALU.mult), reads=[bn, "rs2", "cparam"], writes=["kn32"])
                    R.add("act", lambda e: e.copy(out=dst_bf, in_=kn32[:, 0:n]), reads=["kn32"], writes=[dn])

            def band_unit(sid, q_ap, nq, tiles, out_ap):
                z0, zn0 = banks[2 + sid], f"b{2 + sid}"
                dnb, dnn = banks[4 + sid], f"b{4 + sid}"
                ob, on = banks[6 + sid], f"b{6 + sid}"
                G = len(tiles)
                sn, pn, rn = f"sB{sid}", f"pB{sid}", f"rden{sid}"
                zb2, zn2 = banks[sid], f"b{sid}"

                def zslice(i, nk):
                    if i < 4:
                        return z0[0:nk, i * nq:(i + 1) * nq], zn0
                    return zb2[0:nk, 0:nq], zn2

                def s0():
                    for i, (kT, kn, v, vn, nk, b_ap, kb_ap) in enumerate(tiles):
                        zs, zn = zslice(i, nk)
                        R.add("pe", lambda e, zs=zs, kT=kT: e.matmul(zs, lhsT=kT, rhs=q_ap, start=True, stop=True), reads=[kn, "qbT"], writes=[zn])

                def s1():
                    for i, (kT, kn, v, vn, nk, b_ap, kb_ap) in enumerate(tiles):
                        zs, zn = zslice(i, nk)
                        R.add("dve", lambda e, zs=zs, i=i, nk=nk, b_ap=b_ap: e.tensor_tensor(out=sB[sid][0:nk, i * nq:(i + 1) * nq], in0=zs, in1=b_ap, op=ALU.add), reads=[zn, "bt"], writes=[sn])

                def s2():
                    for i, (kT, kn, v, vn, nk, b_ap, kb_ap) in enumerate(tiles):
                        R.add("act", lambda e, i=i, nk=nk, kb_ap=kb_ap: e.activation(out=pB[sid][0:nk, i * nq:(i + 1) * nq], in_=sB[sid][0:nk, i * nq:(i + 1) * nq], func=AF.Exp, bias=kb_ap), reads=[sn, "kbias", "zeroc"], writes=[pn])

                def s3():
                    for i, (kT, kn, v, vn, nk, b_ap, kb_ap) in enumerate(tiles):
                        R.add("pe", lambda e, i=i, nk=nk: e.matmul(dnb[:, 0:nq], lhsT=onesb[0:nk, :], rhs=pB[sid][0:nk, i * nq:(i + 1) * nq], start=(i == 0), stop=(i == G - 1)), reads=[pn, "onesb"], writes=[dnn])
                    for i, (kT, kn, v, vn, nk, b_ap, kb_ap) in enumerate(tiles):
                        R.add("pe", lambda e, i=i, nk=nk, v=v: e.matmul(ob[:, 0:nq], lhsT=v, rhs=pB[sid][0:nk, i * nq:(i + 1) * nq], start=(i == 0), stop=(i == G - 1)), reads=[pn, vn], writes=[on])

                def s4():
                    R.add("dve", lambda e: e.reciprocal(out=rden[sid][:, 0:nq], in_=dnb[:, 0:nq]), reads=[dnn], writes=[rn])

                def s5():
                    R.add("dve", lambda e: e.tensor_tensor(out=out_ap, in0=ob[:, 0:nq], in1=rden[sid][:, 0:nq], op=ALU.mult), reads=[on, rn], writes=["oBT"])

                return [s0, s1, s2, s3, s4, s5]

            FMB = TBS + [(NOWN, 512)]
            for half in range(2):
                for ty in (3, 4, 5):
                    cb = ty * 2 + half
                    wc, wn = load_wc(cb, nbuf=1)
                    if ty in (3, 4):
                        for hh in range(4):
                            h = half * 4 + hh
                            for (c0, n) in (TBS if ty == 3 else FMB):
                                bi, bn = fm_proj(wc, wn, hh, c0, n)
                                if ty == 3:
                                    band_norm(bi, bn, n, qgs[:, h:h + 1], qbT[:, hh, c0:c0 + n], "qbT", False)
                                else:
                                    own = c0 < NOWN
                                    band_norm(bi, bn, n, kg[:, h:h + 1], kbT[:, hh, c0:c0 + n], "kbT", own)
                                    if own:
                                        for j in range(n // 128):
                                            R.add("pe", lambda e, j=j: e.transpose(out=banks[7][:, j * 128:(j + 1) * 128], in_=kn32[:, j * 128:(j + 1) * 128], identity=identf), reads=["kn32", "identf"], writes=["b7"])
                                        i = osi[0] % 2
                                        osi[0] += 1
                                        ob_ = ostg[i]
                                        R.add("act", lambda e, ob_=ob_, n=n: e.copy(out=ob_[:, 0:n], in_=banks[7][:, 0:n]), reads=["b7"], writes=[f"ostg{i}"])
                                        R.add("sp", lambda e, ob_=ob_, n=n, c0=c0, h=h: e.dma_start(out=kbd_out[c0:c0 + n, h * 128:(h + 1) * 128].rearrange("(j p) d -> p j d", p=128), in_=ob_[:, 0:n].rearrange("p (j d) -> p j d", d=128)), reads=[f"ostg{i}"], dma=f"ostg{i}")
                    else:
                        for (c0, n) in TMT + [(NOWN + t4 * 128, 128) for t4 in range(4)]:
                            bi, bn = tm_proj(wc, wn, c0, n)
                            if c0 < NOWN:
                                tm_out(bi, bn, n, vbd_out[c0:c0 + n, half * 512:(half + 1) * 512])
                            if c0 < 1024:
                                t = c0 // 128
                                R.add("dve", lambda e, bi=bi, t=t: e.tensor_copy(out=vbP[:, 4 + t, :], in_=bk(bi)), reads=[bn], writes=["vbP"])
                            elif c0 < NOWN:
                                s = (c0 - 1024) // 64
                                R.add("dve", lambda e, bi=bi, s=s: e.tensor_copy(out=vbS[0:64, s, :], in_=banks[bi][0:64, :]), reads=[bn], writes=["vbS"])
                            else:
                                t4 = (c0 - NOWN) // 128
                                R.add("dve", lambda e, bi=bi, t4=t4: e.tensor_copy(out=vbP[:, t4, :], in_=bk(bi)), reads=[bn], writes=["vbP"])

                def kcol(kt):
                    return NOWN + kt * 128 if kt < 4 else (kt - 4) * 128

                for hp in range(2):
                    streams = []
                    for sid in range(2):
                        hh = hp * 2 + sid
                        h = half * 4 + hh
                        gl = []
                        for t in range(8):
                            tiles = []
                            for j in range(5):
                                kt = 4 + t - j
                                kb_ap = kbias[:, 20 + kt:21 + kt] if kt < 4 else zeroc
                                bidx = h * 4 + (3 if j == 4 else min(j, 2))
                                tiles.append((kbT[:, hh, kcol(kt):kcol(kt) + 128], "kbT", vbP[:, kt, hh * 128:(hh + 1) * 128], "vbP", 128, bt[:, bidx, :], kb_ap))
                            gl.append(band_unit(sid, qbT[:, hh, t * 128:(t + 1) * 128], 128, tiles, oBT[:, h, t * 128:(t + 1) * 128]))
                        streams.append(gl)
                    interleave(streams)
                for s in range(4):
                    for hp in range(2):
                        streams = []
                        for sid in range(2):
                            hh = hp * 2 + sid
                            h = half * 4 + hh
                            R.add("pool", lambda e, sid=sid, h=h, s=s: e.dma_start(out=kbc[sid], in_=cbk[s, :, h * 128:(h + 1) * 128].rearrange("(t p) d -> p t d", p=128)), writes=[f"kbc{sid}"], dma=f"kbc{sid}")
                            R.add("pool", lambda e, sid=sid, h=h, s=s: e.dma_start(out=vbc[sid], in_=cbv[s, :, h * 128:(h + 1) * 128].rearrange("(t p) d -> p t d", p=128)), writes=[f"vbc{sid}"], dma=f"vbc{sid}")
                            pb = banks[sid][:, 0:256].bitcast(BF16)
                            for j in range(4):
                                R.add("pe", lambda e, pb=pb, j=j, sid=sid: e.transpose(out=pb[:, j * 128:(j + 1) * 128], in_=kbc[sid][:, j, :], identity=identb), reads=[f"kbc{sid}", "identb"], writes=[f"b{sid}"])
                            R.add("act", lambda e, pb=pb, sid=sid: e.copy(out=kbcT[sid], in_=pb), reads=[f"b{sid}"], writes=[f"kbcT{sid}"])
                            c0 = 1024 + s * 64
                            tiles = [(kbT[:, hh, c0:c0 + 64], "kbT", vbS[0:64, s, hh * 128:(hh + 1) * 128], "vbS", 64, bt[0:64, h * 4 + 0, 0:64], zeroc[0:64, :])]
                            for m in range(3, -1, -1):
                                jj = 1 if m == 3 else 2
                                tiles.append((kbcT[sid][:, m * 128:(m + 1) * 128], f"kbcT{sid}", vbc[sid][:, m, :], f"vbc{sid}", 128, bt[:, h * 4 + jj, 0:64], zeroc))
                            streams.append([band_unit(sid, qbT[:, hh, c0:c0 + 64], 64, tiles, oBT[:, h, c0:c0 + 64])])
                        interleave(streams)
            DUMP(R, "oBT", oBT.rearrange("p h n -> p (h n)"), 8 * NOWN, BF16, "oBT")
            FENCE()

            A.set(PH0)
            mgT = A.a(16 * NOWN, BF16).rearrange("p (k n) -> p k n", n=NOWN)
            wg_ = [A.a(16 * 128, BF16).rearrange("p (k n) -> p k n", n=128) for _ in range(2)]
            wgb_ = [A.a(16 * 128, BF16).rearrange("p (k n) -> p k n", n=128) for _ in range(2)]
            wpa_ = [A.a(8 * 128, BF16).rearrange("p (k n) -> p k n", n=128) for _ in range(2)]
            wpb_ = [A.a(8 * 128, BF16).rearrange("p (k n) -> p k n", n=128) for _ in range(2)]
            sga = A.a(512, F32)
            sgb = A.a(512, F32)
            m1 = A.a(512, F32)
            m2 = A.a(512, F32)
            for fc in range(16):
                i = fc % 2
                a0, a1 = fc * 128, (fc + 1) * 128
                R.add("pool", lambda e, i=i, a0=a0, a1=a1: e.dma_start(out=wg_[i], in_=w_in[:, 6144 + a0:6144 + a1].rearrange("(k p) n -> p k n", p=128)), writes=[f"wg{i}"], dma=f"wg{i}")
                R.add("pool", lambda e, i=i, a0=a0, a1=a1: e.dma_start(out=wgb_[i], in_=w_in[:, 8192 + a0:8192 + a1].rearrange("(k p) n -> p k n", p=128)), writes=[f"wgb{i}"], dma=f"wgb{i}")
                R.add("pool", lambda e, i=i, a0=a0, a1=a1: e.dma_start(out=wpa_[i], in_=w_psb[:, a0:a1].rearrange("(k p) n -> p k n", p=128)), writes=[f"wpa{i}"], dma=f"wpa{i}")
                R.add("pool", lambda e, i=i, a0=a0, a1=a1: e.dma_start(out=wpb_[i], in_=w_pbd[:, a0:a1].rearrange("(k p) n -> p k n", p=128)), writes=[f"wpb{i}"], dma=f"wpb{i}")
                for (c0, n) in TBS:
                    for k in range(16):
                        R.add("pe", lambda e, i=i, k=k, c0=c0, n=n: e.matmul(banks[0][:, 0:n], lhsT=wg_[i][:, k, :], rhs=hT[:, k, c0:c0 + n], start=(k == 0), stop=(k == 15)), reads=[f"wg{i}", "hT"], writes=["b0"])
                    for k in range(16):
                        R.add("pe", lambda e, i=i, k=k, c0=c0, n=n: e.matmul(banks[1][:, 0:n], lhsT=wgb_[i][:, k, :], rhs=hT[:, k, c0:c0 + n], start=(k == 0), stop=(k == 15)), reads=[f"wgb{i}", "hT"], writes=["b1"])
                    for k in range(8):
                        R.add("pe", lambda e, i=i, k=k, c0=c0, n=n: e.matmul(banks[2][:, 0:n], lhsT=wpa_[i][:, k, :], rhs=oAT[:, k, c0:c0 + n], start=(k == 0), stop=(k == 7)), reads=[f"wpa{i}", "oAT"], writes=["b2"])
                    for k in range(8):
                        R.add("pe", lambda e, i=i, k=k, c0=c0, n=n: e.matmul(banks[3][:, 0:n], lhsT=wpb_[i][:, k, :], rhs=oBT[:, k, c0:c0 + n], start=(k == 0), stop=(k == 7)), reads=[f"wpb{i}", "oBT"], writes=["b3"])
                    R.add("act", lambda e, n=n: e.activation(out=sga[:, 0:n], in_=banks[0][:, 0:n], func=AF.Sigmoid), reads=["b0"], writes=["sga"])
                    R.add("act", lambda e, n=n: e.activation(out=sgb[:, 0:n], in_=banks[1][:, 0:n], func=AF.Sigmoid), reads=["b1"], writes=["sgb"])
                    R.add("dve", lambda e, n=n: e.tensor_tensor(out=m1[:, 0:n], in0=banks[2][:, 0:n], in1=sga[:, 0:n], op=ALU.mult), reads=["b2", "sga"], writes=["m1"])
                    R.add("dve", lambda e, n=n: e.tensor_tensor(out=m2[:, 0:n], in0=banks[3][:, 0:n], in1=sgb[:, 0:n], op=ALU.mult), reads=["b3", "sgb"], writes=["m2"])
                    R.add("dve", lambda e, n=n, fc=fc, c0=c0: e.tensor_tensor(out=mgT[:, fc, c0:c0 + n], in0=m1[:, 0:n], in1=m2[:, 0:n], op=ALU.add), reads=["m1", "m2"], writes=["mgT"])
            DUMP(R, "mgT", mgT.rearrange("p k n -> p (k n)"), 16 * NOWN, BF16, "mgT")
            FENCE()

            A.set(HT0)
            yacc = A.a(16 * NOWN, F32).rearrange("p (k n) -> p k n", n=NOWN)
            h2T = A.a(16 * NOWN, BF16).rearrange("p (k n) -> p k n", n=NOWN)
            A.set(PH0 + 40 * KB)
            xts = [A.a(D, F32) for _ in range(2)]
            wo = [A.a(16 * 128, BF16).rearrange("p (k n) -> p k n", n=128) for _ in range(2)]
            for t in range(10):
                i = t % 2
                R.add("sp", lambda e, i=i, t=t: e.dma_start(out=xts[i], in_=x_tok[(24 + t) * 128:(25 + t) * 128, :]), writes=[f"xx{i}"], dma=f"xx{i}")
                for q4 in range(4):
                    bi, bn = next_bank()
                    for j in range(4):
                        dc = q4 * 4 + j
                        R.add("pe", lambda e, bi=bi, j=j, dc=dc, i=i: e.transpose(out=banks[bi][:, j * 128:(j + 1) * 128], in_=xts[i][:, dc * 128:(dc + 1) * 128], identity=identf), reads=[f"xx{i}", "identf"], writes=[bn])
                    R.add("act", lambda e, bi=bi, q4=q4, t=t: e.copy(out=yacc[:, q4 * 4:(q4 + 1) * 4, t * 128:(t + 1) * 128], in_=banks[bi][:, :].rearrange("p (j n) -> p j n", n=128)), reads=[bn], writes=["yacc"])
            SEG = [(0, 512, 0), (512, 512, 0)] + [(1024 + s * 64, 64, 1 + s) for s in range(4)]
            for dc in range(16):
                i = dc % 2
                R.add("pool", lambda e, i=i, dc=dc: e.dma_start(out=wo[i], in_=w_out[:, dc * 128:(dc + 1) * 128].rearrange("(k p) n -> p k n", p=128)), writes=[f"wo{i}"], dma=f"wo{i}")
                for (c0, n) in TBS:
                    bi, bn = next_bank()
                    for k in range(16):
                        R.add("pe", lambda e, bi=bi, i=i, k=k, c0=c0, n=n: e.matmul(banks[bi][:, 0:n], lhsT=wo[i][:, k, :], rhs=mgT[:, k, c0:c0 + n], start=(k == 0), stop=(k == 15)), reads=[f"wo{i}", "mgT"], writes=[bn])
                    for (s0_, sn_, seq) in SEG:
                        if s0_ < c0 or s0_ >= c0 + n:
                            continue
                        R.add("dve", lambda e, bi=bi, dc=dc, s0_=s0_, sn_=sn_, seq=seq, c0=c0: e.scalar_tensor_tensor(out=yacc[:, dc, s0_:s0_ + sn_], in0=banks[bi][:, s0_ - c0:s0_ - c0 + sn_], scalar=modT[:, 32 + dc, seq:seq + 1], in1=yacc[:, dc, s0_:s0_ + sn_], op0=ALU.mult, op1=ALU.add), reads=[bn, "modT2", "yacc"], writes=["yacc"])
            DUMP(R, "yacc", yacc.rearrange("p k n -> p (k n)"), 16 * NOWN, F32, "yacc")
            FENCE()

            A.set(128 * KB)
            combT = A.a(NOWN, BF16)
            selb = A.a(32 * 128, BF16).rearrange("p (e m) -> p e m", m=128)
            cbc = [A.a(NOWN, BF16) for _ in range(2)]
            mMoE = A.mark()
            sq2 = A.a(512, BF16)
            r1 = A.a(512, F32)
            r2 = A.a(512, F32)
            t32 = A.a(512, F32)
            wr = A.a(16 * 36, BF16).rearrange("p (k n) -> p k n", n=36)
            brt = A.a(36, F32)
            lg = A.a(36, F32)
            wk = A.a(64, F32)
            comb = A.a(32, F32)
            R.add("pool", lambda e: e.dma_start(out=wr, in_=w_rt.rearrange("(k p) n -> p k n", p=128)), writes=["wr"], dma="wr")
            R.add("sp", lambda e: e.dma_start(out=brt, in_=b_rt.partition_broadcast(128)), writes=["brt"], dma="brt")
            R.add("pool", lambda e: e.memset(selb[0:32], 1.0), writes=["selb"])
            R.add("pool", lambda e: e.affine_select(out=selb[0:32], in_=selb[0:32], pattern=[[-1, 32], [0, 128]], compare_op=ALU.is_equal, fill=0.0, base=0, channel_multiplier=1), reads=["selb"], writes=["selb"])
            for (c0, n) in TBS:
                for dc in range(16):
                    R.add("act", lambda e, dc=dc, c0=c0, n=n: e.activation(out=sq2[:, 0:n], in_=yacc[:, dc, c0:c0 + n], func=AF.Square), reads=["yacc"], writes=["sq2"])
                    R.add("pe", lambda e, dc=dc, n=n: e.matmul(banks[2][:, 0:n], lhsT=onesb, rhs=sq2[:, 0:n], start=(dc == 0), stop=(dc == 15)), reads=["sq2", "onesb"], writes=["b2"])
                R.add("act", lambda e, n=n: e.activation(out=r1[:, 0:n], in_=banks[2][:, 0:n], func=AF.Ln, scale=1.0 / D, bias=epsc), reads=["b2", "epsc"], writes=["r1"])
                R.add("act", lambda e, n=n: e.activation(out=r2[:, 0:n], in_=r1[:, 0:n], func=AF.Exp, scale=-0.5), reads=["r1"], writes=["r2"])
                for dc in range(16):
                    R.add("dve", lambda e, dc=dc, c0=c0, n=n: e.tensor_tensor(out=t32[:, 0:n], in0=yacc[:, dc, c0:c0 + n], in1=r2[:, 0:n], op=ALU.mult), reads=["yacc", "r2"], writes=["t32"])
                    for (s0_, sn_, seq) in SEG:
                        if s0_ < c0 or s0_ >= c0 + n:
                            continue
                        R.add("dve", lambda e, dc=dc, s0_=s0_, sn_=sn_, seq=seq, c0=c0: e.tensor_scalar(out=h2T[:, dc, s0_:s0_ + sn_], in0=t32[:, s0_ - c0:s0_ - c0 + sn_], scalar1=gmf[:, dc, seq:seq + 1], scalar2=modT[:, 48 + dc, seq:seq + 1], op0=ALU.mult, op1=ALU.add), reads=["t32", "gmf", "modT2"], writes=["h2T"])
            AXX = mybir.AxisListType.X
            for t in range(10):
                for k in range(16):
                    R.add("pe", lambda e, k=k, t=t: e.matmul(banks[3][:, 0:36], lhsT=h2T[:, k, t * 128:(t + 1) * 128], rhs=wr[:, k, :], start=(k == 0), stop=(k == 15)), reads=["h2T", "wr"], writes=["b3"])
                V = lambda a, b: wk[:, a:b]
                ops = []
                ops.append(lambda e: e.tensor_tensor(out=lg, in0=banks[3][:, 0:36], in1=brt, op=ALU.add))
                ops.append(lambda e: e.reduce_max(out=V(0, 1), in_=lg[:, 0:4], axis=AXX))
                ops.append(lambda e: e.tensor_scalar(out=V(4, 8), in0=lg[:, 0:4], scalar1=V(0, 1), scalar2=None, op0=ALU.is_ge))
                ops.append(lambda e: e.tensor_scalar(out=V(8, 12), in0=lg[:, 0:4], scalar1=V(0, 1), scalar2=None, op0=ALU.subtract))
                for o in ops:
                    R.add("dve", o, reads=["b3", "brt", "lg", "wk"], writes=["lg", "wk"])
                R.add("dve", lambda e: e.memset(V(1, 2), 0.0), reads=["wk"], writes=["wk"])
                R.add("act", lambda e: e.activation(out=V(8, 12), in_=V(8, 12), func=AF.Exp, accum_out=V(1, 2)), reads=["wk"], writes=["wk"])
                ops = []
                ops.append(lambda e: e.reciprocal(out=V(2, 3), in_=V(1, 2)))
                ops.append(lambda e: e.tensor_scalar(out=V(16, 24), in0=lg[:, 4:12], scalar1=V(4, 5), scalar2=None, op0=ALU.mult))
                for g in range(1, 4):
                    ops.append(lambda e, g=g: e.scalar_tensor_tensor(out=V(16, 24), in0=lg[:, 4 + 8 * g:12 + 8 * g], scalar=V(4 + g, 5 + g), in1=V(16, 24), op0=ALU.mult, op1=ALU.add))
                ops.append(lambda e: e.reduce_max(out=V(3, 4), in_=V(16, 24), axis=AXX))
                ops.append(lambda e: e.tensor_scalar(out=V(24, 32), in0=V(16, 24), scalar1=V(3, 4), scalar2=None, op0=ALU.is_ge))
                ops.append(lambda e: e.scalar_tensor_tensor(out=V(32, 40), in0=V(24, 32), scalar=-1e30, in1=V(16, 24), op0=ALU.mult, op1=ALU.add))
                ops.append(lambda e: e.reduce_max(out=V(12, 13), in_=V(32, 40), axis=AXX))
                ops.append(lambda e: e.tensor_scalar(out=V(40, 48), in0=V(32, 40), scalar1=V(12, 13), scalar2=None, op0=ALU.is_ge))
                ops.append(lambda e: e.tensor_tensor(out=V(13, 14), in0=V(12, 13), in1=V(3, 4), op=ALU.subtract))
                for o in ops:
                    R.add("dve", o, reads=["wk", "lg"], writes=["wk"])
                R.add("act", lambda e: e.activation(out=V(13, 14), in_=V(13, 14), func=AF.Exp), reads=["wk"], writes=["wk"])
                ops = []
                ops.append(lambda e: e.tensor_scalar(out=V(14, 15), in0=V(13, 14), scalar1=1.0, scalar2=None, op0=ALU.add))
                ops.append(lambda e: e.reciprocal(out=V(14, 15), in_=V(14, 15)))
                ops.append(lambda e: e.tensor_tensor(out=V(15, 16), in0=V(13, 14), in1=V(14, 15), op=ALU.mult))
                ops.append(lambda e: e.tensor_scalar(out=V(48, 56), in0=V(24, 32), scalar1=V(14, 15), scalar2=None, op0=ALU.mult))
                ops.append(lambda e: e.scalar_tensor_tensor(out=V(48, 56), in0=V(40, 48), scalar=V(15, 16), in1=V(48, 56), op0=ALU.mult, op1=ALU.add))
                ops.append(lambda e: e.tensor_scalar(out=V(48, 56), in0=V(48, 56), scalar1=V(2, 3), scalar2=None, op0=ALU.mult))
                for g in range(4):
                    ops.append(lambda e, g=g: e.tensor_scalar(out=comb[:, g * 8:(g + 1) * 8], in0=V(48, 56), scalar1=V(4 + g, 5 + g), scalar2=None, op0=ALU.mult))
                for o in ops:
                    R.add("dve", o, reads=["wk", "comb"], writes=["wk", "comb"])
                R.add("pe", lambda e: e.transpose(out=banks[3][0:32, 128:256], in_=comb, identity=identf), reads=["comb", "identf"], writes=["b3"])
                R.add("act", lambda e, t=t: e.copy(out=combT[0:32, t * 128:(t + 1) * 128], in_=banks[3][0:32, 128:256]), reads=["b3"], writes=["combT"])
            DUMP(R, "h2T", h2T.rearrange("p k n -> p (k n)"), 16 * NOWN, BF16, "h2T")
            DUMP(R, "combT", combT, NOWN, BF16, "combT")
            FENCE(soft=True)

            wgr = [A.a(16 * 128, BF16).rearrange("p (k n) -> p k n", n=128) for _ in range(3)]
            wur = [A.a(16 * 128, BF16).rearrange("p (k n) -> p k n", n=128) for _ in range(3)]
            wdr = [A.a(4 * 2048, BF16).rearrange("p (k n) -> p k n", n=2048) for _ in range(1)]
            hid = A.a(4 * NOWN, BF16).rearrange("p (k n) -> p k n", n=NOWN)
            sgt = [A.a(512, BF16) for _ in range(2)]
            ui = [0]
            for ex in range(32):
                ci = ex % 2
                di = 0
                for (c0, n) in TBS:
                    R.add("pe", lambda e, ex=ex, c0=c0, n=n: e.matmul(banks[6][:, 0:n], lhsT=selb[0:32, ex, :], rhs=combT[0:32, c0:c0 + n], start=True, stop=True), reads=["selb", "combT"], writes=["b6"])
                    R.add("act", lambda e, ci=ci, c0=c0, n=n: e.copy(out=cbc[ci][:, c0:c0 + n], in_=banks[6][:, 0:n]), reads=["b6"], writes=[f"cbc{ci}"])
                for fc in range(4):
                    u = ui[0] % 3
                    ui[0] += 1
                    R.add("pool", lambda e, ex=ex, fc=fc, u=u: e.dma_start(out=wgr[u], in_=w_gate[ex][:, fc * 128:(fc + 1) * 128].rearrange("(k p) n -> p k n", p=128)), writes=[f"wgr{u}"], dma=f"wgr{u}")
                    R.add("pool", lambda e, ex=ex, fc=fc, u=u: e.dma_start(out=wur[u], in_=w_up[ex][:, fc * 128:(fc + 1) * 128].rearrange("(k p) n -> p k n", p=128)), writes=[f"wur{u}"], dma=f"wur{u}")
                    if fc == 1:
                        R.add("pool", lambda e, ex=ex, di=di: e.dma_start(out=wdr[di], in_=w_down[ex].rearrange("(k p) n -> p k n", p=128)), writes=[f"wd{di}"], dma=f"wd{di}")
                    for bi_, (c0, n) in enumerate(TBS):
                        pg = (bi_ + fc) % 2
                        gb, ub = banks[pg * 2], banks[pg * 2 + 1]
                        gn, un = f"b{pg * 2}", f"b{pg * 2 + 1}"
                        for k in range(16):
                            R.add("pe", lambda e, k=k, u=u, c0=c0, n=n, gb=gb: e.matmul(gb[:, 0:n], lhsT=wgr[u][:, k, :], rhs=h2T[:, k, c0:c0 + n], start=(k == 0), stop=(k == 15)), reads=[f"wgr{u}", "h2T"], writes=[gn])
                        for k in range(16):
                            R.add("pe", lambda e, k=k, u=u, c0=c0, n=n, ub=ub: e.matmul(ub[:, 0:n], lhsT=wur[u][:, k, :], rhs=h2T[:, k, c0:c0 + n], start=(k == 0), stop=(k == 15)), reads=[f"wur{u}", "h2T"], writes=[un])
                        R.add("act", lambda e, pg=pg, n=n, gb=gb: e.activation(out=sgt[pg][:, 0:n], in_=gb[:, 0:n], func=AF.Silu), reads=[gn], writes=[f"sgt{pg}"])
                        R.add("dve", lambda e, pg=pg, n=n, c0=c0, ci=ci: e.tensor_tensor(out=sgt[pg][:, 0:n], in0=sgt[pg][:, 0:n], in1=cbc[ci][:, c0:c0 + n], op=ALU.mult), reads=[f"sgt{pg}", f"cbc{ci}"], writes=[f"sgt{pg}"])
                        R.add("dve", lambda e, pg=pg, n=n, c0=c0, fc=fc, ub=ub: e.tensor_tensor(out=hid[:, fc, c0:c0 + n], in0=ub[:, 0:n], in1=sgt[pg][:, 0:n], op=ALU.mult), reads=[un, f"sgt{pg}"], writes=["hid"])
                for dc in range(16):
                    for bi_, (c0, n) in enumerate(TBS):
                        yb_i = (4, 5, 7)[bi_]
                        yb, yn = banks[yb_i], f"b{yb_i}"
                        for fc in range(4):
                            R.add("pe", lambda e, fc=fc, dc=dc, c0=c0, n=n, yb=yb, di=di: e.matmul(yb[:, 0:n], lhsT=wdr[di][:, fc, dc * 128:(dc + 1) * 128], rhs=hid[:, fc, c0:c0 + n], start=(fc == 0), stop=(fc == 3)), reads=[f"wd{di}", "hid"], writes=[yn])
                        if c0 == 1024:
                            tb_, tn_ = (r1, "r1") if dc % 2 == 0 else (r2, "r2")
                            for sq_ in range(4):
                                R.add("act", lambda e, yb=yb, dc=dc, sq_=sq_, tb_=tb_: e.activation(out=tb_[:, sq_ * 64:(sq_ + 1) * 64], in_=yb[:, sq_ * 64:(sq_ + 1) * 64], func=AF.Copy, scale=modT[:, 80 + dc, 1 + sq_:2 + sq_]), reads=[yn, "modT2"], writes=[tn_])
                            R.add("dve", lambda e, dc=dc, tb_=tb_: e.tensor_tensor(out=yacc[:, dc, 1024:1280], in0=yacc[:, dc, 1024:1280], in1=tb_[:, 0:256], op=ALU.add), reads=[tn_, "yacc"], writes=["yacc"])
                            continue
                        for (s0_, sn_, seq) in SEG:
                            if s0_ < c0 or s0_ >= c0 + n:
                                continue
                            R.add("dve", lambda e, yb=yb, dc=dc, s0_=s0_, sn_=sn_, seq=seq, c0=c0: e.scalar_tensor_tensor(out=yacc[:, dc, s0_:s0_ + sn_], in0=yb[:, s0_ - c0:s0_ - c0 + sn_], scalar=modT[:, 80 + dc, seq:seq + 1], in1=yacc[:, dc, s0_:s0_ + sn_], op0=ALU.mult, op1=ALU.add), reads=[yn, "modT2", "yacc"], writes=["yacc"])
            FENCE()

            A.set(88 * KB)
            yst = [A.a(D, F32) for _ in range(2)]
            for t in range(10):
                i = t % 2
                for q4 in range(4):
                    bi, bn = next_bank()
                    for j in range(4):
                        dc = q4 * 4 + j
                        R.add("pe", lambda e, bi=bi, j=j, dc=dc, t=t: e.transpose(out=banks[bi][:, j * 128:(j + 1) * 128], in_=yacc[:, dc, t * 128:(t + 1) * 128], identity=identf), reads=["yacc", "identf"], writes=[bn])
                    if q4 % 2 == 0:
                        R.add("act", lambda e, bi=bi, q4=q4, i=i: e.copy(out=yst[i][:, q4 * 512:(q4 + 1) * 512], in_=bk(bi)), reads=[bn], writes=[f"yst{i}"])
                    else:
                        R.add("dve", lambda e, bi=bi, q4=q4, i=i: e.tensor_copy(out=yst[i][:, q4 * 512:(q4 + 1) * 512], in_=bk(bi)), reads=[bn], writes=[f"yst{i}"])
                R.add("sp", lambda e, i=i, t=t: e.dma_start(out=y_out[t * 128:(t + 1) * 128, :], in_=yst[i]), reads=[f"yst{i}"], dma=f"yst{i}")
        except _Stop:
            pass
        R.emit(st)
    _DBG['outs'] = dbg_outs
    return nc


_CACHE = {}


def kernel(**inp):
    f = lambda k: np.ascontiguousarray(np.asarray(inp[k], dtype=np.float32))
    xp, xs = f("x_prompt"), f("x_sample")
    csk_, csv_, cbk_, cbv_ = f("cache_sb_k")[0], f("cache_sb_v")[0], f("cache_band_k")[0], f("cache_band_v")[0]
    cp, cs = f("c_prompt"), f("c_sample")
    tab = f("rel_bias_band")[0]
    kl = np.arange(128)[:, None]
    ql = np.arange(128)[None, :]
    btile = np.zeros((128, 24, 128), np.float32)
    for j in range(3):
        idx = np.clip(128 * j + ql - kl, -128, 128) + 128
        for h in range(8):
            btile[:, h * 3 + j, :] = tab[h][idx]
    w_rt = np.ascontiguousarray(np.concatenate([f("w_router_group")[0], f("w_router_expert")[0]], axis=1))
    b_rt = np.ascontiguousarray(np.concatenate([f("b_router_group")[0].reshape(1, 4), f("b_router_expert")[0].reshape(1, 32)], axis=1))
    shared = dict(
        btile=btile, norm_mix=f("norm_mix")[0].reshape(16, 128), norm_ffn=f("norm_ffn")[0].reshape(16, 128),
        w_ada=f("w_ada")[0], b_ada=f("b_ada")[0].reshape(96, 128), w_in=f("w_in")[0],
        q_norm=f("q_norm_band")[0], k_norm=f("k_norm_band")[0], w_psb=f("w_proj_sb")[0], w_pbd=f("w_proj_band")[0],
        w_out=f("w_out")[0], w_rt=w_rt, b_rt=b_rt, w_gate=f("w_gate")[0], w_up=f("w_up")[0], w_down=f("w_down")[0])
    in_maps = []
    for c in range(8):
        b, qi = c // 4, c % 4
        x_tok = np.zeros((34 * 128, D), np.float32)
        npre = 1024 * qi
        if npre:
            x_tok[3072 - npre:3072] = xp[b, 0:npre]
        x_tok[3072:4096] = xp[b, npre:npre + 1024]
        x_tok[4096:4352] = xs[4 * c:4 * c + 4].reshape(256, D)
        kbias = np.zeros((128, 32), np.float32)
        kbias[:, 0:(3072 - npre) // 128] = NEG
        m = dict(shared)
        m.update(x_tok=x_tok, kbias=kbias, c5=np.ascontiguousarray(np.concatenate([cp[b:b + 1], cs[4 * c:4 * c + 4]], axis=0)),
                 csk=np.ascontiguousarray(csk_[4 * c:4 * c + 4].reshape(4, 4096, 1024)), csv=np.ascontiguousarray(csv_[4 * c:4 * c + 4].reshape(4, 4096, 1024)),
                 cbk=np.ascontiguousarray(cbk_[4 * c:4 * c + 4].reshape(4, 512, 1024)), cbv=np.ascontiguousarray(cbv_[4 * c:4 * c + 4].reshape(4, 512, 1024)))
        in_maps.append(m)
    if inp.get("_maps_only"):
        return in_maps
    if "nc" not in _CACHE:
        _CACHE["nc"] = build()
    res = run_bass_kernel_spmd(_CACHE["nc"], in_maps, core_ids=list(range(8)))
    r = res.results
    yp = np.zeros((2, 4096, D), np.float32)
    ys = np.zeros((32, 64, D), np.float32)
    pk = np.zeros((1, 2, 4096, 8, 128), np.float32)
    pv = np.zeros_like(pk)
    pbk = np.zeros((1, 2, 512, 8, 128), np.float32)
    pbv = np.zeros_like(pbk)
    sk = np.zeros((1, 32, 64, 8, 128), np.float32)
    sv = np.zeros_like(sk)
    sbk = np.zeros_like(sk)
    sbv = np.zeros_like(sk)
    for c in range(8):
        b, qi = c // 4, c % 4
        o = r[c]
        sl = slice(1024 * qi, 1024 * qi + 1024)
        yp[b, sl] = o["y_out"][0:1024]
        ys[4 * c:4 * c + 4] = o["y_out"][1024:1280].reshape(4, 64, D)
        pk[0, b, sl] = o["ksb_out"][0:1024].reshape(1024, 8, 128)
        pv[0, b, sl] = o["vsb_out"][0:1024].reshape(1024, 8, 128)
        sk[0, 4 * c:4 * c + 4] = o["ksb_out"][1024:1280].reshape(4, 64, 8, 128)
        sv[0, 4 * c:4 * c + 4] = o["vsb_out"][1024:1280].reshape(4, 64, 8, 128)
        sbk[0, 4 * c:4 * c + 4] = o["kbd_out"][1024:1280].reshape(4, 64, 8, 128)
        sbv[0, 4 * c:4 * c + 4] = o["vbd_out"][1024:1280].reshape(4, 64, 8, 128)
        if qi == 3:
            pbk[0, b] = o["kbd_out"][512:1024].reshape(512, 8, 128)
            pbv[0, b] = o["vbd_out"][512:1024].reshape(512, 8, 128)
    return (yp, ys, pk, pv, pbk, pbv, sk, sv, sbk, sbv)
```

```python
import numpy as np
from contextlib import ExitStack
from itertools import zip_longest
import concourse.bass as bass
import concourse.mybir as mybir
from concourse.bass_utils import run_bass_kernel_spmd

F32 = mybir.dt.float32
BF16 = mybir.dt.bfloat16
AF = mybir.ActivationFunctionType
ALU = mybir.AluOpType

D = 2048
NCH = 16
DIN = 10240
NPRE = 24
NOWN = 1280
SC = 128 ** -0.5
EPS = 1e-6
NEG = -30000.0
ENGS = ("pe", "act", "dve", "pool", "sp")
EPOCH = 3000
BANKS = {f"b{i}" for i in range(8)}
TBS = [(0, 512), (512, 512), (1024, 256)]


class Rec:
    def __init__(self, nc):
        self.nc = nc
        self.ops = []
        self.last_w = {}
        self.readers = {}
        self.dma_keys = []
        self.fence = None
        self.last_eng = {}
        self.last_dma = {}

    def add(self, eng, fn, reads=(), writes=(), dma=None):
        writes = list(writes) + [r for r in reads if r in BANKS]
        reads = [r for r in reads if r not in BANKS]
        oid = len(self.ops)
        deps = set()
        if self.fence is not None:
            deps.add(self.fence)
        for r in reads:
            w = self.last_w.get(r)
            if w is not None:
                deps.add(w)
        for w_ in writes:
            w = self.last_w.get(w_)
            if w is not None:
                deps.add(w)
            deps.update(self.readers.get(w_, {}).values())
        rk = ("d", dma) if dma is not None else ("e", eng)
        for r in reads:
            self.readers.setdefault(r, {})[rk] = oid
        for w_ in writes:
            self.last_w[w_] = oid
            self.readers[w_] = {}
        if dma is not None:
            if dma not in self.dma_keys:
                self.dma_keys.append(dma)
            self.last_dma[dma] = oid
        else:
            self.last_eng[eng] = oid
        self.ops.append(dict(eng=eng, fn=fn, deps=deps, dma=dma))
        return oid

    def barrier(self, fn):
        oid = len(self.ops)
        deps = set(self.last_eng.values()) | set(self.last_dma.values())
        if self.fence is not None:
            deps.add(self.fence)
        self.ops.append(dict(eng="dve", fn=fn, deps=deps, dma=None))
        self.last_eng["dve"] = oid
        self.fence = oid

    def emit(self, stack):
        nc = self.nc
        ops = self.ops
        src = set()
        for o in ops:
            for d in o["deps"]:
                od = ops[d]
                if od["dma"] is None and od["eng"] == "pe" and o["eng"] == "pe" and o["dma"] is None:
                    continue
                src.add(d)
        cnt = {e: 0 for e in ENGS}
        dcnt = {k: 0 for k in self.dma_keys}
        tick = {}
        for i, o in enumerate(ops):
            if o["dma"] is not None:
                dcnt[o["dma"]] += 16
                tick[i] = ("d", o["dma"], 0, dcnt[o["dma"]])
            elif i in src:
                c = cnt[o["eng"]]
                cnt[o["eng"]] += 1
                tick[i] = ("e", o["eng"], c // EPOCH, c % EPOCH + 1)
        sems = {}
        for e in ENGS:
            for ep in range(max(cnt[e] - 1, 0) // EPOCH + 1):
                sems[("e", e, ep)] = stack.enter_context(nc.semaphore(f"s_{e}_{ep}"))
        for k in self.dma_keys:
            assert dcnt[k] < 60000, (k, dcnt[k])
            sems[("d", k, 0)] = stack.enter_context(nc.semaphore(f"d_{k}"))
        engobj = {"pe": nc.tensor, "act": nc.scalar, "dve": nc.vector, "pool": nc.gpsimd, "sp": nc.sync}
        per = {e: [] for e in ENGS}
        for i, o in enumerate(ops):
            per[o["eng"]].append(i)
        final = dict(dcnt)

        def run_engine(e):
            eng = engobj[e]
            seen = {}
            for i in per[e]:
                o = ops[i]
                need = {}
                for d in o["deps"]:
                    if d not in tick:
                        continue
                    kind, key, ep, val = tick[d]
                    if kind == "e" and key == e and e == "pe" and o["dma"] is None:
                        continue
                    sk = (kind, key, ep)
                    if seen.get(sk, 0) >= val:
                        continue
                    if need.get(sk, 0) < val:
                        need[sk] = val
                items = list(need.items())
                for sk, val in items[:-1]:
                    eng.wait_ge(sems[sk], val)
                    seen[sk] = val
                ins = o["fn"](eng)
                if items:
                    sk, val = items[-1]
                    ins._wait_ge(sems[sk], val)
                    seen[sk] = val
                if i in tick:
                    kind, key, ep, val = tick[i]
                    if kind == "d":
                        ins.then_inc(sems[("d", key, 0)], 16)
                    else:
                        ins.then_inc(sems[("e", key, ep)], 1)
            if e == "sp":
                for k, v in final.items():
                    if v > 0:
                        eng.wait_ge(sems[("d", k, 0)], v)

        with nc.Block() as block:
            @block.tensor
            def _(t):
                run_engine("pe")

            @block.scalar
            def _(t):
                run_engine("act")

            @block.vector
            def _(t):
                run_engine("dve")

            @block.gpsimd
            def _(t):
                run_engine("pool")

            @block.sync
            def _(t):
                run_engine("sp")


class Arena:
    def __init__(self, nc, nbytes):
        self.t = nc.alloc_sbuf_tensor("arena", [128, nbytes], mybir.dt.uint8)
        self.n = nbytes
        self.off = 0

    def mark(self):
        return self.off

    def set(self, off):
        self.off = off

    def release(self, m):
        self.off = m

    def a(self, cols, dt):
        nb = cols * (4 if dt == F32 else 2)
        nb = (nb + 63) // 64 * 64
        assert self.off + nb <= self.n, ("SBUF overflow", self.off, nb)
        ap = self.t[:, self.off:self.off + cols * (4 if dt == F32 else 2)].bitcast(dt)
        self.off += nb
        return ap


_DBG = {}


class _Stop(Exception):
    pass


def build(upto=None, dbg=False):
    nc = bass.Bass("TRN2", target_bir_lowering=False)

    def din(name, shape, dt=F32):
        return nc.dram_tensor(name, list(shape), dt, kind="ExternalInput").ap()

    def dout(name, shape):
        return nc.dram_tensor(name, list(shape), F32, kind="ExternalOutput").ap()

    x_tok = din("x_tok", [34 * 128, D])
    kbias_d = din("kbias", [128, 32])
    c5 = din("c5", [5, D])
    csk = din("csk", [4, 4096, 1024])
    csv = din("csv", [4, 4096, 1024])
    cbk = din("cbk", [4, 512, 1024])
    cbv = din("cbv", [4, 512, 1024])
    btile = din("btile", [128, 24, 128])
    norm_mix = din("norm_mix", [16, 128])
    norm_ffn = din("norm_ffn", [16, 128])
    w_ada = din("w_ada", [D, 6 * D])
    b_ada = din("b_ada", [96, 128])
    w_in = din("w_in", [D, DIN])
    qn_d = din("q_norm", [8, 128])
    kn_d = din("k_norm", [8, 128])
    w_psb = din("w_psb", [1024, D])
    w_pbd = din("w_pbd", [1024, D])
    w_out = din("w_out", [D, D])
    w_rt = din("w_rt", [D, 36])
    b_rt = din("b_rt", [1, 36])
    w_gate = din("w_gate", [32, D, 512])
    w_up = din("w_up", [32, D, 512])
    w_down = din("w_down", [32, 512, D])

    y_out = dout("y_out", [NOWN, D])
    ksb_out = dout("ksb_out", [NOWN, 1024])
    vsb_out = dout("vsb_out", [NOWN, 1024])
    kbd_out = dout("kbd_out", [NOWN, 1024])
    vbd_out = dout("vbd_out", [NOWN, 1024])

    kk = dict(kind="ExternalOutput") if dbg else {}
    KTs = nc.dram_tensor("KTs", [8, 128, 4096], BF16, **kk).ap()
    Vs = nc.dram_tensor("Vs", [8, 4096, 128], BF16, **kk).ap()
    dbg_outs = {}

    def DUMP(R, name, ap, cols, dt, res):
        if not dbg:
            return
        d = nc.dram_tensor("dbg_" + name, [128, cols], dt, kind="ExternalOutput").ap()
        dbg_outs[name] = d
        R.add("sp", lambda e: e.dma_start(out=d, in_=ap), reads=[res], dma="dbg")

    st = ExitStack()
    with st:
        A = Arena(nc, 206 * 1024)
        banks = [nc.alloc_psum_tensor(f"bank{i}", [128, 512], F32) for i in range(8)]
        R = Rec(nc)
        try:
            bk = lambda i: banks[i][:, :]
            uid = [0]

            def U(p):
                uid[0] += 1
                return f"{p}{uid[0]}"

            identf = A.a(128, F32)
            identb = A.a(128, BF16)
            negtri = A.a(128, BF16)
            negones = A.a(128, BF16)
            onesb = A.a(128, BF16)
            dmask = A.a(128, BF16)
            tmpf = A.a(128, F32)
            epsc = A.a(1, F32)
            zeroc = A.a(1, F32)
            onec = A.a(1, F32)
            kbias = A.a(32, F32)
            modT = A.a(96 * 5, F32).rearrange("p (b s) -> p b s", s=5)
            gmm = A.a(80, F32).rearrange("p (b s) -> p b s", s=5)
            gmf = A.a(80, F32).rearrange("p (b s) -> p b s", s=5)
            nmix = A.a(16, F32)
            nffn = A.a(16, F32)
            qgs = A.a(8, F32)
            kg = A.a(8, F32)
            dummy = A.a(16, F32)

            R.add("pool", lambda e: e.memset(identf, 1.0), writes=["identf"])
            R.add("pool", lambda e: e.affine_select(out=identf, in_=identf, pattern=[[-1, 128]], compare_op=ALU.is_equal, fill=0.0, base=0, channel_multiplier=1), reads=["identf"], writes=["identf"])
            R.add("dve", lambda e: e.tensor_copy(out=identb, in_=identf), reads=["identf"], writes=["identb"])
            R.add("pool", lambda e: e.memset(tmpf, -1.0), writes=["tmpf"])
            R.add("pool", lambda e: e.affine_select(out=tmpf, in_=tmpf, pattern=[[-1, 128]], compare_op=ALU.is_ge, fill=0.0, base=0, channel_multiplier=1), reads=["tmpf"], writes=["tmpf"])
            R.add("dve", lambda e: e.tensor_copy(out=negtri, in_=tmpf), reads=["tmpf"], writes=["negtri"])
            R.add("pool", lambda e: e.memset(tmpf, 1.0), reads=["tmpf"], writes=["tmpf"])
            R.add("pool", lambda e: e.affine_select(out=tmpf, in_=tmpf, pattern=[[1, 128]], compare_op=ALU.is_gt, fill=0.0, base=0, channel_multiplier=-1), reads=["tmpf"], writes=["tmpf"])
            R.add("dve", lambda e: e.tensor_copy(out=dmask, in_=tmpf), reads=["tmpf"], writes=["dmask"])
            R.add("pool", lambda e: e.memset(negones, -1.0), writes=["negones"])
            R.add("pool", lambda e: e.memset(onesb, 1.0), writes=["onesb"])
            R.add("pool", lambda e: e.memset(epsc, EPS), writes=["epsc"])
            R.add("pool", lambda e: e.memset(zeroc, 0.0), writes=["zeroc"])
            R.add("pool", lambda e: e.memset(onec, 1.0), writes=["onec"])
            R.add("pool", lambda e: e.memset(dummy, 0.0), writes=["dummy"])
            R.add("sp", lambda e: e.dma_start(out=kbias, in_=kbias_d), writes=["kbias"], dma="kbias")
            stg = A.a(128, F32)

            def load_T(src_ap, rows, dst, post=None):
                n = U("ld")
                R.add("sp", lambda e: e.dma_start(out=stg[0:rows, :], in_=src_ap), writes=["stg"], dma="stg")
                R.add("pe", lambda e: e.transpose(out=banks[7][:, 0:rows], in_=stg[0:rows, :], identity=identf[0:rows, 0:rows]), reads=["stg", "identf"], writes=["b7"])
                if post is None:
                    R.add("dve", lambda e: e.tensor_copy(out=dst, in_=banks[7][:, 0:rows]), reads=["b7"], writes=[n, "cparam"])
                else:
                    R.add("dve", lambda e: e.tensor_scalar(out=dst, in0=banks[7][:, 0:rows], scalar1=post, scalar2=None, op0=ALU.mult), reads=["b7"], writes=[n, "cparam"])

            bT = A.a(96, F32)
            load_T(b_ada, 96, bT)
            load_T(norm_mix, 16, nmix)
            load_T(norm_ffn, 16, nffn)
            load_T(qn_d, 8, qgs, post=SC)
            load_T(kn_d, 8, kg)

            assert A.off <= 8192, A.off
            A.set(173 * 1024)
            scT = A.a(16 * 5, BF16).rearrange("p (k s) -> p k s", s=5)
            A.set(174 * 1024)
            wa = [A.a(16 * 512, BF16).rearrange("p (k n) -> p k n", n=512) for _ in range(2)]
            c_sb = wa[1].rearrange("p k n -> p (k n)")[:, 0:2 * D].bitcast(F32)
            R.add("sp", lambda e: e.dma_start(out=c_sb[0:5, :], in_=c5), writes=["wa1"], dma="c_sb")
            R.add("act", lambda e: e.activation(out=c_sb[0:5, :], in_=c_sb[0:5, :], func=AF.Silu), reads=["wa1"], writes=["wa1"])
            for k in range(16):
                R.add("pe", lambda e, k=k: e.transpose(out=banks[6][:, k * 5:(k + 1) * 5], in_=c_sb[0:5, k * 128:(k + 1) * 128], identity=identf[0:5, 0:5]), reads=["wa1", "identf"], writes=["b6"])
            R.add("dve", lambda e: e.tensor_copy(out=scT.rearrange("p k s -> p (k s)"), in_=banks[6][:, 0:80]), reads=["b6"], writes=["scT"])
            b5v = banks[5][:, 0:480].rearrange("p (b s) -> p b s", s=5)

            def ada_panel(pn):
                wb_ = wa[pn % 2]
                wn = f"wa{pn % 2}"
                R.add("pool", lambda e: e.dma_start(out=wb_, in_=w_ada[:, pn * 512:(pn + 1) * 512].rearrange("(k p) n -> p k n", p=128)), writes=[wn], dma=wn)
                for fb in range(4):
                    blk = pn * 4 + fb
                    for k in range(16):
                        R.add("pe", lambda e, fb=fb, k=k, blk=blk: e.matmul(banks[5][:, blk * 5:(blk + 1) * 5], lhsT=wb_[:, k, fb * 128:(fb + 1) * 128], rhs=scT[:, k, :], start=(k == 0), stop=(k == 15)), reads=[wn, "scT"], writes=["b5"])

            def ada_finish(b0, b1, tok):
                for s in range(5):
                    R.add("dve", lambda e, s=s: e.tensor_tensor(out=modT[:, b0:b1, s], in0=b5v[:, b0:b1, s], in1=bT[:, b0:b1], op=ALU.add), reads=["b5", "cparam"], writes=[tok])

            for pn in range(8):
                ada_panel(pn)
            ada_finish(0, 32, "modT1")
            for s in range(5):
                R.add("dve", lambda e, s=s: e.scalar_tensor_tensor(out=gmm[:, :, s], in0=modT[:, 16:32, s], scalar=1.0, in1=nmix, op0=ALU.add, op1=ALU.mult), reads=["modT1", "cparam"], writes=["gmm"])
            ada_next = [8]

            def ada_more(n):
                for _ in range(n):
                    if ada_next[0] < 24:
                        ada_panel(ada_next[0])
                        ada_next[0] += 1
                        if ada_next[0] == 24:
                            ada_finish(32, 96, "modT2")
                            for s in range(5):
                                R.add("dve", lambda e, s=s: e.scalar_tensor_tensor(out=gmf[:, :, s], in0=modT[:, 64:80, s], scalar=1.0, in1=nffn, op0=ALU.add, op1=ALU.mult), reads=["modT2", "cparam"], writes=["gmf"])


            KB = 1024
            HT0, OAT0, OBT0, PH0 = 8 * KB, 64 * KB, 84 * KB, 104 * KB
            HTC = NOWN + 512

            ncnt = [0]

            def norm_bufs():
                return dict(xts=[A.a(D, F32) for _ in range(2)], junk=A.a(D, BF16), ybs=[A.a(D, BF16) for _ in range(2)], ssq=A.a(4, F32))

            def norm_tile(NB, ti, segs, dst_fn):
                i = ncnt[0] % 2
                ncnt[0] += 1
                xt, yb, junk, ssq = NB["xts"][i], NB["ybs"][i], NB["junk"], NB["ssq"]
                xn, yn = f"xt{i}", f"yb{i}"
                R.add("sp", lambda e: e.dma_start(out=xt, in_=x_tok[ti * 128:(ti + 1) * 128, :]), writes=[xn], dma=xn)
                R.add("dve", lambda e: e.memset(ssq[:, 0:1], 0.0), writes=["ssq"])
                R.add("act", lambda e: e.activation(out=junk, in_=xt, func=AF.Square, accum_out=ssq[:, 0:1]), reads=[xn, "ssq"], writes=["junk", "ssq"])
                R.add("act", lambda e: e.activation(out=ssq[:, 1:2], in_=ssq[:, 0:1], func=AF.Ln, scale=1.0 / D, bias=epsc), reads=["ssq", "epsc"], writes=["ssq"])
                R.add("act", lambda e: e.activation(out=ssq[:, 2:3], in_=ssq[:, 1:2], func=AF.Exp, scale=-0.5), reads=["ssq"], writes=["ssq"])
                R.add("dve", lambda e: e.tensor_scalar(out=yb, in0=xt, scalar1=ssq[:, 2:3], scalar2=None, op0=ALU.mult), reads=[xn, "ssq"], writes=[yn])
                for half in range(2):
                    pb = banks[6 + half][:, 0:512].bitcast(BF16)
                    bn = f"b{6 + half}"
                    for k8 in range(8):
                        dc = half * 8 + k8
                        R.add("pe", lambda e, pb=pb, k8=k8, dc=dc: e.transpose(out=pb[:, k8 * 128:(k8 + 1) * 128], in_=yb[:, dc * 128:(dc + 1) * 128], identity=identb), reads=[yn, "identb"], writes=[bn])
                    for k8 in range(8):
                        dc = half * 8 + k8
                        for (p0, n, seq) in segs:
                            dst, dn = dst_fn(dc, p0, n)
                            R.add("dve", lambda e, pb=pb, k8=k8, dc=dc, p0=p0, n=n, seq=seq, dst=dst: e.tensor_scalar(out=dst, in0=pb[:, k8 * 128 + p0:k8 * 128 + p0 + n], scalar1=gmm[:, dc, seq:seq + 1], scalar2=modT[:, dc, seq:seq + 1], op0=ALU.mult, op1=ALU.add), reads=[bn, "gmm", "modT1"], writes=[dn])

            mmb = [0]

            def next_bank():
                mmb[0] ^= 1
                return mmb[0], f"b{mmb[0]}"

            phase = [0]

            LABELS = ["A", "B1", "SB", "BAND", "MERGE", "WOUT", "ROUTER", "MOE"]

            def FENCE(label=None, soft=False):
                if label is None:
                    label = LABELS[phase[0]]
                    phase[0] += 1
                    if soft and upto != label:
                        return
                elif upto != label:
                    return
                R.barrier(lambda e: e.memset(dummy, 0.0))
                if upto is not None and label == upto:
                    raise _Stop()

            A.set(HT0)
            NB = norm_bufs()
            wkv = A.a(16 * 2048, BF16).rearrange("p (k n) -> p k n", n=2048)
            for q4 in range(4):
                R.add("pool", lambda e, q4=q4: e.dma_start(out=wkv[:, :, q4 * 512:(q4 + 1) * 512], in_=w_in[:, 1024 + q4 * 512:1024 + (q4 + 1) * 512].rearrange("(k p) n -> p k n", p=128)), writes=["wkv"], dma="wkv")
            hTb = [A.a(16 * 512, BF16).rearrange("p (k n) -> p k n", n=512) for _ in range(2)]
            ktblk = [A.a(8 * 512, BF16).rearrange("p (h n) -> p h n", n=512) for _ in range(2)]
            vblk = [A.a(4 * 1024, BF16).rearrange("p (t n) -> p t n", n=1024) for _ in range(2)]
            assert A.off <= 173 * 1024, A.off
            def normA(blk, t4):
                i = blk % 2
                hb, hn = hTb[i], f"hTb{i}"
                norm_tile(NB, blk * 4 + t4, [(0, 128, 0)], lambda dc, p0, n: (hb[:, dc, t4 * 128 + p0:t4 * 128 + p0 + n], hn))

            def projA(blk):
                i = blk % 2
                hb, hn = hTb[i], f"hTb{i}"
                items = []

                def kitem(h):
                    bi, bn = next_bank()
                    for k in range(16):
                        R.add("pe", lambda e, k=k: e.matmul(bk(bi), lhsT=wkv[:, k, h * 128:(h + 1) * 128], rhs=hb[:, k, :], start=(k == 0), stop=(k == 15)), reads=["wkv", hn], writes=[bn])
                    R.add("act", lambda e: e.copy(out=ktblk[i][:, h, :], in_=bk(bi)), reads=[bn], writes=[f"ktblk{i}"])
                    if h == 7:
                        R.add("sp", lambda e: e.dma_start(out=KTs[:, :, blk * 512:(blk + 1) * 512].rearrange("h p n -> p h n"), in_=ktblk[i]), reads=[f"ktblk{i}"], writes=["KTs"], dma=f"ktw{i}")

                def vitem(t4, cb):
                    bi, bn = next_bank()
                    for k in range(16):
                        R.add("pe", lambda e, k=k: e.matmul(bk(bi), lhsT=hb[:, k, t4 * 128:(t4 + 1) * 128], rhs=wkv[:, k, 1024 + cb * 512:1024 + (cb + 1) * 512], start=(k == 0), stop=(k == 15)), reads=["wkv", hn], writes=[bn])
                    R.add("act", lambda e: e.copy(out=vblk[i][:, t4, cb * 512:(cb + 1) * 512], in_=bk(bi)), reads=[bn], writes=[f"vblk{i}"])
                    if cb == 1:
                        R.add("sp", lambda e: e.dma_start(out=Vs[:, blk * 512 + t4 * 128:blk * 512 + (t4 + 1) * 128, :].rearrange("h p d -> p h d"), in_=vblk[i][:, t4, :].rearrange("p (h d) -> p h d", d=128)), reads=[f"vblk{i}"], writes=["Vs"], dma=f"vw{i}")

                for h in range(8):
                    items.append(lambda h=h: kitem(h))
                for t4 in range(4):
                    for cb in range(2):
                        items.append(lambda t4=t4, cb=cb: vitem(t4, cb))
                return items

            for t4 in range(4):
                normA(0, t4)
            for blk in range(6):
                items = projA(blk)
                for t4 in range(4):
                    if blk + 1 < 6:
                        normA(blk + 1, t4)
                    for it in items[t4 * 4:(t4 + 1) * 4]:
                        it()
                ada_more(3)
            ada_more(24)
            FENCE()

            A.set(HT0)
            hT = A.a(16 * HTC, BF16).rearrange("p (k n) -> p k n", n=HTC)
            assert A.off <= OAT0
            A.set(OAT0)
            oAT = A.a(8 * NOWN, BF16).rearrange("p (h n) -> p h n", n=NOWN)
            oBT = A.a(8 * NOWN, BF16).rearrange("p (h n) -> p h n", n=NOWN)
            assert A.off <= PH0
            A.set(PH0)
            qT = A.a(8 * NOWN, BF16).rearrange("p (h n) -> p h n", n=NOWN)
            kTS = A.a(8 * 256, BF16).rearrange("p (h n) -> p h n", n=256)
            vS = A.a(4 * 1024, BF16).rearrange("p (t n) -> p t n", n=1024)
            mB1 = A.mark()
            NB = norm_bufs()
            for t4 in range(4):
                norm_tile(NB, 20 + t4, [(0, 128, 0)], lambda dc, p0, n, t4=t4: (hT[:, dc, NOWN + t4 * 128 + p0:NOWN + t4 * 128 + p0 + n], "hT"))
            for t in range(8):
                norm_tile(NB, 24 + t, [(0, 128, 0)], lambda dc, p0, n, t=t: (hT[:, dc, t * 128 + p0:t * 128 + p0 + n], "hT"))
            for t in range(2):
                norm_tile(NB, 32 + t, [(0, 64, 1 + 2 * t), (64, 64, 2 + 2 * t)], lambda dc, p0, n, t=t: (hT[:, dc, 1024 + t * 128 + p0:1024 + t * 128 + p0 + n], "hT"))

            FENCE("B1n")
            wcs = [A.a(16 * 512, BF16).rearrange("p (k n) -> p k n", n=512) for _ in range(2)]
            ostg = [A.a(512, F32) for _ in range(2)]
            osi = [0]
            ktown = [A.a(512, BF16) for _ in range(2)]
            kti = [0]
            vstg = [A.a(512, BF16) for _ in range(2)]
            vsi = [0]
            TMT = [(t * 128, 128) for t in range(8)] + [(1024 + s * 64, 64) for s in range(4)]
            wci = [0]

            def load_wc(cb, nbuf=2):
                i = wci[0] % nbuf
                wci[0] += 1
                buf = wcs[i]
                R.add("pool", lambda e: e.dma_start(out=buf, in_=w_in[:, cb * 512:(cb + 1) * 512].rearrange("(k p) n -> p k n", p=128)), writes=[f"wc{i}"], dma=f"wc{i}")
                return buf, f"wc{i}"

            def fm_proj(wc, wn, hh, c0, n):
                bi, bn = next_bank()
                for k in range(16):
                    R.add("pe", lambda e, k=k: e.matmul(banks[bi][:, 0:n], lhsT=wc[:, k, hh * 128:(hh + 1) * 128], rhs=hT[:, k, c0:c0 + n], start=(k == 0), stop=(k == 15)), reads=[wn, "hT"], writes=[bn])
                return bi, bn

            def tm_proj(wc, wn, c0, n):
                bi, bn = next_bank()
                for k in range(16):
                    R.add("pe", lambda e, k=k: e.matmul(banks[bi][0:n, :], lhsT=hT[:, k, c0:c0 + n], rhs=wc[:, k, :], start=(k == 0), stop=(k == 15)), reads=[wn, "hT"], writes=[bn])
                return bi, bn

            def tm_out(bi, bn, n, dst_ap):
                i = osi[0] % 2
                osi[0] += 1
                ob_ = ostg[i]
                R.add("act", lambda e: e.copy(out=ob_[0:n, :], in_=banks[bi][0:n, :]), reads=[bn], writes=[f"ostg{i}"])
                R.add("sp", lambda e: e.dma_start(out=dst_ap, in_=ob_[0:n, :]), reads=[f"ostg{i}"], dma=f"ostg{i}")

            for cb in range(6):
                ty = cb // 2
                half = cb % 2
                wc, wn = load_wc(cb)
                if ty in (0, 1):
                    for hh in range(4):
                        h = half * 4 + hh
                        for (c0, n) in TBS:
                            bi, bn = fm_proj(wc, wn, hh, c0, n)
                            if ty == 0:
                                R.add("act", lambda e, bi=bi, n=n, h=h, c0=c0: e.activation(out=qT[:, h, c0:c0 + n], in_=banks[bi][:, 0:n], func=AF.Copy, scale=SC), reads=[bn], writes=["qT"])
                            elif c0 < 1024:
                                i = kti[0] % 2
                                kti[0] += 1
                                R.add("act", lambda e, bi=bi, i=i: e.copy(out=ktown[i], in_=bk(bi)), reads=[bn], writes=[f"ktown{i}"])
                                R.add("sp", lambda e, i=i, h=h, c0=c0: e.dma_start(out=KTs[h, :, 3072 + c0:3072 + c0 + 512], in_=ktown[i]), reads=[f"ktown{i}"], writes=["KTs"], dma=f"ktown{i}")
                            else:
                                R.add("act", lambda e, bi=bi, h=h: e.copy(out=kTS[:, h, :], in_=banks[bi][:, 0:256]), reads=[bn], writes=["kTS"])
                if ty in (1, 2):
                    for (c0, n) in TMT:
                        bi, bn = tm_proj(wc, wn, c0, n)
                        dst = ksb_out if ty == 1 else vsb_out
                        tm_out(bi, bn, n, dst[c0:c0 + n, half * 512:(half + 1) * 512])
                        if ty == 2:
                            if c0 < 1024:
                                i = vsi[0] % 2
                                vsi[0] += 1
                                R.add("dve", lambda e, bi=bi, i=i: e.tensor_copy(out=vstg[i], in_=bk(bi)), reads=[bn], writes=[f"vstg{i}"])
                                R.add("sp", lambda e, i=i, c0=c0, half=half: e.dma_start(out=Vs[half * 4:(half + 1) * 4, 3072 + c0:3072 + c0 + 128, :].rearrange("h p d -> p h d"), in_=vstg[i].rearrange("p (h d) -> p h d", d=128)), reads=[f"vstg{i}"], writes=["Vs"], dma=f"vstg{i}")
                            else:
                                s = (c0 - 1024) // 64
                                R.add("dve", lambda e, bi=bi, s=s, half=half: e.tensor_copy(out=vS[0:64, s, half * 512:(half + 1) * 512], in_=banks[bi][0:64, :]), reads=[bn], writes=["vS"])
                FENCE("B1c%d" % cb)
            DUMP(R, "hT", hT.rearrange("p k n -> p (k n)"), 16 * HTC, BF16, "hT")
            DUMP(R, "qT", qT.rearrange("p h n -> p (h n)"), 8 * NOWN, BF16, "qT")
            DUMP(R, "kTS", kTS.rearrange("p h n -> p (h n)"), 8 * 256, BF16, "kTS")
            FENCE()
            A.release(mB1)

            ktH = [A.a(4096, BF16) for _ in range(2)]
            vH = [A.a(32 * 128, BF16).rearrange("p (t d) -> p t d", d=128) for _ in range(2)]
            kraw = [A.a(32 * 128, BF16).rearrange("p (t d) -> p t d", d=128) for _ in range(2)]
            e_sb = [A.a(512, F32) for _ in range(2)]
            sp_sb = [A.a(512, BF16) for _ in range(2)]
            a_sb = [A.a(512, BF16) for _ in range(2)]
            lsum = [A.a(128, BF16) for _ in range(2)]
            oslot = [0, 0]

            def sb_group(sid, q_ap, nq, tiles, first, last, kb_ap, diag, oslot_i, out_ap):
                zb, zn = banks[2 + sid], f"b{2 + sid}"
                tb, tn = banks[4 + sid], f"b{4 + sid}"
                ob = banks[6 + sid][:, oslot_i * 128:oslot_i * 128 + nq]
                on = f"b{6 + sid}"
                G = len(tiles)
                W = G * nq
                nk0 = tiles[0][4]
                en, spn, an, ln = f"e{sid}", f"sp{sid}", f"a{sid}", f"ls{sid}"

                def s0():
                    for i, (kT, kn, v, vn, nk) in enumerate(tiles):
                        R.add("pe", lambda e, i=i, kT=kT, nk=nk: e.matmul(zb[0:nk, i * nq:(i + 1) * nq], lhsT=kT, rhs=q_ap, start=True, stop=True), reads=[kn, "qT"], writes=[zn])

                def s1():
                    R.add("act", lambda e: e.activation(out=e_sb[sid][0:nk0, 0:W], in_=zb[0:nk0, 0:W], func=AF.Exp, bias=kb_ap[0:nk0, :]), reads=[zn, "kbias", "zeroc"], writes=[en])

                def s2():
                    R.add("act", lambda e: e.activation(out=sp_sb[sid][0:nk0, 0:W], in_=e_sb[sid][0:nk0, 0:W], func=AF.Ln, bias=onec[0:nk0, :]), reads=[en, "onec"], writes=[spn])
                    if diag:
                        R.add("dve", lambda e: e.tensor_tensor(out=sp_sb[sid][0:nk0, 0:nq], in0=sp_sb[sid][0:nk0, 0:nq], in1=dmask[0:nk0, 0:nq], op=ALU.mult), reads=[spn, "dmask"], writes=[spn])

                def s3():
                    for i, (kT, kn, v, vn, nk) in enumerate(tiles):
                        sl = slice(i * nq, (i + 1) * nq)
                        R.add("pe", lambda e, sl=sl, nk=nk: e.matmul(tb[0:nk, sl], lhsT=negtri[0:nk, 0:nk], rhs=sp_sb[sid][0:nk, sl], start=True, stop=False), reads=[spn, "negtri"], writes=[tn])
                        if not first:
                            R.add("pe", lambda e, sl=sl, nk=nk: e.matmul(tb[0:nk, sl], lhsT=negones[:, 0:nk], rhs=lsum[sid][:, 0:nq], start=False, stop=False), reads=[ln, "negones"], writes=[tn])
                        for i2 in range(i):
                            nk2 = tiles[i2][4]
                            R.add("pe", lambda e, sl=sl, nk=nk, i2=i2, nk2=nk2: e.matmul(tb[0:nk, sl], lhsT=negones[0:nk2, 0:nk], rhs=sp_sb[sid][0:nk2, i2 * nq:(i2 + 1) * nq], start=False, stop=False), reads=[spn, "negones"], writes=[tn])
                        R.add("pe", lambda e, sl=sl, nk=nk, kT=kT: e.matmul(tb[0:nk, sl], lhsT=kT, rhs=q_ap, start=False, stop=True), reads=[kn, "qT"], writes=[tn])
                    for i, (kT, kn, v, vn, nk) in enumerate(tiles):
                        if first and i == 0:
                            R.add("dve", lambda e: e.memset(lsum[sid], 0.0), writes=[ln])
                        R.add("dve", lambda e, i=i, nk=nk: e.tensor_tensor(out=lsum[sid][0:nk, 0:nq], in0=lsum[sid][0:nk, 0:nq], in1=sp_sb[sid][0:nk, i * nq:(i + 1) * nq], op=ALU.add), reads=[ln, spn], writes=[ln])

                def s4():
                    R.add("act", lambda e: e.activation(out=a_sb[sid][0:nk0, 0:W], in_=tb[0:nk0, 0:W], func=AF.Exp, bias=kb_ap[0:nk0, :]), reads=[tn, "kbias", "zeroc"], writes=[an])
                    if diag:
                        R.add("dve", lambda e: e.tensor_tensor(out=a_sb[sid][0:nk0, 0:nq], in0=a_sb[sid][0:nk0, 0:nq], in1=dmask[0:nk0, 0:nq], op=ALU.mult), reads=[an, "dmask"], writes=[an])

                def s5():
                    for i, (kT, kn, v, vn, nk) in enumerate(tiles):
                        R.add("pe", lambda e, i=i, v=v, nk=nk: e.matmul(ob, lhsT=v, rhs=a_sb[sid][0:nk, i * nq:(i + 1) * nq], start=(first and i == 0), stop=(last and i == G - 1)), reads=[vn, an], writes=[on])
                    if last:
                        R.add("dve", lambda e: e.tensor_copy(out=out_ap, in_=ob), reads=[on], writes=["oAT"])

                return [s0, s1, s2, s3, s4, s5]

            def interleave(streams):
                rows = list(zip_longest(*streams))

                def call(r, k):
                    if r is None:
                        return
                    for g in r:
                        if g is not None:
                            g[k]()

                if not rows:
                    return
                for k in range(3):
                    call(rows[0], k)
                for ri in range(len(rows)):
                    nxt = rows[ri + 1] if ri + 1 < len(rows) else None
                    call(rows[ri], 3)
                    call(nxt, 0)
                    call(rows[ri], 4)
                    call(nxt, 1)
                    call(nxt, 2)
                    call(rows[ri], 5)

            def chunks(lst, n):
                return [lst[i:i + n] for i in range(0, len(lst), n)]

            for hp in range(4):
                streams = []
                for sid in range(2):
                    h = hp * 2 + sid
                    R.add("sp", lambda e, sid=sid, h=h: e.dma_start(out=ktH[sid], in_=KTs[h]), reads=["KTs"], writes=[f"ktH{sid}"], dma=f"ktH{sid}")
                    R.add("sp", lambda e, sid=sid, h=h: e.dma_start(out=vH[sid], in_=Vs[h].rearrange("(t p) d -> p t d", p=128)), reads=["Vs"], writes=[f"vH{sid}"], dma=f"vH{sid}")
                    gl = []
                    for t in range(8):
                        q_ap = qT[:, h, t * 128:(t + 1) * 128]
                        tl = lambda j, sid=sid: (ktH[sid][:, j * 128:(j + 1) * 128], f"ktH{sid}", vH[sid][:, j, :], f"vH{sid}", 128)
                        groups = [([tl(24 + t)], zeroc, True)]
                        for ch in chunks(list(range(24 + t - 1, 23, -1)), 4):
                            groups.append(([tl(j) for j in ch], zeroc, False))
                        for ch in chunks(list(range(23, -1, -1)), 4):
                            groups.append(([tl(j) for j in ch], kbias[:, ch[0]:ch[0] + 1], False))
                        osl = oslot[sid] % 4
                        oslot[sid] += 1
                        for gi, (tiles, kb_ap, dg) in enumerate(groups):
                            gl.append(sb_group(sid, q_ap, 128, tiles, gi == 0, gi == len(groups) - 1, kb_ap, dg, osl, oAT[:, h, t * 128:(t + 1) * 128]))
                    streams.append(gl)
                interleave(streams)

            for s in range(4):
                for hp in range(4):
                    streams = []
                    for sid in range(2):
                        h = hp * 2 + sid
                        R.add("pool", lambda e, sid=sid, h=h, s=s: e.dma_start(out=kraw[sid], in_=csk[s, :, h * 128:(h + 1) * 128].rearrange("(t p) d -> p t d", p=128)), writes=[f"kraw{sid}"], dma=f"kraw{sid}")
                    for sid in range(2):
                        h = hp * 2 + sid
                        R.add("pool", lambda e, sid=sid, h=h, s=s: e.dma_start(out=vH[sid], in_=csv[s, :, h * 128:(h + 1) * 128].rearrange("(t p) d -> p t d", p=128)), writes=[f"vH{sid}"], dma=f"vH{sid}")
                    for sid in range(2):
                        h = hp * 2 + sid
                        for t8 in range(4):
                            pb = banks[sid][:, 0:512].bitcast(BF16)
                            for j in range(8):
                                R.add("pe", lambda e, pb=pb, j=j, t8=t8, sid=sid: e.transpose(out=pb[:, j * 128:(j + 1) * 128], in_=kraw[sid][:, t8 * 8 + j, :], identity=identb), reads=[f"kraw{sid}", "identb"], writes=[f"b{sid}"])
                            R.add("dve", lambda e, pb=pb, t8=t8, sid=sid: e.tensor_copy(out=ktH[sid][:, t8 * 1024:(t8 + 1) * 1024], in_=pb), reads=[f"b{sid}"], writes=[f"ktH{sid}"])
                        q_ap = qT[:, h, 1024 + s * 64:1024 + (s + 1) * 64]
                        tl = lambda j, sid=sid: (ktH[sid][:, j * 128:(j + 1) * 128], f"ktH{sid}", vH[sid][:, j, :], f"vH{sid}", 128)
                        groups = [([(kTS[:, h, s * 64:(s + 1) * 64], "kTS", vS[0:64, s, h * 128:(h + 1) * 128], "vS", 64)], True)]
                        for ch in chunks(list(range(31, -1, -1)), 4):
                            groups.append(([tl(j) for j in ch], False))
                        osl = oslot[sid] % 4
                        oslot[sid] += 1
                        gl = []
                        for gi, (tiles, dg) in enumerate(groups):
                            gl.append(sb_group(sid, q_ap, 64, tiles, gi == 0, gi == len(groups) - 1, zeroc, dg, osl, oAT[:, h, 1024 + s * 64:1024 + (s + 1) * 64]))
                        streams.append(gl)
                    interleave(streams)
            DUMP(R, "oAT", oAT.rearrange("p h n -> p (h n)"), 8 * NOWN, BF16, "oAT")
            FENCE()

            A.set(PH0)
            bt = A.a(32 * 128, F32).rearrange("p (t q) -> p t q", q=128)
            for h in range(8):
                for j in range(3):
                    R.add("sp", lambda e, h=h, j=j: e.dma_start(out=bt[:, h * 4 + j, :], in_=btile[:, h * 3 + j, :]), writes=["bt"], dma="bt")
            for h in range(8):
                R.add("dve", lambda e, h=h: e.tensor_copy(out=bt[:, h * 4 + 3, :], in_=bt[:, h * 4 + 2, :]), reads=["bt"], writes=["bt"])
                R.add("pool", lambda e, h=h: e.memset(bt[0:64, h * 4 + 3, 64:128], NEG), reads=["bt"], writes=["bt"])
                R.add("pool", lambda e, h=h: e.memset(bt[64:128, h * 4 + 0, 0:64], NEG), reads=["bt"], writes=["bt"])
            qbT = A.a(4 * NOWN, BF16).rearrange("p (h n) -> p h n", n=NOWN)
            kbT = A.a(4 * HTC, BF16).rearrange("p (h n) -> p h n", n=HTC)
            vbP = A.a(12 * 512, BF16).rearrange("p (t n) -> p t n", n=512)
            vbS = A.a(4 * 512, BF16).rearrange("p (t n) -> p t n", n=512)
            wcs = [A.a(16 * 512, BF16).rearrange("p (k n) -> p k n", n=512)]
            ostg = [A.a(512, F32) for _ in range(2)]
            sqb = A.a(512, BF16)
            rs1 = A.a(512, F32)
            rs2 = A.a(512, F32)
            kn32 = A.a(512, F32)
            sB = [A.a(640, F32) for _ in range(2)]
            pB = [A.a(640, BF16) for _ in range(2)]
            rden = [A.a(128, F32) for _ in range(2)]
            kbc = [A.a(4 * 128, BF16).rearrange("p (t d) -> p t d", d=128) for _ in range(2)]
            vbc = [A.a(4 * 128, BF16).rearrange("p (t d) -> p t d", d=128) for _ in range(2)]
            kbcT = [A.a(512, BF16) for _ in range(2)]

            def band_norm(bi, bn, n, gain_ap, dst_bf, dn, want32):
                R.add("act", lambda e: e.activation(out=sqb[:, 0:n], in_=banks[bi][:, 0:n], func=AF.Square), reads=[bn], writes=["sqb"])
                R.add("pe", lambda e: e.matmul(banks[5][:, 0:n], lhsT=onesb, rhs=sqb[:, 0:n], start=True, stop=True), reads=["sqb", "onesb"], writes=["b5"])
                R.add("act", lambda e: e.activation(out=rs1[:, 0:n], in_=banks[5][:, 0:n], func=AF.Ln, scale=1.0 / 128, bias=epsc), reads=["b5", "epsc"], writes=["rs1"])
                R.add("act", lambda e: e.activation(out=rs2[:, 0:n], in_=rs1[:, 0:n], func=AF.Exp, scale=-0.5), reads=["rs1"], writes=["rs2"])
                if not want32:
                    R.add("dve", lambda e: e.scalar_tensor_tensor(out=dst_bf, in0=banks[bi][:, 0:n], scalar=gain_ap, in1=rs2[:, 0:n], op0=ALU.mult, op1=ALU.mult), reads=[bn, "rs2", "cparam"], writes=[dn])
                else:
                    R.add("dve", lambda e: e.scalar_tensor_tensor(out=kn32[:, 0:n], in0=banks[bi][:, 0:n], scalar=gain_ap, in1=rs2[:, 0:n], op0=ALU.mult, op1=ALU.mult), reads=[bn, "rs2", "cparam"], writes=["kn32"])
                    R.add("act", lambda e: e.copy(out=dst_bf, in_=kn32[:, 0:n]), reads=["kn32"], writes=[dn])

            def band_unit(sid, q_ap, nq, tiles, out_ap):
                z0, zn0 = banks[2 + sid], f"b{2 + sid}"
                dnb, dnn = banks[4 + sid], f"b{4 + sid}"
                ob, on = banks[6 + sid], f"b{6 + sid}"
                G = len(tiles)
                sn, pn, rn = f"sB{sid}", f"pB{sid}", f"rden{sid}"
                zb2, zn2 = banks[sid], f"b{sid}"

                def zslice(i, nk):
                    if i < 4:
                        return z0[0:nk, i * nq:(i + 1) * nq], zn0
                    return zb2[0:nk, 0:nq], zn2

                def s0():
                    for i, (kT, kn, v, vn, nk, b_ap, kb_ap) in enumerate(tiles):
                        zs, zn = zslice(i, nk)
                        R.add("pe", lambda e, zs=zs, kT=kT: e.matmul(zs, lhsT=kT, rhs=q_ap, start=True, stop=True), reads=[kn, "qbT"], writes=[zn])

                def s1():
                    for i, (kT, kn, v, vn, nk, b_ap, kb_ap) in enumerate(tiles):
                        zs, zn = zslice(i, nk)
                        R.add("dve", lambda e, zs=zs, i=i, nk=nk, b_ap=b_ap: e.tensor_tensor(out=sB[sid][0:nk, i * nq:(i + 1) * nq], in0=zs, in1=b_ap, op=ALU.add), reads=[zn, "bt"], writes=[sn])

                def s2():
                    for i, (kT, kn, v, vn, nk, b_ap, kb_ap) in enumerate(tiles):
                        R.add("act", lambda e, i=i, nk=nk, kb_ap=kb_ap: e.activation(out=pB[sid][0:nk, i * nq:(i + 1) * nq], in_=sB[sid][0:nk, i * nq:(i + 1) * nq], func=AF.Exp, bias=kb_ap), reads=[sn, "kbias", "zeroc"], writes=[pn])

                def s3():
                    for i, (kT, kn, v, vn, nk, b_ap, kb_ap) in enumerate(tiles):
                        R.add("pe", lambda e, i=i, nk=nk: e.matmul(dnb[:, 0:nq], lhsT=onesb[0:nk, :], rhs=pB[sid][0:nk, i * nq:(i + 1) * nq], start=(i == 0), stop=(i == G - 1)), reads=[pn, "onesb"], writes=[dnn])
                    for i, (kT, kn, v, vn, nk, b_ap, kb_ap) in enumerate(tiles):
                        R.add("pe", lambda e, i=i, nk=nk, v=v: e.matmul(ob[:, 0:nq], lhsT=v, rhs=pB[sid][0:nk, i * nq:(i + 1) * nq], start=(i == 0), stop=(i == G - 1)), reads=[pn, vn], writes=[on])

                def s4():
                    R.add("dve", lambda e: e.reciprocal(out=rden[sid][:, 0:nq], in_=dnb[:, 0:nq]), reads=[dnn], writes=[rn])

                def s5():
                    R.add("dve", lambda e: e.tensor_tensor(out=out_ap, in0=ob[:, 0:nq], in1=rden[sid][:, 0:nq], op=ALU.mult), reads=[on, rn], writes=["oBT"])

                return [s0, s1, s2, s3, s4, s5]

            FMB = TBS + [(NOWN, 512)]
            for half in range(2):
                for ty in (3, 4, 5):
                    cb = ty * 2 + half
                    wc, wn = load_wc(cb, nbuf=1)
                    if ty in (3, 4):
                        for hh in range(4):
                            h = half * 4 + hh
                            for (c0, n) in (TBS if ty == 3 else FMB):
                                bi, bn = fm_proj(wc, wn, hh, c0, n)
                                if ty == 3:
                                    band_norm(bi, bn, n, qgs[:, h:h + 1], qbT[:, hh, c0:c0 + n], "qbT", False)
                                else:
                                    own = c0 < NOWN
                                    band_norm(bi, bn, n, kg[:, h:h + 1], kbT[:, hh, c0:c0 + n], "kbT", own)
                                    if own:
                                        for j in range(n // 128):
                                            R.add("pe", lambda e, j=j: e.transpose(out=banks[7][:, j * 128:(j + 1) * 128], in_=kn32[:, j * 128:(j + 1) * 128], identity=identf), reads=["kn32", "identf"], writes=["b7"])
                                        i = osi[0] % 2
                                        osi[0] += 1
                                        ob_ = ostg[i]
                                        R.add("act", lambda e, ob_=ob_, n=n: e.copy(out=ob_[:, 0:n], in_=banks[7][:, 0:n]), reads=["b7"], writes=[f"ostg{i}"])
                                        R.add("sp", lambda e, ob_=ob_, n=n, c0=c0, h=h: e.dma_start(out=kbd_out[c0:c0 + n, h * 128:(h + 1) * 128].rearrange("(j p) d -> p j d", p=128), in_=ob_[:, 0:n].rearrange("p (j d) -> p j d", d=128)), reads=[f"ostg{i}"], dma=f"ostg{i}")
                    else:
                        for (c0, n) in TMT + [(NOWN + t4 * 128, 128) for t4 in range(4)]:
                            bi, bn = tm_proj(wc, wn, c0, n)
                            if c0 < NOWN:
                                tm_out(bi, bn, n, vbd_out[c0:c0 + n, half * 512:(half + 1) * 512])
                            if c0 < 1024:
                                t = c0 // 128
                                R.add("dve", lambda e, bi=bi, t=t: e.tensor_copy(out=vbP[:, 4 + t, :], in_=bk(bi)), reads=[bn], writes=["vbP"])
                            elif c0 < NOWN:
                                s = (c0 - 1024) // 64
                                R.add("dve", lambda e, bi=bi, s=s: e.tensor_copy(out=vbS[0:64, s, :], in_=banks[bi][0:64, :]), reads=[bn], writes=["vbS"])
                            else:
                                t4 = (c0 - NOWN) // 128
                                R.add("dve", lambda e, bi=bi, t4=t4: e.tensor_copy(out=vbP[:, t4, :], in_=bk(bi)), reads=[bn], writes=["vbP"])

                def kcol(kt):
                    return NOWN + kt * 128 if kt < 4 else (kt - 4) * 128

                for hp in range(2):
                    streams = []
                    for sid in range(2):
                        hh = hp * 2 + sid
                        h = half * 4 + hh
                        gl = []
                        for t in range(8):
                            tiles = []
                            for j in range(5):
                                kt = 4 + t - j
                                kb_ap = kbias[:, 20 + kt:21 + kt] if kt < 4 else zeroc
                                bidx = h * 4 + (3 if j == 4 else min(j, 2))
                                tiles.append((kbT[:, hh, kcol(kt):kcol(kt) + 128], "kbT", vbP[:, kt, hh * 128:(hh + 1) * 128], "vbP", 128, bt[:, bidx, :], kb_ap))
                            gl.append(band_unit(sid, qbT[:, hh, t * 128:(t + 1) * 128], 128, tiles, oBT[:, h, t * 128:(t + 1) * 128]))
                        streams.append(gl)
                    interleave(streams)
                for s in range(4):
                    for hp in range(2):
                        streams = []
                        for sid in range(2):
                            hh = hp * 2 + sid
                            h = half * 4 + hh
                            R.add("pool", lambda e, sid=sid, h=h, s=s: e.dma_start(out=kbc[sid], in_=cbk[s, :, h * 128:(h + 1) * 128].rearrange("(t p) d -> p t d", p=128)), writes=[f"kbc{sid}"], dma=f"kbc{sid}")
                            R.add("pool", lambda e, sid=sid, h=h, s=s: e.dma_start(out=vbc[sid], in_=cbv[s, :, h * 128:(h + 1) * 128].rearrange("(t p) d -> p t d", p=128)), writes=[f"vbc{sid}"], dma=f"vbc{sid}")
                            pb = banks[sid][:, 0:256].bitcast(BF16)
                            for j in range(4):
                                R.add("pe", lambda e, pb=pb, j=j, sid=sid: e.transpose(out=pb[:, j * 128:(j + 1) * 128], in_=kbc[sid][:, j, :], identity=identb), reads=[f"kbc{sid}", "identb"], writes=[f"b{sid}"])
                            R.add("act", lambda e, pb=pb, sid=sid: e.copy(out=kbcT[sid], in_=pb), reads=[f"b{sid}"], writes=[f"kbcT{sid}"])
                            c0 = 1024 + s * 64
                            tiles = [(kbT[:, hh, c0:c0 + 64], "kbT", vbS[0:64, s, hh * 128:(hh + 1) * 128], "vbS", 64, bt[0:64, h * 4 + 0, 0:64], zeroc[0:64, :])]
                            for m in range(3, -1, -1):
                                jj = 1 if m == 3 else 2
                                tiles.append((kbcT[sid][:, m * 128:(m + 1) * 128], f"kbcT{sid}", vbc[sid][:, m, :], f"vbc{sid}", 128, bt[:, h * 4 + jj, 0:64], zeroc))
                            streams.append([band_unit(sid, qbT[:, hh, c0:c0 + 64], 64, tiles, oBT[:, h, c0:c0 + 64])])
                        interleave(streams)
            DUMP(R, "oBT", oBT.rearrange("p h n -> p (h n)"), 8 * NOWN, BF16, "oBT")
            FENCE()

            A.set(PH0)
            mgT = A.a(16 * NOWN, BF16).rearrange("p (k n) -> p k n", n=NOWN)
            wg_ = [A.a(16 * 128, BF16).rearrange("p (k n) -> p k n", n=128) for _ in range(2)]
            wgb_ = [A.a(16 * 128, BF16).rearrange("p (k n) -> p k n", n=128) for _ in range(2)]
            wpa_ = [A.a(8 * 128, BF16).rearrange("p (k n) -> p k n", n=128) for _ in range(2)]
            wpb_ = [A.a(8 * 128, BF16).rearrange("p (k n) -> p k n", n=128) for _ in range(2)]
            sga = A.a(512, F32)
            sgb = A.a(512, F32)
            m1 = A.a(512, F32)
            m2 = A.a(512, F32)
            for fc in range(16):
                i = fc % 2
                a0, a1 = fc * 128, (fc + 1) * 128
                R.add("pool", lambda e, i=i, a0=a0, a1=a1: e.dma_start(out=wg_[i], in_=w_in[:, 6144 + a0:6144 + a1].rearrange("(k p) n -> p k n", p=128)), writes=[f"wg{i}"], dma=f"wg{i}")
                R.add("pool", lambda e, i=i, a0=a0, a1=a1: e.dma_start(out=wgb_[i], in_=w_in[:, 8192 + a0:8192 + a1].rearrange("(k p) n -> p k n", p=128)), writes=[f"wgb{i}"], dma=f"wgb{i}")
                R.add("pool", lambda e, i=i, a0=a0, a1=a1: e.dma_start(out=wpa_[i], in_=w_psb[:, a0:a1].rearrange("(k p) n -> p k n", p=128)), writes=[f"wpa{i}"], dma=f"wpa{i}")
                R.add("pool", lambda e, i=i, a0=a0, a1=a1: e.dma_start(out=wpb_[i], in_=w_pbd[:, a0:a1].rearrange("(k p) n -> p k n", p=128)), writes=[f"wpb{i}"], dma=f"wpb{i}")
                for (c0, n) in TBS:
                    for k in range(16):
                        R.add("pe", lambda e, i=i, k=k, c0=c0, n=n: e.matmul(banks[0][:, 0:n], lhsT=wg_[i][:, k, :], rhs=hT[:, k, c0:c0 + n], start=(k == 0), stop=(k == 15)), reads=[f"wg{i}", "hT"], writes=["b0"])
                    for k in range(16):
                        R.add("pe", lambda e, i=i, k=k, c0=c0, n=n: e.matmul(banks[1][:, 0:n], lhsT=wgb_[i][:, k, :], rhs=hT[:, k, c0:c0 + n], start=(k == 0), stop=(k == 15)), reads=[f"wgb{i}", "hT"], writes=["b1"])
                    for k in range(8):
                        R.add("pe", lambda e, i=i, k=k, c0=c0, n=n: e.matmul(banks[2][:, 0:n], lhsT=wpa_[i][:, k, :], rhs=oAT[:, k, c0:c0 + n], start=(k == 0), stop=(k == 7)), reads=[f"wpa{i}", "oAT"], writes=["b2"])
                    for k in range(8):
                        R.add("pe", lambda e, i=i, k=k, c0=c0, n=n: e.matmul(banks[3][:, 0:n], lhsT=wpb_[i][:, k, :], rhs=oBT[:, k, c0:c0 + n], start=(k == 0), stop=(k == 7)), reads=[f"wpb{i}", "oBT"], writes=["b3"])
                    R.add("act", lambda e, n=n: e.activation(out=sga[:, 0:n], in_=banks[0][:, 0:n], func=AF.Sigmoid), reads=["b0"], writes=["sga"])
                    R.add("act", lambda e, n=n: e.activation(out=sgb[:, 0:n], in_=banks[1][:, 0:n], func=AF.Sigmoid), reads=["b1"], writes=["sgb"])
                    R.add("dve", lambda e, n=n: e.tensor_tensor(out=m1[:, 0:n], in0=banks[2][:, 0:n], in1=sga[:, 0:n], op=ALU.mult), reads=["b2", "sga"], writes=["m1"])
                    R.add("dve", lambda e, n=n: e.tensor_tensor(out=m2[:, 0:n], in0=banks[3][:, 0:n], in1=sgb[:, 0:n], op=ALU.mult), reads=["b3", "sgb"], writes=["m2"])
                    R.add("dve", lambda e, n=n, fc=fc, c0=c0: e.tensor_tensor(out=mgT[:, fc, c0:c0 + n], in0=m1[:, 0:n], in1=m2[:, 0:n], op=ALU.add), reads=["m1", "m2"], writes=["mgT"])
            DUMP(R, "mgT", mgT.rearrange("p k n -> p (k n)"), 16 * NOWN, BF16, "mgT")
            FENCE()

            A.set(HT0)
            yacc = A.a(16 * NOWN, F32).rearrange("p (k n) -> p k n", n=NOWN)
            h2T = A.a(16 * NOWN, BF16).rearrange("p (k n) -> p k n", n=NOWN)
            A.set(PH0 + 40 * KB)
            xts = [A.a(D, F32) for _ in range(2)]
            wo = [A.a(16 * 128, BF16).rearrange("p (k n) -> p k n", n=128) for _ in range(2)]
            for t in range(10):
                i = t % 2
                R.add("sp", lambda e, i=i, t=t: e.dma_start(out=xts[i], in_=x_tok[(24 + t) * 128:(25 + t) * 128, :]), writes=[f"xx{i}"], dma=f"xx{i}")
                for q4 in range(4):
                    bi, bn = next_bank()
                    for j in range(4):
                        dc = q4 * 4 + j
                        R.add("pe", lambda e, bi=bi, j=j, dc=dc, i=i: e.transpose(out=banks[bi][:, j * 128:(j + 1) * 128], in_=xts[i][:, dc * 128:(dc + 1) * 128], identity=identf), reads=[f"xx{i}", "identf"], writes=[bn])
                    R.add("act", lambda e, bi=bi, q4=q4, t=t: e.copy(out=yacc[:, q4 * 4:(q4 + 1) * 4, t * 128:(t + 1) * 128], in_=banks[bi][:, :].rearrange("p (j n) -> p j n", n=128)), reads=[bn], writes=["yacc"])
            SEG = [(0, 512, 0), (512, 512, 0)] + [(1024 + s * 64, 64, 1 + s) for s in range(4)]
            for dc in range(16):
                i = dc % 2
                R.add("pool", lambda e, i=i, dc=dc: e.dma_start(out=wo[i], in_=w_out[:, dc * 128:(dc + 1) * 128].rearrange("(k p) n -> p k n", p=128)), writes=[f"wo{i}"], dma=f"wo{i}")
                for (c0, n) in TBS:
                    bi, bn = next_bank()
                    for k in range(16):
                        R.add("pe", lambda e, bi=bi, i=i, k=k, c0=c0, n=n: e.matmul(banks[bi][:, 0:n], lhsT=wo[i][:, k, :], rhs=mgT[:, k, c0:c0 + n], start=(k == 0), stop=(k == 15)), reads=[f"wo{i}", "mgT"], writes=[bn])
                    for (s0_, sn_, seq) in SEG:
                        if s0_ < c0 or s0_ >= c0 + n:
                            continue
                        R.add("dve", lambda e, bi=bi, dc=dc, s0_=s0_, sn_=sn_, seq=seq, c0=c0: e.scalar_tensor_tensor(out=yacc[:, dc, s0_:s0_ + sn_], in0=banks[bi][:, s0_ - c0:s0_ - c0 + sn_], scalar=modT[:, 32 + dc, seq:seq + 1], in1=yacc[:, dc, s0_:s0_ + sn_], op0=ALU.mult, op1=ALU.add), reads=[bn, "modT2", "yacc"], writes=["yacc"])
            DUMP(R, "yacc", yacc.rearrange("p k n -> p (k n)"), 16 * NOWN, F32, "yacc")
            FENCE()

            A.set(128 * KB)
            combT = A.a(NOWN, BF16)
            selb = A.a(32 * 128, BF16).rearrange("p (e m) -> p e m", m=128)
            cbc = [A.a(NOWN, BF16) for _ in range(2)]
            mMoE = A.mark()
            sq2 = A.a(512, BF16)
            r1 = A.a(512, F32)
            r2 = A.a(512, F32)
            t32 = A.a(512, F32)
            wr = A.a(16 * 36, BF16).rearrange("p (k n) -> p k n", n=36)
            brt = A.a(36, F32)
            lg = A.a(36, F32)
            wk = A.a(64, F32)
            comb = A.a(32, F32)
            R.add("pool", lambda e: e.dma_start(out=wr, in_=w_rt.rearrange("(k p) n -> p k n", p=128)), writes=["wr"], dma="wr")
            R.add("sp", lambda e: e.dma_start(out=brt, in_=b_rt.partition_broadcast(128)), writes=["brt"], dma="brt")
            R.add("pool", lambda e: e.memset(selb[0:32], 1.0), writes=["selb"])
            R.add("pool", lambda e: e.affine_select(out=selb[0:32], in_=selb[0:32], pattern=[[-1, 32], [0, 128]], compare_op=ALU.is_equal, fill=0.0, base=0, channel_multiplier=1), reads=["selb"], writes=["selb"])
            for (c0, n) in TBS:
                for dc in range(16):
                    R.add("act", lambda e, dc=dc, c0=c0, n=n: e.activation(out=sq2[:, 0:n], in_=yacc[:, dc, c0:c0 + n], func=AF.Square), reads=["yacc"], writes=["sq2"])
                    R.add("pe", lambda e, dc=dc, n=n: e.matmul(banks[2][:, 0:n], lhsT=onesb, rhs=sq2[:, 0:n], start=(dc == 0), stop=(dc == 15)), reads=["sq2", "onesb"], writes=["b2"])
                R.add("act", lambda e, n=n: e.activation(out=r1[:, 0:n], in_=banks[2][:, 0:n], func=AF.Ln, scale=1.0 / D, bias=epsc), reads=["b2", "epsc"], writes=["r1"])
                R.add("act", lambda e, n=n: e.activation(out=r2[:, 0:n], in_=r1[:, 0:n], func=AF.Exp, scale=-0.5), reads=["r1"], writes=["r2"])
                for dc in range(16):
                    R.add("dve", lambda e, dc=dc, c0=c0, n=n: e.tensor_tensor(out=t32[:, 0:n], in0=yacc[:, dc, c0:c0 + n], in1=r2[:, 0:n], op=ALU.mult), reads=["yacc", "r2"], writes=["t32"])
                    for (s0_, sn_, seq) in SEG:
                        if s0_ < c0 or s0_ >= c0 + n:
                            continue
                        R.add("dve", lambda e, dc=dc, s0_=s0_, sn_=sn_, seq=seq, c0=c0: e.tensor_scalar(out=h2T[:, dc, s0_:s0_ + sn_], in0=t32[:, s0_ - c0:s0_ - c0 + sn_], scalar1=gmf[:, dc, seq:seq + 1], scalar2=modT[:, 48 + dc, seq:seq + 1], op0=ALU.mult, op1=ALU.add), reads=["t32", "gmf", "modT2"], writes=["h2T"])
            AXX = mybir.AxisListType.X
            for t in range(10):
                for k in range(16):
                    R.add("pe", lambda e, k=k, t=t: e.matmul(banks[3][:, 0:36], lhsT=h2T[:, k, t * 128:(t + 1) * 128], rhs=wr[:, k, :], start=(k == 0), stop=(k == 15)), reads=["h2T", "wr"], writes=["b3"])
                V = lambda a, b: wk[:, a:b]
                ops = []
                ops.append(lambda e: e.tensor_tensor(out=lg, in0=banks[3][:, 0:36], in1=brt, op=ALU.add))
                ops.append(lambda e: e.reduce_max(out=V(0, 1), in_=lg[:, 0:4], axis=AXX))
                ops.append(lambda e: e.tensor_scalar(out=V(4, 8), in0=lg[:, 0:4], scalar1=V(0, 1), scalar2=None, op0=ALU.is_ge))
                ops.append(lambda e: e.tensor_scalar(out=V(8, 12), in0=lg[:, 0:4], scalar1=V(0, 1), scalar2=None, op0=ALU.subtract))
                for o in ops:
                    R.add("dve", o, reads=["b3", "brt", "lg", "wk"], writes=["lg", "wk"])
                R.add("dve", lambda e: e.memset(V(1, 2), 0.0), reads=["wk"], writes=["wk"])
                R.add("act", lambda e: e.activation(out=V(8, 12), in_=V(8, 12), func=AF.Exp, accum_out=V(1, 2)), reads=["wk"], writes=["wk"])
                ops = []
                ops.append(lambda e: e.reciprocal(out=V(2, 3), in_=V(1, 2)))
                ops.append(lambda e: e.tensor_scalar(out=V(16, 24), in0=lg[:, 4:12], scalar1=V(4, 5), scalar2=None, op0=ALU.mult))
                for g in range(1, 4):
                    ops.append(lambda e, g=g: e.scalar_tensor_tensor(out=V(16, 24), in0=lg[:, 4 + 8 * g:12 + 8 * g], scalar=V(4 + g, 5 + g), in1=V(16, 24), op0=ALU.mult, op1=ALU.add))
                ops.append(lambda e: e.reduce_max(out=V(3, 4), in_=V(16, 24), axis=AXX))
                ops.append(lambda e: e.tensor_scalar(out=V(24, 32), in0=V(16, 24), scalar1=V(3, 4), scalar2=None, op0=ALU.is_ge))
                ops.append(lambda e: e.scalar_tensor_tensor(out=V(32, 40), in0=V(24, 32), scalar=-1e30, in1=V(16, 24), op0=ALU.mult, op1=ALU.add))
                ops.append(lambda e: e.reduce_max(out=V(12, 13), in_=V(32, 40), axis=AXX))
                ops.append(lambda e: e.tensor_scalar(out=V(40, 48), in0=V(32, 40), scalar1=V(12, 13), scalar2=None, op0=ALU.is_ge))
                ops.append(lambda e: e.tensor_tensor(out=V(13, 14), in0=V(12, 13), in1=V(3, 4), op=ALU.subtract))
                for o in ops:
                    R.add("dve", o, reads=["wk", "lg"], writes=["wk"])
                R.add("act", lambda e: e.activation(out=V(13, 14), in_=V(13, 14), func=AF.Exp), reads=["wk"], writes=["wk"])
                ops = []
                ops.append(lambda e: e.tensor_scalar(out=V(14, 15), in0=V(13, 14), scalar1=1.0, scalar2=None, op0=ALU.add))
                ops.append(lambda e: e.reciprocal(out=V(14, 15), in_=V(14, 15)))
                ops.append(lambda e: e.tensor_tensor(out=V(15, 16), in0=V(13, 14), in1=V(14, 15), op=ALU.mult))
                ops.append(lambda e: e.tensor_scalar(out=V(48, 56), in0=V(24, 32), scalar1=V(14, 15), scalar2=None, op0=ALU.mult))
                ops.append(lambda e: e.scalar_tensor_tensor(out=V(48, 56), in0=V(40, 48), scalar=V(15, 16), in1=V(48, 56), op0=ALU.mult, op1=ALU.add))
                ops.append(lambda e: e.tensor_scalar(out=V(48, 56), in0=V(48, 56), scalar1=V(2, 3), scalar2=None, op0=ALU.mult))
                for g in range(4):
                    ops.append(lambda e, g=g: e.tensor_scalar(out=comb[:, g * 8:(g + 1) * 8], in0=V(48, 56), scalar1=V(4 + g, 5 + g), scalar2=None, op0=ALU.mult))
                for o in ops:
                    R.add("dve", o, reads=["wk", "comb"], writes=["wk", "comb"])
                R.add("pe", lambda e: e.transpose(out=banks[3][0:32, 128:256], in_=comb, identity=identf), reads=["comb", "identf"], writes=["b3"])
                R.add("act", lambda e, t=t: e.copy(out=combT[0:32, t * 128:(t + 1) * 128], in_=banks[3][0:32, 128:256]), reads=["b3"], writes=["combT"])
            DUMP(R, "h2T", h2T.rearrange("p k n -> p (k n)"), 16 * NOWN, BF16, "h2T")
            DUMP(R, "combT", combT, NOWN, BF16, "combT")
            FENCE(soft=True)

            wgr = [A.a(16 * 128, BF16).rearrange("p (k n) -> p k n", n=128) for _ in range(3)]
            wur = [A.a(16 * 128, BF16).rearrange("p (k n) -> p k n", n=128) for _ in range(3)]
            wdr = [A.a(4 * 2048, BF16).rearrange("p (k n) -> p k n", n=2048) for _ in range(1)]
            hid = A.a(4 * NOWN, BF16).rearrange("p (k n) -> p k n", n=NOWN)
            sgt = [A.a(512, BF16) for _ in range(2)]
            ui = [0]
            for ex in range(32):
                ci = ex % 2
                di = 0
                for (c0, n) in TBS:
                    R.add("pe", lambda e, ex=ex, c0=c0, n=n: e.matmul(banks[6][:, 0:n], lhsT=selb[0:32, ex, :], rhs=combT[0:32, c0:c0 + n], start=True, stop=True), reads=["selb", "combT"], writes=["b6"])
                    R.add("act", lambda e, ci=ci, c0=c0, n=n: e.copy(out=cbc[ci][:, c0:c0 + n], in_=banks[6][:, 0:n]), reads=["b6"], writes=[f"cbc{ci}"])
                for fc in range(4):
                    u = ui[0] % 3
                    ui[0] += 1
                    R.add("pool", lambda e, ex=ex, fc=fc, u=u: e.dma_start(out=wgr[u], in_=w_gate[ex][:, fc * 128:(fc + 1) * 128].rearrange("(k p) n -> p k n", p=128)), writes=[f"wgr{u}"], dma=f"wgr{u}")
                    R.add("pool", lambda e, ex=ex, fc=fc, u=u: e.dma_start(out=wur[u], in_=w_up[ex][:, fc * 128:(fc + 1) * 128].rearrange("(k p) n -> p k n", p=128)), writes=[f"wur{u}"], dma=f"wur{u}")
                    if fc == 1:
                        R.add("pool", lambda e, ex=ex, di=di: e.dma_start(out=wdr[di], in_=w_down[ex].rearrange("(k p) n -> p k n", p=128)), writes=[f"wd{di}"], dma=f"wd{di}")
                    for bi_, (c0, n) in enumerate(TBS):
                        pg = (bi_ + fc) % 2
                        gb, ub = banks[pg * 2], banks[pg * 2 + 1]
                        gn, un = f"b{pg * 2}", f"b{pg * 2 + 1}"
                        for k in range(16):
                            R.add("pe", lambda e, k=k, u=u, c0=c0, n=n, gb=gb: e.matmul(gb[:, 0:n], lhsT=wgr[u][:, k, :], rhs=h2T[:, k, c0:c0 + n], start=(k == 0), stop=(k == 15)), reads=[f"wgr{u}", "h2T"], writes=[gn])
                        for k in range(16):
                            R.add("pe", lambda e, k=k, u=u, c0=c0, n=n, ub=ub: e.matmul(ub[:, 0:n], lhsT=wur[u][:, k, :], rhs=h2T[:, k, c0:c0 + n], start=(k == 0), stop=(k == 15)), reads=[f"wur{u}", "h2T"], writes=[un])
                        R.add("act", lambda e, pg=pg, n=n, gb=gb: e.activation(out=sgt[pg][:, 0:n], in_=gb[:, 0:n], func=AF.Silu), reads=[gn], writes=[f"sgt{pg}"])
                        R.add("dve", lambda e, pg=pg, n=n, c0=c0, ci=ci: e.tensor_tensor(out=sgt[pg][:, 0:n], in0=sgt[pg][:, 0:n], in1=cbc[ci][:, c0:c0 + n], op=ALU.mult), reads=[f"sgt{pg}", f"cbc{ci}"], writes=[f"sgt{pg}"])
                        R.add("dve", lambda e, pg=pg, n=n, c0=c0, fc=fc, ub=ub: e.tensor_tensor(out=hid[:, fc, c0:c0 + n], in0=ub[:, 0:n], in1=sgt[pg][:, 0:n], op=ALU.mult), reads=[un, f"sgt{pg}"], writes=["hid"])
                for dc in range(16):
                    for bi_, (c0, n) in enumerate(TBS):
                        yb_i = (4, 5, 7)[bi_]
                        yb, yn = banks[yb_i], f"b{yb_i}"
                        for fc in range(4):
                            R.add("pe", lambda e, fc=fc, dc=dc, c0=c0, n=n, yb=yb, di=di: e.matmul(yb[:, 0:n], lhsT=wdr[di][:, fc, dc * 128:(dc + 1) * 128], rhs=hid[:, fc, c0:c0 + n], start=(fc == 0), stop=(fc == 3)), reads=[f"wd{di}", "hid"], writes=[yn])
                        if c0 == 1024:
                            tb_, tn_ = (r1, "r1") if dc % 2 == 0 else (r2, "r2")
                            for sq_ in range(4):
                                R.add("act", lambda e, yb=yb, dc=dc, sq_=sq_, tb_=tb_: e.activation(out=tb_[:, sq_ * 64:(sq_ + 1) * 64], in_=yb[:, sq_ * 64:(sq_ + 1) * 64], func=AF.Copy, scale=modT[:, 80 + dc, 1 + sq_:2 + sq_]), reads=[yn, "modT2"], writes=[tn_])
                            R.add("dve", lambda e, dc=dc, tb_=tb_: e.tensor_tensor(out=yacc[:, dc, 1024:1280], in0=yacc[:, dc, 1024:1280], in1=tb_[:, 0:256], op=ALU.add), reads=[tn_, "yacc"], writes=["yacc"])
                            continue
                        for (s0_, sn_, seq) in SEG:
                            if s0_ < c0 or s0_ >= c0 + n:
                                continue
                            R.add("dve", lambda e, yb=yb, dc=dc, s0_=s0_, sn_=sn_, seq=seq, c0=c0: e.scalar_tensor_tensor(out=yacc[:, dc, s0_:s0_ + sn_], in0=yb[:, s0_ - c0:s0_ - c0 + sn_], scalar=modT[:, 80 + dc, seq:seq + 1], in1=yacc[:, dc, s0_:s0_ + sn_], op0=ALU.mult, op1=ALU.add), reads=[yn, "modT2", "yacc"], writes=["yacc"])
            FENCE()

            A.set(88 * KB)
            yst = [A.a(D, F32) for _ in range(2)]
            for t in range(10):
                i = t % 2
                for q4 in range(4):
                    bi, bn = next_bank()
                    for j in range(4):
                        dc = q4 * 4 + j
                        R.add("pe", lambda e, bi=bi, j=j, dc=dc, t=t: e.transpose(out=banks[bi][:, j * 128:(j + 1) * 128], in_=yacc[:, dc, t * 128:(t + 1) * 128], identity=identf), reads=["yacc", "identf"], writes=[bn])
                    if q4 % 2 == 0:
                        R.add("act", lambda e, bi=bi, q4=q4, i=i: e.copy(out=yst[i][:, q4 * 512:(q4 + 1) * 512], in_=bk(bi)), reads=[bn], writes=[f"yst{i}"])
                    else:
                        R.add("dve", lambda e, bi=bi, q4=q4, i=i: e.tensor_copy(out=yst[i][:, q4 * 512:(q4 + 1) * 512], in_=bk(bi)), reads=[bn], writes=[f"yst{i}"])
                R.add("sp", lambda e, i=i, t=t: e.dma_start(out=y_out[t * 128:(t + 1) * 128, :], in_=yst[i]), reads=[f"yst{i}"], dma=f"yst{i}")
        except _Stop:
            pass
        R.emit(st)
    _DBG['outs'] = dbg_outs
    return nc


_CACHE = {}


def kernel(**inp):
    f = lambda k: np.ascontiguousarray(np.asarray(inp[k], dtype=np.float32))
    xp, xs = f("x_prompt"), f("x_sample")
    csk_, csv_, cbk_, cbv_ = f("cache_sb_k")[0], f("cache_sb_v")[0], f("cache_band_k")[0], f("cache_band_v")[0]
    cp, cs = f("c_prompt"), f("c_sample")
    tab = f("rel_bias_band")[0]
    kl = np.arange(128)[:, None]
    ql = np.arange(128)[None, :]
    btile = np.zeros((128, 24, 128), np.float32)
    for j in range(3):
        idx = np.clip(128 * j + ql - kl, -128, 128) + 128
        for h in range(8):
            btile[:, h * 3 + j, :] = tab[h][idx]
    w_rt = np.ascontiguousarray(np.concatenate([f("w_router_group")[0], f("w_router_expert")[0]], axis=1))
    b_rt = np.ascontiguousarray(np.concatenate([f("b_router_group")[0].reshape(1, 4), f("b_router_expert")[0].reshape(1, 32)], axis=1))
    shared = dict(
        btile=btile, norm_mix=f("norm_mix")[0].reshape(16, 128), norm_ffn=f("norm_ffn")[0].reshape(16, 128),
        w_ada=f("w_ada")[0], b_ada=f("b_ada")[0].reshape(96, 128), w_in=f("w_in")[0],
        q_norm=f("q_norm_band")[0], k_norm=f("k_norm_band")[0], w_psb=f("w_proj_sb")[0], w_pbd=f("w_proj_band")[0],
        w_out=f("w_out")[0], w_rt=w_rt, b_rt=b_rt, w_gate=f("w_gate")[0], w_up=f("w_up")[0], w_down=f("w_down")[0])
    in_maps = []
    for c in range(8):
        b, qi = c // 4, c % 4
        x_tok = np.zeros((34 * 128, D), np.float32)
        npre = 1024 * qi
        if npre:
            x_tok[3072 - npre:3072] = xp[b, 0:npre]
        x_tok[3072:4096] = xp[b, npre:npre + 1024]
        x_tok[4096:4352] = xs[4 * c:4 * c + 4].reshape(256, D)
        kbias = np.zeros((128, 32), np.float32)
        kbias[:, 0:(3072 - npre) // 128] = NEG
        m = dict(shared)
        m.update(x_tok=x_tok, kbias=kbias, c5=np.ascontiguousarray(np.concatenate([cp[b:b + 1], cs[4 * c:4 * c + 4]], axis=0)),
                 csk=np.ascontiguousarray(csk_[4 * c:4 * c + 4].reshape(4, 4096, 1024)), csv=np.ascontiguousarray(csv_[4 * c:4 * c + 4].reshape(4, 4096, 1024)),
                 cbk=np.ascontiguousarray(cbk_[4 * c:4 * c + 4].reshape(4, 512, 1024)), cbv=np.ascontiguousarray(cbv_[4 * c:4 * c + 4].reshape(4, 512, 1024)))
        in_maps.append(m)
    if inp.get("_maps_only"):
        return in_maps
    if "nc" not in _CACHE:
        _CACHE["nc"] = build()
    res = run_bass_kernel_spmd(_CACHE["nc"], in_maps, core_ids=list(range(8)))
    r = res.results
    yp = np.zeros((2, 4096, D), np.float32)
    ys = np.zeros((32, 64, D), np.float32)
    pk = np.zeros((1, 2, 4096, 8, 128), np.float32)
    pv = np.zeros_like(pk)
    pbk = np.zeros((1, 2, 512, 8, 128), np.float32)
    pbv = np.zeros_like(pbk)
    sk = np.zeros((1, 32, 64, 8, 128), np.float32)
    sv = np.zeros_like(sk)
    sbk = np.zeros_like(sk)
    sbv = np.zeros_like(sk)
    for c in range(8):
        b, qi = c // 4, c % 4
        o = r[c]
        sl = slice(1024 * qi, 1024 * qi + 1024)
        yp[b, sl] = o["y_out"][0:1024]
        ys[4 * c:4 * c + 4] = o["y_out"][1024:1280].reshape(4, 64, D)
        pk[0, b, sl] = o["ksb_out"][0:1024].reshape(1024, 8, 128)
        pv[0, b, sl] = o["vsb_out"][0:1024].reshape(1024, 8, 128)
        sk[0, 4 * c:4 * c + 4] = o["ksb_out"][1024:1280].reshape(4, 64, 8, 128)
        sv[0, 4 * c:4 * c + 4] = o["vsb_out"][1024:1280].reshape(4, 64, 8, 128)
        sbk[0, 4 * c:4 * c + 4] = o["kbd_out"][1024:1280].reshape(4, 64, 8, 128)
        sbv[0, 4 * c:4 * c + 4] = o["vbd_out"][1024:1280].reshape(4, 64, 8, 128)
        if qi == 3:
            pbk[0, b] = o["kbd_out"][512:1024].reshape(512, 8, 128)
            pbv[0, b] = o["vbd_out"][512:1024].reshape(512, 8, 128)
    return (yp, ys, pk, pv, pbk, pbv, sk, sv, sbk, sbv)
```

```python
import numpy as np
from contextlib import ExitStack
from itertools import zip_longest
import concourse.bass as bass
import concourse.mybir as mybir
from concourse.bass_utils import run_bass_kernel_spmd

F32 = mybir.dt.float32
BF16 = mybir.dt.bfloat16
AF = mybir.ActivationFunctionType
ALU = mybir.AluOpType

D = 2048
NCH = 16
DIN = 10240
NPRE = 24
NOWN = 1280
SC = 128 ** -0.5
EPS = 1e-6
NEG = -30000.0
ENGS = ("pe", "act", "dve", "pool", "sp")
EPOCH = 3000
BANKS = {f"b{i}" for i in range(8)}
TBS = [(0, 512), (512, 512), (1024, 256)]


class Rec:
    def __init__(self, nc):
        self.nc = nc
        self.ops = []
        self.last_w = {}
        self.readers = {}
        self.dma_keys = []
        self.fence = None
        self.last_eng = {}
        self.last_dma = {}

    def add(self, eng, fn, reads=(), writes=(), dma=None):
        writes = list(writes) + [r for r in reads if r in BANKS]
        reads = [r for r in reads if r not in BANKS]
        oid = len(self.ops)
        deps = set()
        if self.fence is not None:
            deps.add(self.fence)
        for r in reads:
            w = self.last_w.get(r)
            if w is not None:
                deps.add(w)
        for w_ in writes:
            w = self.last_w.get(w_)
            if w is not None:
                deps.add(w)
            deps.update(self.readers.get(w_, {}).values())
        rk = ("d", dma) if dma is not None else ("e", eng)
        for r in reads:
            self.readers.setdefault(r, {})[rk] = oid
        for w_ in writes:
            self.last_w[w_] = oid
            self.readers[w_] = {}
        if dma is not None:
            if dma not in self.dma_keys:
                self.dma_keys.append(dma)
            self.last_dma[dma] = oid
        else:
            self.last_eng[eng] = oid
        self.ops.append(dict(eng=eng, fn=fn, deps=deps, dma=dma))
        return oid

    def barrier(self, fn):
        oid = len(self.ops)
        deps = set(self.last_eng.values()) | set(self.last_dma.values())
        if self.fence is not None:
            deps.add(self.fence)
        self.ops.append(dict(eng="dve", fn=fn, deps=deps, dma=None))
        self.last_eng["dve"] = oid
        self.fence = oid

    def emit(self, stack):
        nc = self.nc
        ops = self.ops
        src = set()
        for o in ops:
            for d in o["deps"]:
                od = ops[d]
                if od["dma"] is None and od["eng"] == "pe" and o["eng"] == "pe" and o["dma"] is None:
                    continue
                src.add(d)
        cnt = {e: 0 for e in ENGS}
        dcnt = {k: 0 for k in self.dma_keys}
        tick = {}
        for i, o in enumerate(ops):
            if o["dma"] is not None:
                dcnt[o["dma"]] += 16
                tick[i] = ("d", o["dma"], 0, dcnt[o["dma"]])
            elif i in src:
                c = cnt[o["eng"]]
                cnt[o["eng"]] += 1
                tick[i] = ("e", o["eng"], c // EPOCH, c % EPOCH + 1)
        sems = {}
        for e in ENGS:
            for ep in range(max(cnt[e] - 1, 0) // EPOCH + 1):
                sems[("e", e, ep)] = stack.enter_context(nc.semaphore(f"s_{e}_{ep}"))
        for k in self.dma_keys:
            assert dcnt[k] < 60000, (k, dcnt[k])
            sems[("d", k, 0)] = stack.enter_context(nc.semaphore(f"d_{k}"))
        engobj = {"pe": nc.tensor, "act": nc.scalar, "dve": nc.vector, "pool": nc.gpsimd, "sp": nc.sync}
        per = {e: [] for e in ENGS}
        for i, o in enumerate(ops):
            per[o["eng"]].append(i)
        final = dict(dcnt)

        def run_engine(e):
            eng = engobj[e]
            seen = {}
            for i in per[e]:
                o = ops[i]
                need = {}
                for d in o["deps"]:
                    if d not in tick:
                        continue
                    kind, key, ep, val = tick[d]
                    if kind == "e" and key == e and e == "pe" and o["dma"] is None:
                        continue
                    sk = (kind, key, ep)
                    if seen.get(sk, 0) >= val:
                        continue
                    if need.get(sk, 0) < val:
                        need[sk] = val
                items = list(need.items())
                for sk, val in items[:-1]:
                    eng.wait_ge(sems[sk], val)
                    seen[sk] = val
                ins = o["fn"](eng)
                if items:
                    sk, val = items[-1]
                    ins._wait_ge(sems[sk], val)
                    seen[sk] = val
                if i in tick:
                    kind, key, ep, val = tick[i]
                    if kind == "d":
                        ins.then_inc(sems[("d", key, 0)], 16)
                    else:
                        ins.then_inc(sems[("e", key, ep)], 1)
            if e == "sp":
                for k, v in final.items():
                    if v > 0:
                        eng.wait_ge(sems[("d", k, 0)], v)

        with nc.Block() as block:
            @block.tensor
            def _(t):
                run_engine("pe")

            @block.scalar
            def _(t):
                run_engine("act")

            @block.vector
            def _(t):
                run_engine("dve")

            @block.gpsimd
            def _(t):
                run_engine("pool")

            @block.sync
            def _(t):
                run_engine("sp")


class Arena:
    def __init__(self, nc, nbytes):
        self.t = nc.alloc_sbuf_tensor("arena", [128, nbytes], mybir.dt.uint8)
        self.n = nbytes
        self.off = 0

    def mark(self):
        return self.off

    def set(self, off):
        self.off = off

    def release(self, m):
        self.off = m

    def a(self, cols, dt):
        nb = cols * (4 if dt == F32 else 2)
        nb = (nb + 63) // 64 * 64
        assert self.off + nb <= self.n, ("SBUF overflow", self.off, nb)
        ap = self.t[:, self.off:self.off + cols * (4 if dt == F32 else 2)].bitcast(dt)
        self.off += nb
        return ap


_DBG = {}


class _Stop(Exception):
    pass


def build(upto=None, dbg=False):
    nc = bass.Bass("TRN2", target_bir_lowering=False)

    def din(name, shape, dt=F32):
        return nc.dram_tensor(name, list(shape), dt, kind="ExternalInput").ap()

    def dout(name, shape):
        return nc.dram_tensor(name, list(shape), F32, kind="ExternalOutput").ap()

    x_tok = din("x_tok", [34 * 128, D])
    kbias_d = din("kbias", [128, 32])
    c5 = din("c5", [5, D])
    csk = din("csk", [4, 4096, 1024])
    csv = din("csv", [4, 4096, 1024])
    cbk = din("cbk", [4, 512, 1024])
    cbv = din("cbv", [4, 512, 1024])
    btile = din("btile", [128, 24, 128])
    norm_mix = din("norm_mix", [16, 128])
    norm_ffn = din("norm_ffn", [16, 128])
    w_ada = din("w_ada", [D, 6 * D])
    b_ada = din("b_ada", [96, 128])
    w_in = din("w_in", [D, DIN])
    qn_d = din("q_norm", [8, 128])
    kn_d = din("k_norm", [8, 128])
    w_psb = din("w_psb", [1024, D])
    w_pbd = din("w_pbd", [1024, D])
    w_out = din("w_out", [D, D])
    w_rt = din("w_rt", [D, 36])
    b_rt = din("b_rt", [1, 36])
    w_gate = din("w_gate", [32, D, 512])
    w_up = din("w_up", [32, D, 512])
    w_down = din("w_down", [32, 512, D])

    y_out = dout("y_out", [NOWN, D])
    ksb_out = dout("ksb_out", [NOWN, 1024])
    vsb_out = dout("vsb_out", [NOWN, 1024])
    kbd_out = dout("kbd_out", [NOWN, 1024])
    vbd_out = dout("vbd_out", [NOWN, 1024])

    kk = dict(kind="ExternalOutput") if dbg else {}
    KTs = nc.dram_tensor("KTs", [8, 128, 4096], BF16, **kk).ap()
    Vs = nc.dram_tensor("Vs", [8, 4096, 128], BF16, **kk).ap()
    dbg_outs = {}

    def DUMP(R, name, ap, cols, dt, res):
        if not dbg:
            return
        d = nc.dram_tensor("dbg_" + name, [128, cols], dt, kind="ExternalOutput").ap()
        dbg_outs[name] = d
        R.add("sp", lambda e: e.dma_start(out=d, in_=ap), reads=[res], dma="dbg")

    st = ExitStack()
    with st:
        A = Arena(nc, 206 * 1024)
        banks = [nc.alloc_psum_tensor(f"bank{i}", [128, 512], F32) for i in range(8)]
        R = Rec(nc)
        try:
            bk = lambda i: banks[i][:, :]
            uid = [0]

            def U(p):
                uid[0] += 1
                return f"{p}{uid[0]}"

            identf = A.a(128, F32)
            identb = A.a(128, BF16)
            negtri = A.a(128, BF16)
            negones = A.a(128, BF16)
            onesb = A.a(128, BF16)
            dmask = A.a(128, BF16)
            tmpf = A.a(128, F32)
            epsc = A.a(1, F32)
            zeroc = A.a(1, F32)
            onec = A.a(1, F32)
            kbias = A.a(32, F32)
            modT = A.a(96 * 5, F32).rearrange("p (b s) -> p b s", s=5)
            gmm = A.a(80, F32).rearrange("p (b s) -> p b s", s=5)
            gmf = A.a(80, F32).rearrange("p (b s) -> p b s", s=5)
            nmix = A.a(16, F32)
            nffn = A.a(16, F32)
            qgs = A.a(8, F32)
            kg = A.a(8, F32)
            dummy = A.a(16, F32)

            R.add("pool", lambda e: e.memset(identf, 1.0), writes=["identf"])
            R.add("pool", lambda e: e.affine_select(out=identf, in_=identf, pattern=[[-1, 128]], compare_op=ALU.is_equal, fill=0.0, base=0, channel_multiplier=1), reads=["identf"], writes=["identf"])
            R.add("dve", lambda e: e.tensor_copy(out=identb, in_=identf), reads=["identf"], writes=["identb"])
            R.add("pool", lambda e: e.memset(tmpf, -1.0), writes=["tmpf"])
            R.add("pool", lambda e: e.affine_select(out=tmpf, in_=tmpf, pattern=[[-1, 128]], compare_op=ALU.is_ge, fill=0.0, base=0, channel_multiplier=1), reads=["tmpf"], writes=["tmpf"])
            R.add("dve", lambda e: e.tensor_copy(out=negtri, in_=tmpf), reads=["tmpf"], writes=["negtri"])
            R.add("pool", lambda e: e.memset(tmpf, 1.0), reads=["tmpf"], writes=["tmpf"])
            R.add("pool", lambda e: e.affine_select(out=tmpf, in_=tmpf, pattern=[[1, 128]], compare_op=ALU.is_gt, fill=0.0, base=0, channel_multiplier=-1), reads=["tmpf"], writes=["tmpf"])
            R.add("dve", lambda e: e.tensor_copy(out=dmask, in_=tmpf), reads=["tmpf"], writes=["dmask"])
            R.add("pool", lambda e: e.memset(negones, -1.0), writes=["negones"])
            R.add("pool", lambda e: e.memset(onesb, 1.0), writes=["onesb"])
            R.add("pool", lambda e: e.memset(epsc, EPS), writes=["epsc"])
            R.add("pool", lambda e: e.memset(zeroc, 0.0), writes=["zeroc"])
            R.add("pool", lambda e: e.memset(onec, 1.0), writes=["onec"])
            R.add("pool", lambda e: e.memset(dummy, 0.0), writes=["dummy"])
            R.add("sp", lambda e: e.dma_start(out=kbias, in_=kbias_d), writes=["kbias"], dma="kbias")
            stg = A.a(128, F32)

            def load_T(src_ap, rows, dst, post=None):
                n = U("ld")
                R.add("sp", lambda e: e.dma_start(out=stg[0:rows, :], in_=src_ap), writes=["stg"], dma="stg")
                R.add("pe", lambda e: e.transpose(out=banks[7][:, 0:rows], in_=stg[0:rows, :], identity=identf[0:rows, 0:rows]), reads=["stg", "identf"], writes=["b7"])
                if post is None:
                    R.add("dve", lambda e: e.tensor_copy(out=dst, in_=banks[7][:, 0:rows]), reads=["b7"], writes=[n, "cparam"])
                else:
                    R.add("dve", lambda e: e.tensor_scalar(out=dst, in0=banks[7][:, 0:rows], scalar1=post, scalar2=None, op0=ALU.mult), reads=["b7"], writes=[n, "cparam"])

            bT = A.a(96, F32)
            load_T(b_ada, 96, bT)
            load_T(norm_mix, 16, nmix)
            load_T(norm_ffn, 16, nffn)
            load_T(qn_d, 8, qgs, post=SC)
            load_T(kn_d, 8, kg)

            assert A.off <= 8192, A.off
            A.set(173 * 1024)
            scT = A.a(16 * 5, BF16).rearrange("p (k s) -> p k s", s=5)
            A.set(174 * 1024)
            wa = [A.a(16 * 512, BF16).rearrange("p (k n) -> p k n", n=512) for _ in range(2)]
            c_sb = wa[1].rearrange("p k n -> p (k n)")[:, 0:2 * D].bitcast(F32)
            R.add("sp", lambda e: e.dma_start(out=c_sb[0:5, :], in_=c5), writes=["wa1"], dma="c_sb")
            R.add("act", lambda e: e.activation(out=c_sb[0:5, :], in_=c_sb[0:5, :], func=AF.Silu), reads=["wa1"], writes=["wa1"])
            for k in range(16):
                R.add("pe", lambda e, k=k: e.transpose(out=banks[6][:, k * 5:(k + 1) * 5], in_=c_sb[0:5, k * 128:(k + 1) * 128], identity=identf[0:5, 0:5]), reads=["wa1", "identf"], writes=["b6"])
            R.add("dve", lambda e: e.tensor_copy(out=scT.rearrange("p k s -> p (k s)"), in_=banks[6][:, 0:80]), reads=["b6"], writes=["scT"])
            b5v = banks[5][:, 0:480].rearrange("p (b s) -> p b s", s=5)

            def ada_panel(pn):
                wb_ = wa[pn % 2]
                wn = f"wa{pn % 2}"
                R.add("pool", lambda e: e.dma_start(out=wb_, in_=w_ada[:, pn * 512:(pn + 1) * 512].rearrange("(k p) n -> p k n", p=128)), writes=[wn], dma=wn)
                for fb in range(4):
                    blk = pn * 4 + fb
                    for k in range(16):
                        R.add("pe", lambda e, fb=fb, k=k, blk=blk: e.matmul(banks[5][:, blk * 5:(blk + 1) * 5], lhsT=wb_[:, k, fb * 128:(fb + 1) * 128], rhs=scT[:, k, :], start=(k == 0), stop=(k == 15)), reads=[wn, "scT"], writes=["b5"])

            def ada_finish(b0, b1, tok):
                for s in range(5):
                    R.add("dve", lambda e, s=s: e.tensor_tensor(out=modT[:, b0:b1, s], in0=b5v[:, b0:b1, s], in1=bT[:, b0:b1], op=ALU.add), reads=["b5", "cparam"], writes=[tok])

            for pn in range(8):
                ada_panel(pn)
            ada_finish(0, 32, "modT1")
            for s in range(5):
                R.add("dve", lambda e, s=s: e.scalar_tensor_tensor(out=gmm[:, :, s], in0=modT[:, 16:32, s], scalar=1.0, in1=nmix, op0=ALU.add, op1=ALU.mult), reads=["modT1", "cparam"], writes=["gmm"])
            ada_next = [8]

            def ada_more(n):
                for _ in range(n):
                    if ada_next[0] < 24:
                        ada_panel(ada_next[0])
                        ada_next[0] += 1
                        if ada_next[0] == 24:
                            ada_finish(32, 96, "modT2")
                            for s in range(5):
                                R.add("dve", lambda e, s=s: e.scalar_tensor_tensor(out=gmf[:, :, s], in0=modT[:, 64:80, s], scalar=1.0, in1=nffn, op0=ALU.add, op1=ALU.mult), reads=["modT2", "cparam"], writes=["gmf"])


            KB = 1024
            HT0, OAT0, OBT0, PH0 = 8 * KB, 64 * KB, 84 * KB, 104 * KB
            HTC = NOWN + 512

            ncnt = [0]

            def norm_bufs():
                return dict(xts=[A.a(D, F32) for _ in range(2)], junk=A.a(D, BF16), ybs=[A.a(D, BF16) for _ in range(2)], ssq=A.a(4, F32))

            def norm_tile(NB, ti, segs, dst_fn):
                i = ncnt[0] % 2
                ncnt[0] += 1
                xt, yb, junk, ssq = NB["xts"][i], NB["ybs"][i], NB["junk"], NB["ssq"]
                xn, yn = f"xt{i}", f"yb{i}"
                R.add("sp", lambda e: e.dma_start(out=xt, in_=x_tok[ti * 128:(ti + 1) * 128, :]), writes=[xn], dma=xn)
                R.add("dve", lambda e: e.memset(ssq[:, 0:1], 0.0), writes=["ssq"])
                R.add("act", lambda e: e.activation(out=junk, in_=xt, func=AF.Square, accum_out=ssq[:, 0:1]), reads=[xn, "ssq"], writes=["junk", "ssq"])
                R.add("act", lambda e: e.activation(out=ssq[:, 1:2], in_=ssq[:, 0:1], func=AF.Ln, scale=1.0 / D, bias=epsc), reads=["ssq", "epsc"], writes=["ssq"])
                R.add("act", lambda e: e.activation(out=ssq[:, 2:3], in_=ssq[:, 1:2], func=AF.Exp, scale=-0.5), reads=["ssq"], writes=["ssq"])
                R.add("dve", lambda e: e.tensor_scalar(out=yb, in0=xt, scalar1=ssq[:, 2:3], scalar2=None, op0=ALU.mult), reads=[xn, "ssq"], writes=[yn])
                for half in range(2):
                    pb = banks[6 + half][:, 0:512].bitcast(BF16)
                    bn = f"b{6 + half}"
                    for k8 in range(8):
                        dc = half * 8 + k8
                        R.add("pe", lambda e, pb=pb, k8=k8, dc=dc: e.transpose(out=pb[:, k8 * 128:(k8 + 1) * 128], in_=yb[:, dc * 128:(dc + 1) * 128], identity=identb), reads=[yn, "identb"], writes=[bn])
                    for k8 in range(8):
                        dc = half * 8 + k8
                        for (p0, n, seq) in segs:
                            dst, dn = dst_fn(dc, p0, n)
                            R.add("dve", lambda e, pb=pb, k8=k8, dc=dc, p0=p0, n=n, seq=seq, dst=dst: e.tensor_scalar(out=dst, in0=pb[:, k8 * 128 + p0:k8 * 128 + p0 + n], scalar1=gmm[:, dc, seq:seq + 1], scalar2=modT[:, dc, seq:seq + 1], op0=ALU.mult, op1=ALU.add), reads=[bn, "gmm", "modT1"], writes=[dn])

            mmb = [0]

            def next_bank():
                mmb[0] ^= 1
                return mmb[0], f"b{mmb[0]}"

            phase = [0]

            LABELS = ["A", "B1", "SB", "BAND", "MERGE", "WOUT", "ROUTER", "MOE"]

            def FENCE(label=None, soft=False):
                if label is None:
                    label = LABELS[phase[0]]
                    phase[0] += 1
                    if soft and upto != label:
                        return
                elif upto != label:
                    return
                R.barrier(lambda e: e.memset(dummy, 0.0))
                if upto is not None and label == upto:
                    raise _Stop()

            A.set(HT0)
            NB = norm_bufs()
            wkv = A.a(16 * 2048, BF16).rearrange("p (k n) -> p k n", n=2048)
            for q4 in range(4):
                R.add("pool", lambda e, q4=q4: e.dma_start(out=wkv[:, :, q4 * 512:(q4 + 1) * 512], in_=w_in[:, 1024 + q4 * 512:1024 + (q4 + 1) * 512].rearrange("(k p) n -> p k n", p=128)), writes=["wkv"], dma="wkv")
            hTb = [A.a(16 * 512, BF16).rearrange("p (k n) -> p k n", n=512) for _ in range(2)]
            ktblk = [A.a(8 * 512, BF16).rearrange("p (h n) -> p h n", n=512) for _ in range(2)]
            vblk = [A.a(4 * 1024, BF16).rearrange("p (t n) -> p t n", n=1024) for _ in range(2)]
            assert A.off <= 173 * 1024, A.off
            def normA(blk, t4):
                i = blk % 2
                hb, hn = hTb[i], f"hTb{i}"
                norm_tile(NB, blk * 4 + t4, [(0, 128, 0)], lambda dc, p0, n: (hb[:, dc, t4 * 128 + p0:t4 * 128 + p0 + n], hn))

            def projA(blk):
                i = blk % 2
                hb, hn = hTb[i], f"hTb{i}"
                items = []

                def kitem(h):
                    bi, bn = next_bank()
                    for k in range(16):
                        R.add("pe", lambda e, k=k: e.matmul(bk(bi), lhsT=wkv[:, k, h * 128:(h + 1) * 128], rhs=hb[:, k, :], start=(k == 0), stop=(k == 15)), reads=["wkv", hn], writes=[bn])
                    R.add("act", lambda e: e.copy(out=ktblk[i][:, h, :], in_=bk(bi)), reads=[bn], writes=[f"ktblk{i}"])
                    if h == 7:
                        R.add("sp", lambda e: e.dma_start(out=KTs[:, :, blk * 512:(blk + 1) * 512].rearrange("h p n -> p h n"), in_=ktblk[i]), reads=[f"ktblk{i}"], writes=["KTs"], dma=f"ktw{i}")

                def vitem(t4, cb):
                    bi, bn = next_bank()
                    for k in range(16):
                        R.add("pe", lambda e, k=k: e.matmul(bk(bi), lhsT=hb[:, k, t4 * 128:(t4 + 1) * 128], rhs=wkv[:, k, 1024 + cb * 512:1024 + (cb + 1) * 512], start=(k == 0), stop=(k == 15)), reads=["wkv", hn], writes=[bn])
                    R.add("act", lambda e: e.copy(out=vblk[i][:, t4, cb * 512:(cb + 1) * 512], in_=bk(bi)), reads=[bn], writes=[f"vblk{i}"])
                    if cb == 1:
                        R.add("sp", lambda e: e.dma_start(out=Vs[:, blk * 512 + t4 * 128:blk * 512 + (t4 + 1) * 128, :].rearrange("h p d -> p h d"), in_=vblk[i][:, t4, :].rearrange("p (h d) -> p h d", d=128)), reads=[f"vblk{i}"], writes=["Vs"], dma=f"vw{i}")

                for h in range(8):
                    items.append(lambda h=h: kitem(h))
                for t4 in range(4):
                    for cb in range(2):
                        items.append(lambda t4=t4, cb=cb: vitem(t4, cb))
                return items

            for t4 in range(4):
                normA(0, t4)
            for blk in range(6):
                items = projA(blk)
                for t4 in range(4):
                    if blk + 1 < 6:
                        normA(blk + 1, t4)
                    for it in items[t4 * 4:(t4 + 1) * 4]:
                        it()
                ada_more(3)
            ada_more(24)
            FENCE()

            A.set(HT0)
            hT = A.a(16 * HTC, BF16).rearrange("p (k n) -> p k n", n=HTC)
            assert A.off <= OAT0
            A.set(OAT0)
            oAT = A.a(8 * NOWN, BF16).rearrange("p (h n) -> p h n", n=NOWN)
            oBT = A.a(8 * NOWN, BF16).rearrange("p (h n) -> p h n", n=NOWN)
            assert A.off <= PH0
            A.set(PH0)
            qT = A.a(8 * NOWN, BF16).rearrange("p (h n) -> p h n", n=NOWN)
            kTS = A.a(8 * 256, BF16).rearrange("p (h n) -> p h n", n=256)
            vS = A.a(4 * 1024, BF16).rearrange("p (t n) -> p t n", n=1024)
            mB1 = A.mark()
            NB = norm_bufs()
            for t4 in range(4):
                norm_tile(NB, 20 + t4, [(0, 128, 0)], lambda dc, p0, n, t4=t4: (hT[:, dc, NOWN + t4 * 128 + p0:NOWN + t4 * 128 + p0 + n], "hT"))
            for t in range(8):
                norm_tile(NB, 24 + t, [(0, 128, 0)], lambda dc, p0, n, t=t: (hT[:, dc, t * 128 + p0:t * 128 + p0 + n], "hT"))
            for t in range(2):
                norm_tile(NB, 32 + t, [(0, 64, 1 + 2 * t), (64, 64, 2 + 2 * t)], lambda dc, p0, n, t=t: (hT[:, dc, 1024 + t * 128 + p0:1024 + t * 128 + p0 + n], "hT"))

            FENCE("B1n")
            wcs = [A.a(16 * 512, BF16).rearrange("p (k n) -> p k n", n=512) for _ in range(2)]
            ostg = [A.a(512, F32) for _ in range(2)]
            osi = [0]
            ktown = [A.a(512, BF16) for _ in range(2)]
            kti = [0]
            vstg = [A.a(512, BF16) for _ in range(2)]
            vsi = [0]
            TMT = [(t * 128, 128) for t in range(8)] + [(1024 + s * 64, 64) for s in range(4)]
            wci = [0]

            def load_wc(cb, nbuf=2):
                i = wci[0] % nbuf
                wci[0] += 1
                buf = wcs[i]
                R.add("pool", lambda e: e.dma_start(out=buf, in_=w_in[:, cb * 512:(cb + 1) * 512].rearrange("(k p) n -> p k n", p=128)), writes=[f"wc{i}"], dma=f"wc{i}")
                return buf, f"wc{i}"

            def fm_proj(wc, wn, hh, c0, n):
                bi, bn = next_bank()
                for k in range(16):
                    R.add("pe", lambda e, k=k: e.matmul(banks[bi][:, 0:n], lhsT=wc[:, k, hh * 128:(hh + 1) * 128], rhs=hT[:, k, c0:c0 + n], start=(k == 0), stop=(k == 15)), reads=[wn, "hT"], writes=[bn])
                return bi, bn

            def tm_proj(wc, wn, c0, n):
                bi, bn = next_bank()
                for k in range(16):
                    R.add("pe", lambda e, k=k: e.matmul(banks[bi][0:n, :], lhsT=hT[:, k, c0:c0 + n], rhs=wc[:, k, :], start=(k == 0), stop=(k == 15)), reads=[wn, "hT"], writes=[bn])
                return bi, bn

            def tm_out(bi, bn, n, dst_ap):
                i = osi[0] % 2
                osi[0] += 1
                ob_ = ostg[i]
                R.add("act", lambda e: e.copy(out=ob_[0:n, :], in_=banks[bi][0:n, :]), reads=[bn], writes=[f"ostg{i}"])
                R.add("sp", lambda e: e.dma_start(out=dst_ap, in_=ob_[0:n, :]), reads=[f"ostg{i}"], dma=f"ostg{i}")

            for cb in range(6):
                ty = cb // 2
                half = cb % 2
                wc, wn = load_wc(cb)
                if ty in (0, 1):
                    for hh in range(4):
                        h = half * 4 + hh
                        for (c0, n) in TBS:
                            bi, bn = fm_proj(wc, wn, hh, c0, n)
                            if ty == 0:
                                R.add("act", lambda e, bi=bi, n=n, h=h, c0=c0: e.activation(out=qT[:, h, c0:c0 + n], in_=banks[bi][:, 0:n], func=AF.Copy, scale=SC), reads=[bn], writes=["qT"])
                            elif c0 < 1024:
                                i = kti[0] % 2
                                kti[0] += 1
                                R.add("act", lambda e, bi=bi, i=i: e.copy(out=ktown[i], in_=bk(bi)), reads=[bn], writes=[f"ktown{i}"])
                                R.add("sp", lambda e, i=i, h=h, c0=c0: e.dma_start(out=KTs[h, :, 3072 + c0:3072 + c0 + 512], in_=ktown[i]), reads=[f"ktown{i}"], writes=["KTs"], dma=f"ktown{i}")
                            else:
                                R.add("act", lambda e, bi=bi, h=h: e.copy(out=kTS[:, h, :], in_=banks[bi][:, 0:256]), reads=[bn], writes=["kTS"])
                if ty in (1, 2):
                    for (c0, n) in TMT:
                        bi, bn = tm_proj(wc, wn, c0, n)
                        dst = ksb_out if ty == 1 else vsb_out
                        tm_out(bi, bn, n, dst[c0:c0 + n, half * 512:(half + 1) * 512])
                        if ty == 2:
                            if c0 < 1024:
                                i = vsi[0] % 2
                                vsi[0] += 1
                                R.add("dve", lambda e, bi=bi, i=i: e.tensor_copy(out=vstg[i], in_=bk(bi)), reads=[bn], writes=[f"vstg{i}"])
                                R.add("sp", lambda e, i=i, c0=c0, half=half: e.dma_start(out=Vs[half * 4:(half + 1) * 4, 3072 + c0:3072 + c0 + 128, :].rearrange("h p d -> p h d"), in_=vstg[i].rearrange("p (h d) -> p h d", d=128)), reads=[f"vstg{i}"], writes=["Vs"], dma=f"vstg{i}")
                            else:
                                s = (c0 - 1024) // 64
                                R.add("dve", lambda e, bi=bi, s=s, half=half: e.tensor_copy(out=vS[0:64, s, half * 512:(half + 1) * 512], in_=banks[bi][0:64, :]), reads=[bn], writes=["vS"])
                FENCE("B1c%d" % cb)
            DUMP(R, "hT", hT.rearrange("p k n -> p (k n)"), 16 * HTC, BF16, "hT")
            DUMP(R, "qT", qT.rearrange("p h n -> p (h n)"), 8 * NOWN, BF16, "qT")
            DUMP(R, "kTS", kTS.rearrange("p h n -> p (h n)"), 8 * 256, BF16, "kTS")
            FENCE()
            A.release(mB1)

            ktH = [A.a(4096, BF16) for _ in range(2)]
            vH = [A.a(32 * 128, BF16).rearrange("p (t d) -> p t d", d=128) for _ in range(2)]
            kraw = [A.a(32 * 128, BF16).rearrange("p (t d) -> p t d", d=128) for _ in range(2)]
            e_sb = [A.a(512, F32) for _ in range(2)]
            sp_sb = [A.a(512, BF16) for _ in range(2)]
            a_sb = [A.a(512, BF16) for _ in range(2)]
            lsum = [A.a(128, BF16) for _ in range(2)]
            oslot = [0, 0]

            def sb_group(sid, q_ap, nq, tiles, first, last, kb_ap, diag, oslot_i, out_ap):
                zb, zn = banks[2 + sid], f"b{2 + sid}"
                tb, tn = banks[4 + sid], f"b{4 + sid}"
                ob = banks[6 + sid][:, oslot_i * 128:oslot_i * 128 + nq]
                on = f"b{6 + sid}"
                G = len(tiles)
                W = G * nq
                nk0 = tiles[0][4]
                en, spn, an, ln = f"e{sid}", f"sp{sid}", f"a{sid}", f"ls{sid}"

                def s0():
                    for i, (kT, kn, v, vn, nk) in enumerate(tiles):
                        R.add("pe", lambda e, i=i, kT=kT, nk=nk: e.matmul(zb[0:nk, i * nq:(i + 1) * nq], lhsT=kT, rhs=q_ap, start=True, stop=True), reads=[kn, "qT"], writes=[zn])

                def s1():
                    R.add("act", lambda e: e.activation(out=e_sb[sid][0:nk0, 0:W], in_=zb[0:nk0, 0:W], func=AF.Exp, bias=kb_ap[0:nk0, :]), reads=[zn, "kbias", "zeroc"], writes=[en])

                def s2():
                    R.add("act", lambda e: e.activation(out=sp_sb[sid][0:nk0, 0:W], in_=e_sb[sid][0:nk0, 0:W], func=AF.Ln, bias=onec[0:nk0, :]), reads=[en, "onec"], writes=[spn])
                    if diag:
                        R.add("dve", lambda e: e.tensor_tensor(out=sp_sb[sid][0:nk0, 0:nq], in0=sp_sb[sid][0:nk0, 0:nq], in1=dmask[0:nk0, 0:nq], op=ALU.mult), reads=[spn, "dmask"], writes=[spn])

                def s3():
                    for i, (kT, kn, v, vn, nk) in enumerate(tiles):
                        sl = slice(i * nq, (i + 1) * nq)
                        R.add("pe", lambda e, sl=sl, nk=nk: e.matmul(tb[0:nk, sl], lhsT=negtri[0:nk, 0:nk], rhs=sp_sb[sid][0:nk, sl], start=True, stop=False), reads=[spn, "negtri"], writes=[tn])
                        if not first:
                            R.add("pe", lambda e, sl=sl, nk=nk: e.matmul(tb[0:nk, sl], lhsT=negones[:, 0:nk], rhs=lsum[sid][:, 0:nq], start=False, stop=False), reads=[ln, "negones"], writes=[tn])
                        for i2 in range(i):
                            nk2 = tiles[i2][4]
                            R.add("pe", lambda e, sl=sl, nk=nk, i2=i2, nk2=nk2: e.matmul(tb[0:nk, sl], lhsT=negones[0:nk2, 0:nk], rhs=sp_sb[sid][0:nk2, i2 * nq:(i2 + 1) * nq], start=False, stop=False), reads=[spn, "negones"], writes=[tn])
                        R.add("pe", lambda e, sl=sl, nk=nk, kT=kT: e.matmul(tb[0:nk, sl], lhsT=kT, rhs=q_ap, start=False, stop=True), reads=[kn, "qT"], writes=[tn])
                    for i, (kT, kn, v, vn, nk) in enumerate(tiles):
                        if first and i == 0:
                            R.add("dve", lambda e: e.memset(lsum[sid], 0.0), writes=[ln])
                        R.add("dve", lambda e, i=i, nk=nk: e.tensor_tensor(out=lsum[sid][0:nk, 0:nq], in0=lsum[sid][0:nk, 0:nq], in1=sp_sb[sid][0:nk, i * nq:(i + 1) * nq], op=ALU.add), reads=[ln, spn], writes=[ln])

                def s4():
                    R.add("act", lambda e: e.activation(out=a_sb[sid][0:nk0, 0:W], in_=tb[0:nk0, 0:W], func=AF.Exp, bias=kb_ap[0:nk0, :]), reads=[tn, "kbias", "zeroc"], writes=[an])
                    if diag:
                        R.add("dve", lambda e: e.tensor_tensor(out=a_sb[sid][0:nk0, 0:nq], in0=a_sb[sid][0:nk0, 0:nq], in1=dmask[0:nk0, 0:nq], op=ALU.mult), reads=[an, "dmask"], writes=[an])

                def s5():
                    for i, (kT, kn, v, vn, nk) in enumerate(tiles):
                        R.add("pe", lambda e, i=i, v=v, nk=nk: e.matmul(ob, lhsT=v, rhs=a_sb[sid][0:nk, i * nq:(i + 1) * nq], start=(first and i == 0), stop=(last and i == G - 1)), reads=[vn, an], writes=[on])
                    if last:
                        R.add("dve", lambda e: e.tensor_copy(out=out_ap, in_=ob), reads=[on], writes=["oAT"])

                return [s0, s1, s2, s3, s4, s5]

            def interleave(streams):
                rows = list(zip_longest(*streams))

                def call(r, k):
                    if r is None:
                        return
                    for g in r:
                        if g is not None:
                            g[k]()

                if not rows:
                    return
                for k in range(3):
                    call(rows[0], k)
                for ri in range(len(rows)):
                    nxt = rows[ri + 1] if ri + 1 < len(rows) else None
                    call(rows[ri], 3)
                    call(nxt, 0)
                    call(rows[ri], 4)
                    call(nxt, 1)
                    call(nxt, 2)
                    call(rows[ri], 5)

            def chunks(lst, n):
                return [lst[i:i + n] for i in range(0, len(lst), n)]

            for hp in range(4):
                streams = []
                for sid in range(2):
                    h = hp * 2 + sid
                    R.add("sp", lambda e, sid=sid, h=h: e.dma_start(out=ktH[sid], in_=KTs[h]), reads=["KTs"], writes=[f"ktH{sid}"], dma=f"ktH{sid}")
                    R.add("sp", lambda e, sid=sid, h=h: e.dma_start(out=vH[sid], in_=Vs[h].rearrange("(t p) d -> p t d", p=128)), reads=["Vs"], writes=[f"vH{sid}"], dma=f"vH{sid}")
                    gl = []
                    for t in range(8):
                        q_ap = qT[:, h, t * 128:(t + 1) * 128]
                        tl = lambda j, sid=sid: (ktH[sid][:, j * 128:(j + 1) * 128], f"ktH{sid}", vH[sid][:, j, :], f"vH{sid}", 128)
                        groups = []
                        for ci_, ch in enumerate(chunks(list(range(24 + t, 23, -1)), 4)):
                            groups.append(([tl(j) for j in ch], zeroc, ci_ == 0))
                        for ch in chunks(list(range(23, -1, -1)), 4):
                            groups.append(([tl(j) for j in ch], kbias[:, ch[0]:ch[0] + 1], False))
                        osl = oslot[sid] % 4
                        oslot[sid] += 1
                        for gi, (tiles, kb_ap, dg) in enumerate(groups):
                            gl.append(sb_group(sid, q_ap, 128, tiles, gi == 0, gi == len(groups) - 1, kb_ap, dg, osl, oAT[:, h, t * 128:(t + 1) * 128]))
                    streams.append(gl)
                interleave(streams)

            for s in range(4):
                for hp in range(4):
                    streams = []
                    for sid in range(2):
                        h = hp * 2 + sid
                        R.add("pool", lambda e, sid=sid, h=h, s=s: e.dma_start(out=kraw[sid], in_=csk[s, :, h * 128:(h + 1) * 128].rearrange("(t p) d -> p t d", p=128)), writes=[f"kraw{sid}"], dma=f"kraw{sid}")
                    for sid in range(2):
                        h = hp * 2 + sid
                        R.add("pool", lambda e, sid=sid, h=h, s=s: e.dma_start(out=vH[sid], in_=csv[s, :, h * 128:(h + 1) * 128].rearrange("(t p) d -> p t d", p=128)), writes=[f"vH{sid}"], dma=f"vH{sid}")
                    for sid in range(2):
                        h = hp * 2 + sid
                        for t8 in range(4):
                            pb = banks[sid][:, 0:512].bitcast(BF16)
                            for j in range(8):
                                R.add("pe", lambda e, pb=pb, j=j, t8=t8, sid=sid: e.transpose(out=pb[:, j * 128:(j + 1) * 128], in_=kraw[sid][:, t8 * 8 + j, :], identity=identb), reads=[f"kraw{sid}", "identb"], writes=[f"b{sid}"])
                            R.add("dve", lambda e, pb=pb, t8=t8, sid=sid: e.tensor_copy(out=ktH[sid][:, t8 * 1024:(t8 + 1) * 1024], in_=pb), reads=[f"b{sid}"], writes=[f"ktH{sid}"])
                        q_ap = qT[:, h, 1024 + s * 64:1024 + (s + 1) * 64]
                        tl = lambda j, sid=sid: (ktH[sid][:, j * 128:(j + 1) * 128], f"ktH{sid}", vH[sid][:, j, :], f"vH{sid}", 128)
                        groups = [([(kTS[:, h, s * 64:(s + 1) * 64], "kTS", vS[0:64, s, h * 128:(h + 1) * 128], "vS", 64)], True)]
                        for ch in chunks(list(range(31, -1, -1)), 4):
                            groups.append(([tl(j) for j in ch], False))
                        osl = oslot[sid] % 4
                        oslot[sid] += 1
                        gl = []
                        for gi, (tiles, dg) in enumerate(groups):
                            gl.append(sb_group(sid, q_ap, 64, tiles, gi == 0, gi == len(groups) - 1, zeroc, dg, osl, oAT[:, h, 1024 + s * 64:1024 + (s + 1) * 64]))
                        streams.append(gl)
                    interleave(streams)
            DUMP(R, "oAT", oAT.rearrange("p h n -> p (h n)"), 8 * NOWN, BF16, "oAT")
            FENCE()

            A.set(PH0)
            bt = A.a(32 * 128, F32).rearrange("p (t q) -> p t q", q=128)
            for h in range(8):
                for j in range(3):
                    R.add("sp", lambda e, h=h, j=j: e.dma_start(out=bt[:, h * 4 + j, :], in_=btile[:, h * 3 + j, :]), writes=["bt"], dma="bt")
            for h in range(8):
                R.add("dve", lambda e, h=h: e.tensor_copy(out=bt[:, h * 4 + 3, :], in_=bt[:, h * 4 + 2, :]), reads=["bt"], writes=["bt"])
                R.add("pool", lambda e, h=h: e.memset(bt[0:64, h * 4 + 3, 64:128], NEG), reads=["bt"], writes=["bt"])
                R.add("pool", lambda e, h=h: e.memset(bt[64:128, h * 4 + 0, 0:64], NEG), reads=["bt"], writes=["bt"])
            qbT = A.a(4 * NOWN, BF16).rearrange("p (h n) -> p h n", n=NOWN)
            kbT = A.a(4 * HTC, BF16).rearrange("p (h n) -> p h n", n=HTC)
            vbP = A.a(12 * 512, BF16).rearrange("p (t n) -> p t n", n=512)
            vbS = A.a(4 * 512, BF16).rearrange("p (t n) -> p t n", n=512)
            wcs = [A.a(16 * 512, BF16).rearrange("p (k n) -> p k n", n=512)]
            ostg = [A.a(512, F32) for _ in range(2)]
            sqb = A.a(512, BF16)
            rs1 = A.a(512, F32)
            rs2 = A.a(512, F32)
            kn32 = A.a(512, F32)
            sB = [A.a(640, F32) for _ in range(2)]
            pB = [A.a(640, BF16) for _ in range(2)]
            rden = [A.a(128, F32) for _ in range(2)]
            kbc = [A.a(4 * 128, BF16).rearrange("p (t d) -> p t d", d=128) for _ in range(2)]
            vbc = [A.a(4 * 128, BF16).rearrange("p (t d) -> p t d", d=128) for _ in range(2)]
            kbcT = [A.a(512, BF16) for _ in range(2)]

            def band_norm(bi, bn, n, gain_ap, dst_bf, dn, want32):
                R.add("act", lambda e: e.activation(out=sqb[:, 0:n], in_=banks[bi][:, 0:n], func=AF.Square), reads=[bn], writes=["sqb"])
                R.add("pe", lambda e: e.matmul(banks[5][:, 0:n], lhsT=onesb, rhs=sqb[:, 0:n], start=True, stop=True), reads=["sqb", "onesb"], writes=["b5"])
                R.add("act", lambda e: e.activation(out=rs1[:, 0:n], in_=banks[5][:, 0:n], func=AF.Ln, scale=1.0 / 128, bias=epsc), reads=["b5", "epsc"], writes=["rs1"])
                R.add("act", lambda e: e.activation(out=rs2[:, 0:n], in_=rs1[:, 0:n], func=AF.Exp, scale=-0.5), reads=["rs1"], writes=["rs2"])
                if not want32:
                    R.add("dve", lambda e: e.scalar_tensor_tensor(out=dst_bf, in0=banks[bi][:, 0:n], scalar=gain_ap, in1=rs2[:, 0:n], op0=ALU.mult, op1=ALU.mult), reads=[bn, "rs2", "cparam"], writes=[dn])
                else:
                    R.add("dve", lambda e: e.scalar_tensor_tensor(out=kn32[:, 0:n], in0=banks[bi][:, 0:n], scalar=gain_ap, in1=rs2[:, 0:n], op0=ALU.mult, op1=ALU.mult), reads=[bn, "rs2", "cparam"], writes=["kn32"])
                    R.add("act", lambda e: e.copy(out=dst_bf, in_=kn32[:, 0:n]), reads=["kn32"], writes=[dn])

            def band_unit(sid, q_ap, nq, tiles, out_ap):
                z0, zn0 = banks[2 + sid], f"b{2 + sid}"
                dnb, dnn = banks[4 + sid], f"b{4 + sid}"
                ob, on = banks[6 + sid], f"b{6 + sid}"
                G = len(tiles)
                sn, pn, rn = f"sB{sid}", f"pB{sid}", f"rden{sid}"
                zb2, zn2 = banks[sid], f"b{sid}"

                def zslice(i, nk):
                    if i < 4:
                        return z0[0:nk, i * nq:(i + 1) * nq], zn0
                    return zb2[0:nk, 0:nq], zn2

                def s0():
                    for i, (kT, kn, v, vn, nk, b_ap, kb_ap) in enumerate(tiles):
                        zs, zn = zslice(i, nk)
                        R.add("pe", lambda e, zs=zs, kT=kT: e.matmul(zs, lhsT=kT, rhs=q_ap, start=True, stop=True), reads=[kn, "qbT"], writes=[zn])

                def s1():
                    for i, (kT, kn, v, vn, nk, b_ap, kb_ap) in enumerate(tiles):
                        zs, zn = zslice(i, nk)
                        R.add("dve", lambda e, zs=zs, i=i, nk=nk, b_ap=b_ap: e.tensor_tensor(out=sB[sid][0:nk, i * nq:(i + 1) * nq], in0=zs, in1=b_ap, op=ALU.add), reads=[zn, "bt"], writes=[sn])

                def s2():
                    for i, (kT, kn, v, vn, nk, b_ap, kb_ap) in enumerate(tiles):
                        R.add("act", lambda e, i=i, nk=nk, kb_ap=kb_ap: e.activation(out=pB[sid][0:nk, i * nq:(i + 1) * nq], in_=sB[sid][0:nk, i * nq:(i + 1) * nq], func=AF.Exp, bias=kb_ap), reads=[sn, "kbias", "zeroc"], writes=[pn])

                def s3():
                    for i, (kT, kn, v, vn, nk, b_ap, kb_ap) in enumerate(tiles):
                        R.add("pe", lambda e, i=i, nk=nk: e.matmul(dnb[:, 0:nq], lhsT=onesb[0:nk, :], rhs=pB[sid][0:nk, i * nq:(i + 1) * nq], start=(i == 0), stop=(i == G - 1)), reads=[pn, "onesb"], writes=[dnn])
                    for i, (kT, kn, v, vn, nk, b_ap, kb_ap) in enumerate(tiles):
                        R.add("pe", lambda e, i=i, nk=nk, v=v: e.matmul(ob[:, 0:nq], lhsT=v, rhs=pB[sid][0:nk, i * nq:(i + 1) * nq], start=(i == 0), stop=(i == G - 1)), reads=[pn, vn], writes=[on])

                def s4():
                    R.add("dve", lambda e: e.reciprocal(out=rden[sid][:, 0:nq], in_=dnb[:, 0:nq]), reads=[dnn], writes=[rn])

                def s5():
                    R.add("dve", lambda e: e.tensor_tensor(out=out_ap, in0=ob[:, 0:nq], in1=rden[sid][:, 0:nq], op=ALU.mult), reads=[on, rn], writes=["oBT"])

                return [s0, s1, s2, s3, s4, s5]

            FMB = TBS + [(NOWN, 512)]
            for half in range(2):
                for ty in (3, 4, 5):
                    cb = ty * 2 + half
                    wc, wn = load_wc(cb, nbuf=1)
                    if ty in (3, 4):
                        for hh in range(4):
                            h = half * 4 + hh
                            for (c0, n) in (TBS if ty == 3 else FMB):
                                bi, bn = fm_proj(wc, wn, hh, c0, n)
                                if ty == 3:
                                    band_norm(bi, bn, n, qgs[:, h:h + 1], qbT[:, hh, c0:c0 + n], "qbT", False)
                                else:
                                    own = c0 < NOWN
                                    band_norm(bi, bn, n, kg[:, h:h + 1], kbT[:, hh, c0:c0 + n], "kbT", own)
                                    if own:
                                        for j in range(n // 128):
                                            R.add("pe", lambda e, j=j: e.transpose(out=banks[7][:, j * 128:(j + 1) * 128], in_=kn32[:, j * 128:(j + 1) * 128], identity=identf), reads=["kn32", "identf"], writes=["b7"])
                                        i = osi[0] % 2
                                        osi[0] += 1
                                        ob_ = ostg[i]
                                        R.add("act", lambda e, ob_=ob_, n=n: e.copy(out=ob_[:, 0:n], in_=banks[7][:, 0:n]), reads=["b7"], writes=[f"ostg{i}"])
                                        R.add("sp", lambda e, ob_=ob_, n=n, c0=c0, h=h: e.dma_start(out=kbd_out[c0:c0 + n, h * 128:(h + 1) * 128].rearrange("(j p) d -> p j d", p=128), in_=ob_[:, 0:n].rearrange("p (j d) -> p j d", d=128)), reads=[f"ostg{i}"], dma=f"ostg{i}")
                    else:
                        for (c0, n) in TMT + [(NOWN + t4 * 128, 128) for t4 in range(4)]:
                            bi, bn = tm_proj(wc, wn, c0, n)
                            if c0 < NOWN:
                                tm_out(bi, bn, n, vbd_out[c0:c0 + n, half * 512:(half + 1) * 512])
                            if c0 < 1024:
                                t = c0 // 128
                                R.add("dve", lambda e, bi=bi, t=t: e.tensor_copy(out=vbP[:, 4 + t, :], in_=bk(bi)), reads=[bn], writes=["vbP"])
                            elif c0 < NOWN:
                                s = (c0 - 1024) // 64
                                R.add("dve", lambda e, bi=bi, s=s: e.tensor_copy(out=vbS[0:64, s, :], in_=banks[bi][0:64, :]), reads=[bn], writes=["vbS"])
                            else:
                                t4 = (c0 - NOWN) // 128
                                R.add("dve", lambda e, bi=bi, t4=t4: e.tensor_copy(out=vbP[:, t4, :], in_=bk(bi)), reads=[bn], writes=["vbP"])

                def kcol(kt):
                    return NOWN + kt * 128 if kt < 4 else (kt - 4) * 128

                for hp in range(2):
                    streams = []
                    for sid in range(2):
                        hh = hp * 2 + sid
                        h = half * 4 + hh
                        gl = []
                        for t in range(8):
                            tiles = []
                            for j in range(5):
                                kt = 4 + t - j
                                kb_ap = kbias[:, 20 + kt:21 + kt] if kt < 4 else zeroc
                                bidx = h * 4 + (3 if j == 4 else min(j, 2))
                                tiles.append((kbT[:, hh, kcol(kt):kcol(kt) + 128], "kbT", vbP[:, kt, hh * 128:(hh + 1) * 128], "vbP", 128, bt[:, bidx, :], kb_ap))
                            gl.append(band_unit(sid, qbT[:, hh, t * 128:(t + 1) * 128], 128, tiles, oBT[:, h, t * 128:(t + 1) * 128]))
                        streams.append(gl)
                    interleave(streams)
                for s in range(4):
                    for hp in range(2):
                        streams = []
                        for sid in range(2):
                            hh = hp * 2 + sid
                            h = half * 4 + hh
                            R.add("pool", lambda e, sid=sid, h=h, s=s: e.dma_start(out=kbc[sid], in_=cbk[s, :, h * 128:(h + 1) * 128].rearrange("(t p) d -> p t d", p=128)), writes=[f"kbc{sid}"], dma=f"kbc{sid}")
                            R.add("pool", lambda e, sid=sid, h=h, s=s: e.dma_start(out=vbc[sid], in_=cbv[s, :, h * 128:(h + 1) * 128].rearrange("(t p) d -> p t d", p=128)), writes=[f"vbc{sid}"], dma=f"vbc{sid}")
                            pb = banks[sid][:, 0:256].bitcast(BF16)
                            for j in range(4):
                                R.add("pe", lambda e, pb=pb, j=j, sid=sid: e.transpose(out=pb[:, j * 128:(j + 1) * 128], in_=kbc[sid][:, j, :], identity=identb), reads=[f"kbc{sid}", "identb"], writes=[f"b{sid}"])
                            R.add("act", lambda e, pb=pb, sid=sid: e.copy(out=kbcT[sid], in_=pb), reads=[f"b{sid}"], writes=[f"kbcT{sid}"])
                            c0 = 1024 + s * 64
                            tiles = [(kbT[:, hh, c0:c0 + 64], "kbT", vbS[0:64, s, hh * 128:(hh + 1) * 128], "vbS", 64, bt[0:64, h * 4 + 0, 0:64], zeroc[0:64, :])]
                            for m in range(3, -1, -1):
                                jj = 1 if m == 3 else 2
                                tiles.append((kbcT[sid][:, m * 128:(m + 1) * 128], f"kbcT{sid}", vbc[sid][:, m, :], f"vbc{sid}", 128, bt[:, h * 4 + jj, 0:64], zeroc))
                            streams.append([band_unit(sid, qbT[:, hh, c0:c0 + 64], 64, tiles, oBT[:, h, c0:c0 + 64])])
                        interleave(streams)
            DUMP(R, "oBT", oBT.rearrange("p h n -> p (h n)"), 8 * NOWN, BF16, "oBT")
            FENCE()

            A.set(PH0)
            mgT = A.a(16 * NOWN, BF16).rearrange("p (k n) -> p k n", n=NOWN)
            wg_ = [A.a(16 * 128, BF16).rearrange("p (k n) -> p k n", n=128) for _ in range(2)]
            wgb_ = [A.a(16 * 128, BF16).rearrange("p (k n) -> p k n", n=128) for _ in range(2)]
            wpa_ = [A.a(8 * 128, BF16).rearrange("p (k n) -> p k n", n=128) for _ in range(2)]
            wpb_ = [A.a(8 * 128, BF16).rearrange("p (k n) -> p k n", n=128) for _ in range(2)]
            sga = A.a(512, F32)
            sgb = A.a(512, F32)
            m1 = A.a(512, F32)
            m2 = A.a(512, F32)
            for fc in range(16):
                i = fc % 2
                a0, a1 = fc * 128, (fc + 1) * 128
                R.add("pool", lambda e, i=i, a0=a0, a1=a1: e.dma_start(out=wg_[i], in_=w_in[:, 6144 + a0:6144 + a1].rearrange("(k p) n -> p k n", p=128)), writes=[f"wg{i}"], dma=f"wg{i}")
                R.add("pool", lambda e, i=i, a0=a0, a1=a1: e.dma_start(out=wgb_[i], in_=w_in[:, 8192 + a0:8192 + a1].rearrange("(k p) n -> p k n", p=128)), writes=[f"wgb{i}"], dma=f"wgb{i}")
                R.add("pool", lambda e, i=i, a0=a0, a1=a1: e.dma_start(out=wpa_[i], in_=w_psb[:, a0:a1].rearrange("(k p) n -> p k n", p=128)), writes=[f"wpa{i}"], dma=f"wpa{i}")
                R.add("pool", lambda e, i=i, a0=a0, a1=a1: e.dma_start(out=wpb_[i], in_=w_pbd[:, a0:a1].rearrange("(k p) n -> p k n", p=128)), writes=[f"wpb{i}"], dma=f"wpb{i}")
                for (c0, n) in TBS:
                    for k in range(16):
                        R.add("pe", lambda e, i=i, k=k, c0=c0, n=n: e.matmul(banks[0][:, 0:n], lhsT=wg_[i][:, k, :], rhs=hT[:, k, c0:c0 + n], start=(k == 0), stop=(k == 15)), reads=[f"wg{i}", "hT"], writes=["b0"])
                    for k in range(16):
                        R.add("pe", lambda e, i=i, k=k, c0=c0, n=n: e.matmul(banks[1][:, 0:n], lhsT=wgb_[i][:, k, :], rhs=hT[:, k, c0:c0 + n], start=(k == 0), stop=(k == 15)), reads=[f"wgb{i}", "hT"], writes=["b1"])
                    for k in range(8):
                        R.add("pe", lambda e, i=i, k=k, c0=c0, n=n: e.matmul(banks[2][:, 0:n], lhsT=wpa_[i][:, k, :], rhs=oAT[:, k, c0:c0 + n], start=(k == 0), stop=(k == 7)), reads=[f"wpa{i}", "oAT"], writes=["b2"])
                    for k in range(8):
                        R.add("pe", lambda e, i=i, k=k, c0=c0, n=n: e.matmul(banks[3][:, 0:n], lhsT=wpb_[i][:, k, :], rhs=oBT[:, k, c0:c0 + n], start=(k == 0), stop=(k == 7)), reads=[f"wpb{i}", "oBT"], writes=["b3"])
                    R.add("act", lambda e, n=n: e.activation(out=sga[:, 0:n], in_=banks[0][:, 0:n], func=AF.Sigmoid), reads=["b0"], writes=["sga"])
                    R.add("act", lambda e, n=n: e.activation(out=sgb[:, 0:n], in_=banks[1][:, 0:n], func=AF.Sigmoid), reads=["b1"], writes=["sgb"])
                    R.add("dve", lambda e, n=n: e.tensor_tensor(out=m1[:, 0:n], in0=banks[2][:, 0:n], in1=sga[:, 0:n], op=ALU.mult), reads=["b2", "sga"], writes=["m1"])
                    R.add("dve", lambda e, n=n: e.tensor_tensor(out=m2[:, 0:n], in0=banks[3][:, 0:n], in1=sgb[:, 0:n], op=ALU.mult), reads=["b3", "sgb"], writes=["m2"])
                    R.add("dve", lambda e, n=n, fc=fc, c0=c0: e.tensor_tensor(out=mgT[:, fc, c0:c0 + n], in0=m1[:, 0:n], in1=m2[:, 0:n], op=ALU.add), reads=["m1", "m2"], writes=["mgT"])
            DUMP(R, "mgT", mgT.rearrange("p k n -> p (k n)"), 16 * NOWN, BF16, "mgT")
            FENCE()

            A.set(HT0)
            yacc = A.a(16 * NOWN, F32).rearrange("p (k n) -> p k n", n=NOWN)
            h2T = A.a(16 * NOWN, BF16).rearrange("p (k n) -> p k n", n=NOWN)
            A.set(PH0 + 40 * KB)
            xts = [A.a(D, F32) for _ in range(2)]
            wo = [A.a(16 * 128, BF16).rearrange("p (k n) -> p k n", n=128) for _ in range(2)]
            for t in range(10):
                i = t % 2
                R.add("sp", lambda e, i=i, t=t: e.dma_start(out=xts[i], in_=x_tok[(24 + t) * 128:(25 + t) * 128, :]), writes=[f"xx{i}"], dma=f"xx{i}")
                for q4 in range(4):
                    bi, bn = next_bank()
                    for j in range(4):
                        dc = q4 * 4 + j
                        R.add("pe", lambda e, bi=bi, j=j, dc=dc, i=i: e.transpose(out=banks[bi][:, j * 128:(j + 1) * 128], in_=xts[i][:, dc * 128:(dc + 1) * 128], identity=identf), reads=[f"xx{i}", "identf"], writes=[bn])
                    R.add("act", lambda e, bi=bi, q4=q4, t=t: e.copy(out=yacc[:, q4 * 4:(q4 + 1) * 4, t * 128:(t + 1) * 128], in_=banks[bi][:, :].rearrange("p (j n) -> p j n", n=128)), reads=[bn], writes=["yacc"])
            SEG = [(0, 512, 0), (512, 512, 0)] + [(1024 + s * 64, 64, 1 + s) for s in range(4)]
            for dc in range(16):
                i = dc % 2
                R.add("pool", lambda e, i=i, dc=dc: e.dma_start(out=wo[i], in_=w_out[:, dc * 128:(dc + 1) * 128].rearrange("(k p) n -> p k n", p=128)), writes=[f"wo{i}"], dma=f"wo{i}")
                for (c0, n) in TBS:
                    bi, bn = next_bank()
                    for k in range(16):
                        R.add("pe", lambda e, bi=bi, i=i, k=k, c0=c0, n=n: e.matmul(banks[bi][:, 0:n], lhsT=wo[i][:, k, :], rhs=mgT[:, k, c0:c0 + n], start=(k == 0), stop=(k == 15)), reads=[f"wo{i}", "mgT"], writes=[bn])
                    for (s0_, sn_, seq) in SEG:
                        if s0_ < c0 or s0_ >= c0 + n:
                            continue
                        R.add("dve", lambda e, bi=bi, dc=dc, s0_=s0_, sn_=sn_, seq=seq, c0=c0: e.scalar_tensor_tensor(out=yacc[:, dc, s0_:s0_ + sn_], in0=banks[bi][:, s0_ - c0:s0_ - c0 + sn_], scalar=modT[:, 32 + dc, seq:seq + 1], in1=yacc[:, dc, s0_:s0_ + sn_], op0=ALU.mult, op1=ALU.add), reads=[bn, "modT2", "yacc"], writes=["yacc"])
            DUMP(R, "yacc", yacc.rearrange("p k n -> p (k n)"), 16 * NOWN, F32, "yacc")
            FENCE()

            A.set(128 * KB)
            combT = A.a(NOWN, BF16)
            selb = A.a(32 * 128, BF16).rearrange("p (e m) -> p e m", m=128)
            cbc = [A.a(NOWN, BF16) for _ in range(2)]
            mMoE = A.mark()
            sq2 = A.a(512, BF16)
            r1 = A.a(512, F32)
            r2 = A.a(512, F32)
            t32 = A.a(512, F32)
            wr = A.a(16 * 36, BF16).rearrange("p (k n) -> p k n", n=36)
            brt = A.a(36, F32)
            lg = A.a(36, F32)
            wk = A.a(64, F32)
            comb = A.a(32, F32)
            R.add("pool", lambda e: e.dma_start(out=wr, in_=w_rt.rearrange("(k p) n -> p k n", p=128)), writes=["wr"], dma="wr")
            R.add("sp", lambda e: e.dma_start(out=brt, in_=b_rt.partition_broadcast(128)), writes=["brt"], dma="brt")
            R.add("pool", lambda e: e.memset(selb[0:32], 1.0), writes=["selb"])
            R.add("pool", lambda e: e.affine_select(out=selb[0:32], in_=selb[0:32], pattern=[[-1, 32], [0, 128]], compare_op=ALU.is_equal, fill=0.0, base=0, channel_multiplier=1), reads=["selb"], writes=["selb"])
            for (c0, n) in TBS:
                for dc in range(16):
                    R.add("act", lambda e, dc=dc, c0=c0, n=n: e.activation(out=sq2[:, 0:n], in_=yacc[:, dc, c0:c0 + n], func=AF.Square), reads=["yacc"], writes=["sq2"])
                    R.add("pe", lambda e, dc=dc, n=n: e.matmul(banks[2][:, 0:n], lhsT=onesb, rhs=sq2[:, 0:n], start=(dc == 0), stop=(dc == 15)), reads=["sq2", "onesb"], writes=["b2"])
                R.add("act", lambda e, n=n: e.activation(out=r1[:, 0:n], in_=banks[2][:, 0:n], func=AF.Ln, scale=1.0 / D, bias=epsc), reads=["b2", "epsc"], writes=["r1"])
                R.add("act", lambda e, n=n: e.activation(out=r2[:, 0:n], in_=r1[:, 0:n], func=AF.Exp, scale=-0.5), reads=["r1"], writes=["r2"])
                for dc in range(16):
                    R.add("dve", lambda e, dc=dc, c0=c0, n=n: e.tensor_tensor(out=t32[:, 0:n], in0=yacc[:, dc, c0:c0 + n], in1=r2[:, 0:n], op=ALU.mult), reads=["yacc", "r2"], writes=["t32"])
                    for (s0_, sn_, seq) in SEG:
                        if s0_ < c0 or s0_ >= c0 + n:
                            continue
                        R.add("dve", lambda e, dc=dc, s0_=s0_, sn_=sn_, seq=seq, c0=c0: e.tensor_scalar(out=h2T[:, dc, s0_:s0_ + sn_], in0=t32[:, s0_ - c0:s0_ - c0 + sn_], scalar1=gmf[:, dc, seq:seq + 1], scalar2=modT[:, 48 + dc, seq:seq + 1], op0=ALU.mult, op1=ALU.add), reads=["t32", "gmf", "modT2"], writes=["h2T"])
            AXX = mybir.AxisListType.X
            for t in range(10):
                for k in range(16):
                    R.add("pe", lambda e, k=k, t=t: e.matmul(banks[3][:, 0:36], lhsT=h2T[:, k, t * 128:(t + 1) * 128], rhs=wr[:, k, :], start=(k == 0), stop=(k == 15)), reads=["h2T", "wr"], writes=["b3"])
                V = lambda a, b: wk[:, a:b]
                ops = []
                ops.append(lambda e: e.tensor_tensor(out=lg, in0=banks[3][:, 0:36], in1=brt, op=ALU.add))
                ops.append(lambda e: e.reduce_max(out=V(0, 1), in_=lg[:, 0:4], axis=AXX))
                ops.append(lambda e: e.tensor_scalar(out=V(4, 8), in0=lg[:, 0:4], scalar1=V(0, 1), scalar2=None, op0=ALU.is_ge))
                ops.append(lambda e: e.tensor_scalar(out=V(8, 12), in0=lg[:, 0:4], scalar1=V(0, 1), scalar2=None, op0=ALU.subtract))
                for o in ops:
                    R.add("dve", o, reads=["b3", "brt", "lg", "wk"], writes=["lg", "wk"])
                R.add("dve", lambda e: e.memset(V(1, 2), 0.0), reads=["wk"], writes=["wk"])
                R.add("act", lambda e: e.activation(out=V(8, 12), in_=V(8, 12), func=AF.Exp, accum_out=V(1, 2)), reads=["wk"], writes=["wk"])
                ops = []
                ops.append(lambda e: e.reciprocal(out=V(2, 3), in_=V(1, 2)))
                ops.append(lambda e: e.tensor_scalar(out=V(16, 24), in0=lg[:, 4:12], scalar1=V(4, 5), scalar2=None, op0=ALU.mult))
                for g in range(1, 4):
                    ops.append(lambda e, g=g: e.scalar_tensor_tensor(out=V(16, 24), in0=lg[:, 4 + 8 * g:12 + 8 * g], scalar=V(4 + g, 5 + g), in1=V(16, 24), op0=ALU.mult, op1=ALU.add))
                ops.append(lambda e: e.reduce_max(out=V(3, 4), in_=V(16, 24), axis=AXX))
                ops.append(lambda e: e.tensor_scalar(out=V(24, 32), in0=V(16, 24), scalar1=V(3, 4), scalar2=None, op0=ALU.is_ge))
                ops.append(lambda e: e.scalar_tensor_tensor(out=V(32, 40), in0=V(24, 32), scalar=-1e30, in1=V(16, 24), op0=ALU.mult, op1=ALU.add))
                ops.append(lambda e: e.reduce_max(out=V(12, 13), in_=V(32, 40), axis=AXX))
                ops.append(lambda e: e.tensor_scalar(out=V(40, 48), in0=V(32, 40), scalar1=V(12, 13), scalar2=None, op0=ALU.is_ge))
                ops.append(lambda e: e.tensor_tensor(out=V(13, 14), in0=V(12, 13), in1=V(3, 4), op=ALU.subtract))
                for o in ops:
                    R.add("dve", o, reads=["wk", "lg"], writes=["wk"])
                R.add("act", lambda e: e.activation(out=V(13, 14), in_=V(13, 14), func=AF.Exp), reads=["wk"], writes=["wk"])
                ops = []
                ops.append(lambda e: e.tensor_scalar(out=V(14, 15), in0=V(13, 14), scalar1=1.0, scalar2=None, op0=ALU.add))
                ops.append(lambda e: e.reciprocal(out=V(14, 15), in_=V(14, 15)))
                ops.append(lambda e: e.tensor_tensor(out=V(15, 16), in0=V(13, 14), in1=V(14, 15), op=ALU.mult))
                ops.append(lambda e: e.tensor_scalar(out=V(48, 56), in0=V(24, 32), scalar1=V(14, 15), scalar2=None, op0=ALU.mult))
                ops.append(lambda e: e.scalar_tensor_tensor(out=V(48, 56), in0=V(40, 48), scalar=V(15, 16), in1=V(48, 56), op0=ALU.mult, op1=ALU.add))
                ops.append(lambda e: e.tensor_scalar(out=V(48, 56), in0=V(48, 56), scalar1=V(2, 3), scalar2=None, op0=ALU.mult))
                for g in range(4):
                    ops.append(lambda e, g=g: e.tensor_scalar(out=comb[:, g * 8:(g + 1) * 8], in0=V(48, 56), scalar1=V(4 + g, 5 + g), scalar2=None, op0=ALU.mult))
                for o in ops:
                    R.add("dve", o, reads=["wk", "comb"], writes=["wk", "comb"])
                R.add("pe", lambda e: e.transpose(out=banks[3][0:32, 128:256], in_=comb, identity=identf), reads=["comb", "identf"], writes=["b3"])
                R.add("act", lambda e, t=t: e.copy(out=combT[0:32, t * 128:(t + 1) * 128], in_=banks[3][0:32, 128:256]), reads=["b3"], writes=["combT"])
            DUMP(R, "h2T", h2T.rearrange("p k n -> p (k n)"), 16 * NOWN, BF16, "h2T")
            DUMP(R, "combT", combT, NOWN, BF16, "combT")
            FENCE(soft=True)

            wgr = [A.a(16 * 128, BF16).rearrange("p (k n) -> p k n", n=128) for _ in range(3)]
            wur = [A.a(16 * 128, BF16).rearrange("p (k n) -> p k n", n=128) for _ in range(3)]
            wdr = [A.a(4 * 2048, BF16).rearrange("p (k n) -> p k n", n=2048) for _ in range(1)]
            hid = A.a(4 * NOWN, BF16).rearrange("p (k n) -> p k n", n=NOWN)
            sgt = [A.a(512, BF16) for _ in range(2)]
            ui = [0]
            for ex in range(32):
                ci = ex % 2
                di = 0
                for (c0, n) in TBS:
                    R.add("pe", lambda e, ex=ex, c0=c0, n=n: e.matmul(banks[6][:, 0:n], lhsT=selb[0:32, ex, :], rhs=combT[0:32, c0:c0 + n], start=True, stop=True), reads=["selb", "combT"], writes=["b6"])
                    R.add("act", lambda e, ci=ci, c0=c0, n=n: e.copy(out=cbc[ci][:, c0:c0 + n], in_=banks[6][:, 0:n]), reads=["b6"], writes=[f"cbc{ci}"])
                for fc in range(4):
                    u = ui[0] % 3
                    ui[0] += 1
                    R.add("pool", lambda e, ex=ex, fc=fc, u=u: e.dma_start(out=wgr[u], in_=w_gate[ex][:, fc * 128:(fc + 1) * 128].rearrange("(k p) n -> p k n", p=128)), writes=[f"wgr{u}"], dma=f"wgr{u}")
                    R.add("pool", lambda e, ex=ex, fc=fc, u=u: e.dma_start(out=wur[u], in_=w_up[ex][:, fc * 128:(fc + 1) * 128].rearrange("(k p) n -> p k n", p=128)), writes=[f"wur{u}"], dma=f"wur{u}")
                    if fc == 1:
                        R.add("pool", lambda e, ex=ex, di=di: e.dma_start(out=wdr[di], in_=w_down[ex].rearrange("(k p) n -> p k n", p=128)), writes=[f"wd{di}"], dma=f"wd{di}")
                    for bi_, (c0, n) in enumerate(TBS):
                        pg = (bi_ + fc) % 2
                        gb, ub = banks[pg * 2], banks[pg * 2 + 1]
                        gn, un = f"b{pg * 2}", f"b{pg * 2 + 1}"
                        for k in range(16):
                            R.add("pe", lambda e, k=k, u=u, c0=c0, n=n, gb=gb: e.matmul(gb[:, 0:n], lhsT=wgr[u][:, k, :], rhs=h2T[:, k, c0:c0 + n], start=(k == 0), stop=(k == 15)), reads=[f"wgr{u}", "h2T"], writes=[gn])
                        for k in range(16):
                            R.add("pe", lambda e, k=k, u=u, c0=c0, n=n, ub=ub: e.matmul(ub[:, 0:n], lhsT=wur[u][:, k, :], rhs=h2T[:, k, c0:c0 + n], start=(k == 0), stop=(k == 15)), reads=[f"wur{u}", "h2T"], writes=[un])
                        R.add("act", lambda e, pg=pg, n=n, gb=gb: e.activation(out=sgt[pg][:, 0:n], in_=gb[:, 0:n], func=AF.Silu), reads=[gn], writes=[f"sgt{pg}"])
                        R.add("dve", lambda e, pg=pg, n=n, c0=c0, ci=ci: e.tensor_tensor(out=sgt[pg][:, 0:n], in0=sgt[pg][:, 0:n], in1=cbc[ci][:, c0:c0 + n], op=ALU.mult), reads=[f"sgt{pg}", f"cbc{ci}"], writes=[f"sgt{pg}"])
                        R.add("dve", lambda e, pg=pg, n=n, c0=c0, fc=fc, ub=ub: e.tensor_tensor(out=hid[:, fc, c0:c0 + n], in0=ub[:, 0:n], in1=sgt[pg][:, 0:n], op=ALU.mult), reads=[un, f"sgt{pg}"], writes=["hid"])
                for dc in range(16):
                    for bi_, (c0, n) in enumerate(TBS):
                        yb_i = (4, 5, 7)[bi_]
                        yb, yn = banks[yb_i], f"b{yb_i}"
                        for fc in range(4):
                            R.add("pe", lambda e, fc=fc, dc=dc, c0=c0, n=n, yb=yb, di=di: e.matmul(yb[:, 0:n], lhsT=wdr[di][:, fc, dc * 128:(dc + 1) * 128], rhs=hid[:, fc, c0:c0 + n], start=(fc == 0), stop=(fc == 3)), reads=[f"wd{di}", "hid"], writes=[yn])
                        if c0 == 1024:
                            tb_, tn_ = (r1, "r1") if dc % 2 == 0 else (r2, "r2")
                            for sq_ in range(4):
                                R.add("act", lambda e, yb=yb, dc=dc, sq_=sq_, tb_=tb_: e.activation(out=tb_[:, sq_ * 64:(sq_ + 1) * 64], in_=yb[:, sq_ * 64:(sq_ + 1) * 64], func=AF.Copy, scale=modT[:, 80 + dc, 1 + sq_:2 + sq_]), reads=[yn, "modT2"], writes=[tn_])
                            R.add("dve", lambda e, dc=dc, tb_=tb_: e.tensor_tensor(out=yacc[:, dc, 1024:1280], in0=yacc[:, dc, 1024:1280], in1=tb_[:, 0:256], op=ALU.add), reads=[tn_, "yacc"], writes=["yacc"])
                            continue
                        for (s0_, sn_, seq) in SEG:
                            if s0_ < c0 or s0_ >= c0 + n:
                                continue
                            R.add("dve", lambda e, yb=yb, dc=dc, s0_=s0_, sn_=sn_, seq=seq, c0=c0: e.scalar_tensor_tensor(out=yacc[:, dc, s0_:s0_ + sn_], in0=yb[:, s0_ - c0:s0_ - c0 + sn_], scalar=modT[:, 80 + dc, seq:seq + 1], in1=yacc[:, dc, s0_:s0_ + sn_], op0=ALU.mult, op1=ALU.add), reads=[yn, "modT2", "yacc"], writes=["yacc"])
            FENCE()

            A.set(88 * KB)
            yst = [A.a(D, F32) for _ in range(2)]
            for t in range(10):
                i = t % 2
                for q4 in range(4):
                    bi, bn = next_bank()
                    for j in range(4):
                        dc = q4 * 4 + j
                        R.add("pe", lambda e, bi=bi, j=j, dc=dc, t=t: e.transpose(out=banks[bi][:, j * 128:(j + 1) * 128], in_=yacc[:, dc, t * 128:(t + 1) * 128], identity=identf), reads=["yacc", "identf"], writes=[bn])
                    if q4 % 2 == 0:
                        R.add("act", lambda e, bi=bi, q4=q4, i=i: e.copy(out=yst[i][:, q4 * 512:(q4 + 1) * 512], in_=bk(bi)), reads=[bn], writes=[f"yst{i}"])
                    else:
                        R.add("dve", lambda e, bi=bi, q4=q4, i=i: e.tensor_copy(out=yst[i][:, q4 * 512:(q4 + 1) * 512], in_=bk(bi)), reads=[bn], writes=[f"yst{i}"])
                R.add("sp", lambda e, i=i, t=t: e.dma_start(out=y_out[t * 128:(t + 1) * 128, :], in_=yst[i]), reads=[f"yst{i}"], dma=f"yst{i}")
        except _Stop:
            pass
        R.emit(st)
    _DBG['outs'] = dbg_outs
    return nc


_CACHE = {}


def kernel(**inp):
    f = lambda k: np.ascontiguousarray(np.asarray(inp[k], dtype=np.float32))
    xp, xs = f("x_prompt"), f("x_sample")
    csk_, csv_, cbk_, cbv_ = f("cache_sb_k")[0], f("cache_sb_v")[0], f("cache_band_k")[0], f("cache_band_v")[0]
    cp, cs = f("c_prompt"), f("c_sample")
    tab = f("rel_bias_band")[0]
    kl = np.arange(128)[:, None]
    ql = np.arange(128)[None, :]
    btile = np.zeros((128, 24, 128), np.float32)
    for j in range(3):
        idx = np.clip(128 * j + ql - kl, -128, 128) + 128
        for h in range(8):
            btile[:, h * 3 + j, :] = tab[h][idx]
    w_rt = np.ascontiguousarray(np.concatenate([f("w_router_group")[0], f("w_router_expert")[0]], axis=1))
    b_rt = np.ascontiguousarray(np.concatenate([f("b_router_group")[0].reshape(1, 4), f("b_router_expert")[0].reshape(1, 32)], axis=1))
    shared = dict(
        btile=btile, norm_mix=f("norm_mix")[0].reshape(16, 128), norm_ffn=f("norm_ffn")[0].reshape(16, 128),
        w_ada=f("w_ada")[0], b_ada=f("b_ada")[0].reshape(96, 128), w_in=f("w_in")[0],
        q_norm=f("q_norm_band")[0], k_norm=f("k_norm_band")[0], w_psb=f("w_proj_sb")[0], w_pbd=f("w_proj_band")[0],
        w_out=f("w_out")[0], w_rt=w_rt, b_rt=b_rt, w_gate=f("w_gate")[0], w_up=f("w_up")[0], w_down=f("w_down")[0])
    in_maps = []
    for c in range(8):
        b, qi = c // 4, c % 4
        x_tok = np.zeros((34 * 128, D), np.float32)
        npre = 1024 * qi
        if npre:
            x_tok[3072 - npre:3072] = xp[b, 0:npre]
        x_tok[3072:4096] = xp[b, npre:npre + 1024]
        x_tok[4096:4352] = xs[4 * c:4 * c + 4].reshape(256, D)
        kbias = np.zeros((128, 32), np.float32)
        kbias[:, 0:(3072 - npre) // 128] = NEG
        m = dict(shared)
        m.update(x_tok=x_tok, kbias=kbias, c5=np.ascontiguousarray(np.concatenate([cp[b:b + 1], cs[4 * c:4 * c + 4]], axis=0)),
                 csk=np.ascontiguousarray(csk_[4 * c:4 * c + 4].reshape(4, 4096, 1024)), csv=np.ascontiguousarray(csv_[4 * c:4 * c + 4].reshape(4, 4096, 1024)),
                 cbk=np.ascontiguousarray(cbk_[4 * c:4 * c + 4].reshape(4, 512, 1024)), cbv=np.ascontiguousarray(cbv_[4 * c:4 * c + 4].reshape(4, 512, 1024)))
        in_maps.append(m)
    if inp.get("_maps_only"):
        return in_maps
    if "nc" not in _CACHE:
        _CACHE["nc"] = build()
    res = run_bass_kernel_spmd(_CACHE["nc"], in_maps, core_ids=list(range(8)))
    r = res.results
    yp = np.zeros((2, 4096, D), np.float32)
    ys = np.zeros((32, 64, D), np.float32)
    pk = np.zeros((1, 2, 4096, 8, 128), np.float32)
    pv = np.zeros_like(pk)
    pbk = np.zeros((1, 2, 512, 8, 128), np.float32)
    pbv = np.zeros_like(pbk)
    sk = np.zeros((1, 32, 64, 8, 128), np.float32)
    sv = np.zeros_like(sk)
    sbk = np.zeros_like(sk)
    sbv = np.zeros_like(sk)
    for c in range(8):
        b, qi = c // 4, c % 4
        o = r[c]
        sl = slice(1024 * qi, 1024 * qi + 1024)
        yp[b, sl] = o["y_out"][0:1024]
        ys[4 * c:4 * c + 4] = o["y_out"][1024:1280].reshape(4, 64, D)
        pk[0, b, sl] = o["ksb_out"][0:1024].reshape(1024, 8, 128)
        pv[0, b, sl] = o["vsb_out"][0:1024].reshape(1024, 8, 128)
        sk[0, 4 * c:4 * c + 4] = o["ksb_out"][1024:1280].reshape(4, 64, 8, 128)
        sv[0, 4 * c:4 * c + 4] = o["vsb_out"][1024:1280].reshape(4, 64, 8, 128)
        sbk[0, 4 * c:4 * c + 4] = o["kbd_out"][1024:1280].reshape(4, 64, 8, 128)
        sbv[0, 4 * c:4 * c + 4] = o["vbd_out"][1024:1280].reshape(4, 64, 8, 128)
        if qi == 3:
            pbk[0, b] = o["kbd_out"][512:1024].reshape(512, 8, 128)
            pbv[0, b] = o["vbd_out"][512:1024].reshape(512, 8, 128)
    return (yp, ys, pk, pv, pbk, pbv, sk, sv, sbk, sbv)
```
